# Optimizing a Trainium2 kernel written in Bass

```python
import jax, jax.numpy as jnp
from jax import lax
import numpy as np

D_MODEL = 1024
BATCH = 2
SEQ = 8192
DEPTH = 1

RET_HEADS = 4
RET_HEAD_DIM = D_MODEL // 2 // RET_HEADS
RET_WIDTH = RET_HEADS * RET_HEAD_DIM
RET_CHUNK = 128
DN_HEADS = 4
DN_HEAD_DIM = D_MODEL // 2 // DN_HEADS
DN_WIDTH = DN_HEADS * DN_HEAD_DIM
DN_CHUNK = 64
CONV_WIDTH = 4
ROPE_THETA = 10000.0
N_GROUPS = 4
EXPERTS_PER_GROUP = 8
N_EXPERTS = N_GROUPS * EXPERTS_PER_GROUP
TOP_K = 2
D_EXPERT = D_MODEL // 2
N_MOD = 6
NORM_EPS = 1e-6
IN_COLS = 4 * RET_WIDTH + 4 * DN_WIDTH + 2 * DN_HEADS

kernel_name = 'hybrid_retention_gdn_hmoe_adaln'


def rmsnorm(x, w=None):
    xf = x.astype(jnp.float32)
    y = xf * lax.rsqrt(jnp.mean(xf * xf, axis=-1, keepdims=True) + NORM_EPS)
    if w is not None:
        y = y * w.astype(jnp.float32)
    return y.astype(x.dtype)


def l2norm(x):
    return x * lax.rsqrt(jnp.sum(x * x, axis=-1, keepdims=True) + NORM_EPS)


def modulate(h, shift, scale):
    return h * (1.0 + scale[:, None, :]) + shift[:, None, :]


def rotary(x, positions):
    dh = x.shape[-1]
    inv_freq = ROPE_THETA ** (-jnp.arange(0, dh, 2, dtype=jnp.float32) / dh)
    ang = positions.astype(jnp.float32)[..., None] * inv_freq
    cos, sin = jnp.cos(ang)[:, :, None, :], jnp.sin(ang)[:, :, None, :]
    xf = x.astype(jnp.float32)
    x1, x2 = xf[..., : dh // 2], xf[..., dh // 2:]
    return jnp.concatenate([x1 * cos - x2 * sin, x2 * cos + x1 * sin], axis=-1)


def retention(q, k, v):
    B, T, H, Dh = q.shape
    C = RET_CHUNK
    N = T // C
    log_gamma = jnp.log1p(-jnp.power(2.0, -5.0 - jnp.arange(H, dtype=jnp.float32)))
    q = q.astype(jnp.float32).reshape(B, N, C, H, Dh)
    k = (k.astype(jnp.float32) * Dh ** -0.5).reshape(B, N, C, H, Dh)
    v = v.astype(jnp.float32).reshape(B, N, C, H, Dh)
    idx = jnp.arange(C, dtype=jnp.float32)
    rel = idx[:, None] - idx[None, :]
    causal = rel >= 0
    decay = jnp.where(causal[None], jnp.exp(log_gamma[:, None, None] * jnp.where(causal, rel, 0.0)[None]), 0.0)
    scores = jnp.einsum('bnihd,bnjhd->bnhij', q, k) * decay[None, None]
    inner = jnp.einsum('bnhij,bnjhd->bnihd', scores, v)
    k_w = k * jnp.exp(log_gamma[None, :] * (C - 1 - idx)[:, None])[:, :, None]
    incr = jnp.einsum('bnjhk,bnjhv->nbhkv', k_w, v)
    chunk_decay = jnp.exp(log_gamma * C)[None, :, None, None]

    def step(state, incr_n):
        return chunk_decay * state + incr_n, state

    _, s_prev = lax.scan(step, jnp.zeros((B, H, Dh, Dh), jnp.float32), incr)
    q_w = q * jnp.exp(log_gamma[None, :] * (idx + 1.0)[:, None])[:, :, None]
    cross = jnp.einsum('bnihk,nbhkv->bnihv', q_w, s_prev)
    return (inner + cross).reshape(B, T, H, Dh)


def causal_dwconv(x, w):
    k = w.shape[0]
    return lax.conv_general_dilated(x, w[:, None, :].astype(x.dtype), window_strides=(1,), padding=((k - 1, 0),),
                                    dimension_numbers=('NWC', 'WIO', 'NWC'), feature_group_count=x.shape[-1])


def gated_delta_rule(q, k, v, g, beta):
    B, T, H, Dh = q.shape
    C = DN_CHUNK
    N = T // C
    to_chunks = lambda t: t.reshape(B, N, C, H, -1).transpose(0, 3, 1, 2, 4)
    q, k, v = to_chunks(q), to_chunks(k), to_chunks(v)
    g = jnp.cumsum(g.reshape(B, N, C, H).transpose(0, 3, 1, 2), axis=-1)
    beta = beta.reshape(B, N, C, H).transpose(0, 3, 1, 2)
    k_beta = k * beta[..., None]
    v_beta = v * beta[..., None]
    tri_incl = jnp.tril(jnp.ones((C, C), bool))
    tri_strict = jnp.tril(jnp.ones((C, C), bool), -1)
    decay = jnp.exp(jnp.where(tri_incl, g[..., :, None] - g[..., None, :], -jnp.inf))
    a_mat = jnp.where(tri_strict, (k_beta @ jnp.swapaxes(k, -1, -2)) * decay, 0.0)
    eye = jnp.eye(C, dtype=jnp.float32)
    t_mat = lax.linalg.triangular_solve(eye + a_mat, jnp.broadcast_to(eye, a_mat.shape),
                                        left_side=True, lower=True, unit_diagonal=True)
    u = t_mat @ v_beta
    w = t_mat @ (k_beta * jnp.exp(g)[..., None])
    attn = (q @ jnp.swapaxes(k, -1, -2)) * decay
    q_g = q * jnp.exp(g)[..., None]
    k_tail = k * jnp.exp(g[..., -1:] - g)[..., None]
    g_last = jnp.exp(g[..., -1])
    lead = lambda t: jnp.moveaxis(t, 2, 0)

    def step(state, inp):
        u_n, w_n, qg_n, kt_n, attn_n, gl_n = inp
        v_new = u_n - w_n @ state
        o_n = qg_n @ state + attn_n @ v_new
        state = state * gl_n[..., None, None] + jnp.swapaxes(kt_n, -1, -2) @ v_new
        return state, o_n

    _, o = lax.scan(step, jnp.zeros((B, H, Dh, Dh), jnp.float32),
                    (lead(u), lead(w), lead(q_g), lead(k_tail), lead(attn), lead(g_last)))
    return o.transpose(1, 0, 3, 2, 4).reshape(B, T, H, Dh)


def hierarchical_moe(h, w_group, b_group, w_expert, b_expert, w1, w3, w2):
    B, T, D = h.shape
    xf = h.reshape(-1, D)
    n = xf.shape[0]
    group_probs = jax.nn.softmax((xf @ w_group + b_group).astype(jnp.float32), axis=-1)
    p_group, g_idx = lax.top_k(group_probs, 1)
    e_logits = (xf @ w_expert + b_expert).astype(jnp.float32).reshape(n, N_GROUPS, EXPERTS_PER_GROUP)
    e_logits = jnp.take_along_axis(e_logits, g_idx[:, :, None], axis=1)[:, 0]
    p_exp, e_local = lax.top_k(jax.nn.softmax(e_logits, axis=-1), TOP_K)
    gate = p_group * p_exp / jnp.sum(p_exp, axis=-1, keepdims=True)
    flat_e = (g_idx * EXPERTS_PER_GROUP + e_local).reshape(-1)
    order = jnp.argsort(flat_e)
    tok = order // TOP_K
    group_sizes = jnp.bincount(flat_e, length=N_EXPERTS).astype(jnp.int32)
    xs = xf[tok]
    hid = jax.nn.silu(lax.ragged_dot(xs, w1, group_sizes)) * lax.ragged_dot(xs, w3, group_sizes)
    ys = lax.ragged_dot(hid.astype(xs.dtype), w2, group_sizes) * gate.reshape(-1)[order][:, None].astype(xs.dtype)
    return jax.ops.segment_sum(ys, tok, num_segments=n).reshape(B, T, D)


def setup_inputs(seed: int = 0) -> dict:
    key = jax.random.key(seed)
    ks = jax.random.split(key, 24)
    f32 = jnp.float32
    nrm = lambda k, shape, s: jax.random.normal(k, shape, f32) * s
    dt = jnp.exp(jax.random.uniform(ks[9], (DEPTH, DN_HEADS), f32, np.log(1e-3), np.log(1e-1)))
    return {
        'x': nrm(ks[0], (BATCH, SEQ, D_MODEL), 1.0),
        'c': nrm(ks[1], (BATCH, D_MODEL), 1.0),
        'positions': jnp.broadcast_to(jnp.arange(SEQ, dtype=jnp.int32)[None], (BATCH, SEQ)),
        'w_ada': nrm(ks[2], (DEPTH, D_MODEL, N_MOD * D_MODEL), D_MODEL ** -0.5),
        'b_ada': nrm(ks[3], (DEPTH, N_MOD * D_MODEL), 0.01),
        'norm1_w': 1.0 + nrm(ks[4], (DEPTH, D_MODEL), 0.01),
        'w_in': nrm(ks[5], (DEPTH, D_MODEL, IN_COLS), D_MODEL ** -0.5),
        'conv_w': nrm(ks[6], (DEPTH, CONV_WIDTH, 3 * DN_WIDTH), CONV_WIDTH ** -0.5),
        'a_log': jnp.log(jax.random.uniform(ks[7], (DEPTH, DN_HEADS), f32, 1.0, 16.0)),
        'dt_bias': dt + jnp.log(-jnp.expm1(-dt)),
        'dn_norm_w': 1.0 + nrm(ks[8], (DEPTH, DN_HEAD_DIM), 0.01),
        'w_out': nrm(ks[10], (DEPTH, D_MODEL, D_MODEL), D_MODEL ** -0.5),
        'norm2_w': 1.0 + nrm(ks[11], (DEPTH, D_MODEL), 0.01),
        'w_group': nrm(ks[12], (DEPTH, D_MODEL, N_GROUPS), D_MODEL ** -0.5),
        'b_group': nrm(ks[13], (DEPTH, N_GROUPS), 0.01),
        'w_expert': nrm(ks[14], (DEPTH, D_MODEL, N_EXPERTS), D_MODEL ** -0.5),
        'b_expert': nrm(ks[15], (DEPTH, N_EXPERTS), 0.01),
        'w1': nrm(ks[16], (DEPTH, N_EXPERTS, D_MODEL, D_EXPERT), D_MODEL ** -0.5),
        'w3': nrm(ks[17], (DEPTH, N_EXPERTS, D_MODEL, D_EXPERT), D_MODEL ** -0.5),
        'w2': nrm(ks[18], (DEPTH, N_EXPERTS, D_EXPERT, D_MODEL), D_EXPERT ** -0.5),
        'final_norm_w': 1.0 + nrm(ks[19], (D_MODEL,), 0.01),
    }


def reference(x, c, positions, w_ada, b_ada, norm1_w, w_in, conv_w, a_log, dt_bias, dn_norm_w, w_out,
              norm2_w, w_group, b_group, w_expert, b_expert, w1, w3, w2, final_norm_w):
    B, T, D = x.shape
    cuts = [RET_WIDTH, 2 * RET_WIDTH, 3 * RET_WIDTH, 4 * RET_WIDTH,
            4 * RET_WIDTH + DN_WIDTH, 4 * RET_WIDTH + 2 * DN_WIDTH, 4 * RET_WIDTH + 3 * DN_WIDTH,
            4 * RET_WIDTH + 4 * DN_WIDTH, 4 * RET_WIDTH + 4 * DN_WIDTH + DN_HEADS]
    for l in range(DEPTH):
        mod = jax.nn.silu(c) @ w_ada[l] + b_ada[l]
        shift1, scale1, gate1, shift2, scale2, gate2 = jnp.split(mod, N_MOD, axis=-1)

        h = modulate(rmsnorm(x, norm1_w[l]), shift1, scale1)
        proj = h @ w_in[l]
        r_q, r_k, r_v, r_g, d_q, d_k, d_v, d_z, d_a, d_b = jnp.split(proj, cuts, axis=-1)

        heads = lambda t, nh: t.reshape(B, T, nh, -1)
        rq = rotary(heads(r_q, RET_HEADS), positions)
        rk = rotary(heads(r_k, RET_HEADS), positions)
        ret = retention(rq, rk, heads(r_v, RET_HEADS))
        ret = rmsnorm(ret).reshape(B, T, RET_WIDTH) * jax.nn.silu(r_g.astype(jnp.float32))

        qkv = jax.nn.silu(causal_dwconv(jnp.concatenate([d_q, d_k, d_v], axis=-1), conv_w[l]))
        cq, ck, cv = jnp.split(qkv.astype(jnp.float32), 3, axis=-1)
        dq = l2norm(heads(cq, DN_HEADS)) * DN_HEAD_DIM ** -0.5
        dk = l2norm(heads(ck, DN_HEADS))
        beta = jax.nn.sigmoid(d_b.astype(jnp.float32))
        g = -jnp.exp(a_log[l].astype(jnp.float32)) * jax.nn.softplus((d_a + dt_bias[l]).astype(jnp.float32))
        dn = gated_delta_rule(dq, dk, heads(cv, DN_HEADS), g, beta)
        dn = rmsnorm(dn, dn_norm_w[l]).reshape(B, T, DN_WIDTH) * jax.nn.silu(d_z.astype(jnp.float32))

        mix = jnp.concatenate([ret, dn], axis=-1).astype(x.dtype) @ w_out[l]
        x = x + gate1[:, None, :] * mix

        h2 = modulate(rmsnorm(x, norm2_w[l]), shift2, scale2)
        ffn = hierarchical_moe(h2, w_group[l], b_group[l], w_expert[l], b_expert[l], w1[l], w3[l], w2[l])
        x = x + gate2[:, None, :] * ffn.astype(x.dtype)
    return rmsnorm(x, final_norm_w)
```

```python
import math
import os
from contextlib import ExitStack
import numpy as np
import concourse.bass as bass
import concourse.mybir as mybir
from concourse.bass_utils import run_bass_kernel_spmd

F32 = mybir.dt.float32
BF16 = mybir.dt.bfloat16
I32 = mybir.dt.int32
AF = mybir.ActivationFunctionType
ALU = mybir.AluOpType
AX = mybir.AxisListType

D = 1024
NHEAD = 4
EPS = 1e-6
NEXP = 32
DEXP = 512
TWO_PI = 2.0 * math.pi


class Buf:
    def __init__(self, ap, name):
        self.ap = ap
        self.name = name
        self.ws = {}
        self.rs = {}
        self.dsem = None
        self.dcnt = 0
        self.psum = False

    def __getitem__(self, k):
        return self.ap[k]


class Sched:
    EPOCH = 30000

    def __init__(self, nc, es):
        self.nc = nc
        self.es = es
        self.eng = {'pe': nc.tensor, 'act': nc.scalar, 'dve': nc.vector, 'pool': nc.gpsimd, 'sp': nc.sync}
        self.sem = {}
        self.cnt = {}
        self.waited = {e: {} for e in self.eng}
        self.nsem = 0
        self.allsems = {}
        for e in self.eng:
            self._newsem(e)
        self.bufs = []
        self.ninst = {e: 0 for e in self.eng}
        self.cache = {}
        self.pbanks = []
        self.pi = 0

    def _mksem(self, name):
        self.nsem += 1
        s = self.es.enter_context(self.nc.semaphore(name))
        return s

    def _newsem(self, e):
        self.sem[e] = self._mksem(f"c_{e}_{self.nsem}")
        self.cnt[e] = 0

    def sb(self, name, shape, dt, es=None):
        self.uid = getattr(self, 'uid', 0) + 1
        name = f"s{self.uid}_{name}"
        t = (es or self.es).enter_context(self.nc.sbuf_tensor(name, list(shape), dt))
        b = Buf(t, name)
        self.bufs.append(b)
        return b

    def g(self, name, shape, dt):
        if name not in self.cache:
            self.cache[name] = self.sb(name, shape, dt, es=self.cache_es)
        return self.cache[name]

    def mkpsum(self):
        for i in range(8):
            t = self.es.enter_context(self.nc.psum_tensor(f"pb{i}", [128, 512], F32))
            b = Buf(t, f"pb{i}")
            b.psum = True
            self.bufs.append(b)
            self.pbanks.append(b)

    def P(self):
        b = self.pbanks[self.pi % 8]
        self.pi += 1
        return b

    def dram(self, name, shape, dt, kind="Internal"):
        t = self.nc.dram_tensor(name, list(shape), dt, kind=kind).ap()
        b = Buf(t, name)
        self.bufs.append(b)
        return b

    def _deps(self, reads, writes, e=None):
        deps = {}

        def add(d):
            for k, (s, v) in d.items():
                if k not in deps or deps[k][1] < v:
                    deps[k] = (s, v)
        for b in reads:
            add(b.ws)
            if b.psum:
                own = id(self.sem[e]) if e in self.sem else None
                add({k: v for k, v in b.rs.items() if k != own})
        for b in writes:
            add(b.ws)
            add(b.rs)
        return deps

    def _wait(self, e, deps):
        for k, (s, v) in deps.items():
            if e == 'pe' and s is self.sem['pe']:
                continue
            if self.waited[e].get(k, 0) >= v:
                continue
            self.eng[e].wait_ge(s, v)
            self.ninst[e] += 1
            self.waited[e][k] = v

    def _record(self, ev, reads, writes):
        k = id(ev[0])
        for b in reads:
            if k not in b.rs or b.rs[k][1] < ev[1]:
                b.rs[k] = ev
        for b in writes:
            if k not in b.ws or b.ws[k][1] < ev[1]:
                b.ws[k] = ev

    def op(self, e, fn, reads=(), writes=()):
        if e == 'pool' and os.environ.get('KNOPOOL'):
            e = 'dve'
        self._wait(e, self._deps(reads, writes, e))
        ins = fn(self.eng[e])
        if self.cnt[e] >= self.EPOCH:
            self._newsem(e)
        self.cnt[e] += 1
        ins.then_inc(self.sem[e], 1)
        self.ninst[e] += 1
        self._record((self.sem[e], self.cnt[e]), reads, writes)
        return ins

    def dma(self, q, out_ap, in_ap, reads=(), writes=(), sembuf=None, **kw):
        self._wait(q, self._deps(reads, writes))
        if sembuf.dsem is None:
            sembuf.dsem = self._mksem(f"d_{sembuf.name}")
        ins = self.eng[q].dma_start(out=out_ap, in_=in_ap, **kw)
        sembuf.dcnt += 1
        ins.then_inc(sembuf.dsem, 16)
        self.ninst[q] += 1
        self._record((sembuf.dsem, 16 * sembuf.dcnt), reads, writes)
        return ins

    def barrier(self, engines=('pe', 'act', 'dve', 'pool', 'sp')):
        deps = {}
        for b in self.bufs:
            for d in (b.ws, b.rs):
                for k, (s, v) in d.items():
                    if k not in deps or deps[k][1] < v:
                        deps[k] = (s, v)
        for e in engines:
            self._wait(e, deps)

    def act(self, out, in_, func, reads, writes, **kw):
        return self.op('act', lambda e: e.activation(out=out, in_=in_, func=func, **kw), reads, writes)

    def tt(self, eng, out, a, b, op, reads, writes):
        return self.op(eng, lambda e: e.tensor_tensor(out=out, in0=a, in1=b, op=op), reads, writes)

    def ts(self, eng, out, a, s1, op0, reads, writes, s2=None, op1=None):
        if op1 is None:
            return self.op(eng, lambda e: e.tensor_scalar(out=out, in0=a, scalar1=s1, scalar2=None, op0=op0), reads, writes)
        return self.op(eng, lambda e: e.tensor_scalar(out=out, in0=a, scalar1=s1, scalar2=s2, op0=op0, op1=op1), reads, writes)

    def stt(self, out, a, sc, b, op0, op1, reads, writes):
        return self.op('dve', lambda e: e.scalar_tensor_tensor(out=out, in0=a, scalar=sc, in1=b, op0=op0, op1=op1), reads, writes)

    def cp(self, eng, out, in_, reads, writes):
        if eng == 'act':
            return self.act(out, in_, AF.Copy, reads, writes)
        return self.op(eng, lambda e: e.tensor_copy(out=out, in_=in_), reads, writes)

    def mm(self, out, lhsT, rhs, reads, writes, start=True, stop=True):
        return self.op('pe', lambda e: e.matmul(out, lhsT=lhsT, rhs=rhs, start=start, stop=stop), reads, writes)

    def tr(self, out, in_, ident, reads, writes):
        return self.op('pe', lambda e: e.transpose(out=out, in_=in_, identity=ident), reads, writes)

    def rsqrt(self, out, in_, scale, reads_in, tmp, eps=EPS):
        self.act(tmp[0], in_, AF.Ln, list(reads_in) + [self.epsbuf], [tmp[1]], bias=self.epsc[:in_.shape[0], 0:1], scale=scale)
        self.act(out[0], tmp[0], AF.Exp, [tmp[1]], [out[1]], scale=-0.5)


def _consts():
    i = np.arange(128)
    c = {}
    c['ident'] = np.eye(128, dtype=np.float32)
    c['triU'] = (i[:, None] <= i[None, :]).astype(np.float32)
    c['negtriU'] = -c['triU']
    c['negmask'] = np.where(i[None, :] <= i[:, None], 0.0, -1e30).astype(np.float32)
    c['m2L'] = ((i[:, None] == i[None, :] + 1) & (i[:, None] % 2 == 1)).astype(np.float32)
    for s in (4, 8, 16, 32, 64):
        c[f'bm{s}'] = ((i[:, None] // s) == (i[None, :] // s)).astype(np.float32)
    gam = 1.0 - np.power(2.0, -5.0 - np.arange(NHEAD))
    lg = np.log1p(-np.power(2.0, -5.0 - np.arange(NHEAD, dtype=np.float64)))
    rel = i[None, :] - i[:, None]
    for h in range(NHEAD):
        c[f'decT{h}'] = (np.where(rel >= 0, np.exp(lg[h] * np.maximum(rel, 0)), 0.0) * 128 ** -0.5).astype(np.float32)
        c[f'qdec{h}'] = np.broadcast_to(np.exp(lg[h] * (i + 1.0))[None, :], (128, 128)).astype(np.float32).copy()
    kws = np.stack([np.exp(lg[h] * (127 - i)) * 128 ** -0.5 for h in range(NHEAD)], axis=1)
    cd = [float(np.exp(lg[h] * 128)) for h in range(NHEAD)]
    invf = (10000.0 ** (-(np.arange(0, 128, 2, dtype=np.float32)) / 128.0)).astype(np.float32)
    col = np.zeros((128, 8), np.float32)
    col[:, 0] = np.concatenate([invf, invf])
    col[:, 1] = np.concatenate([-np.ones(64), np.ones(64)])
    col[:, 2] = EPS
    col[:, 3] = math.pi / 2
    col[:, 4:8] = kws
    names = ['ident', 'triU', 'negtriU', 'negmask', 'm2L', 'bm4', 'bm8', 'bm16', 'bm32', 'bm64'] + \
            [f'decT{h}' for h in range(NHEAD)] + [f'qdec{h}' for h in range(NHEAD)]
    big = np.concatenate([c[n] for n in names], axis=1).astype(np.float32)
    return names, big, col, cd


CNAMES, CBIG, CCOL, CD = _consts()


def build(T, TS, SBW=256, dbg=False):
    NH = NHEAD
    NSB = T // SBW
    NT = SBW // 128
    NCOL = NH * 10 * 128
    NE = int(os.environ.get('KNEXP', NEXP))
    nc = bass.Bass("TRN2", target_bir_lowering=False)
    es = ExitStack()
    with es:
        S = Sched(nc, es)
        S.mkpsum()
        dt_in = {}

        def din(name, shape, dt=F32):
            dt_in[name] = nc.dram_tensor(name, list(shape), dt, kind="ExternalInput").ap()
            return dt_in[name]
        xT = din("xT", [D, T])
        xs = din("xs", [TS, D])
        pos = din("pos", [1, T], I32)
        cT = din("cT", [128, 8])
        w_ada = din("w_ada", [D, 6 * D])
        b_adaT = din("b_adaT", [128, 48])
        n12 = din("n12", [128, 16])
        w_inA = din("w_inA", [D, NCOL])
        w_ab = din("w_ab", [D, 2 * NH])
        convw = din("convw", [128, NH * 12])
        hv = din("hv", [128, 2 * NH])
        dnw = din("dnw", [128, 1])
        w_out = din("w_out", [D, D])
        w_r = din("w_r", [D, 36])
        b_r = din("b_r", [128, 36])
        w1 = din("w1", [NE, D, DEXP])
        w3 = din("w3", [NE, D, DEXP])
        w2 = din("w2", [NE, DEXP, D])
        fnw = din("fnw", [128, D])
        cbig = din("cbig", [128, CBIG.shape[1]])
        ccol = din("ccol", [128, 8])
        selm_in = din("selm", [128, 8])
        out = nc.dram_tensor("out", [TS, D], F32, kind="ExternalOutput").ap()
        x1_d = S.dram("x1_d", [TS, D], F32)
        dbg_outs = {}

        cb = S.sb("cbig", [128, CBIG.shape[1]], F32)
        S.dma('sp', cb[:], cbig[:, :], writes=[cb], sembuf=cb)
        cc = S.sb("ccol", [128, 8], F32)
        S.dma('sp', cc[:], ccol[:, :], writes=[cc], sembuf=cc)
        S.epsc = cc.ap[:, 2:3]
        S.epsbuf = cc
        C = {n: cb.ap[:, k * 128:(k + 1) * 128] for k, n in enumerate(CNAMES)}
        ident_b = S.sb("ident_b", [128, 128], BF16)
        S.cp('dve', ident_b[:], C['ident'], [cb], [ident_b])
        ones_b = S.sb("ones_b", [128, 128], BF16)
        S.op('pool', lambda e: e.memset(ones_b[:], 1.0), [], [ones_b])
        ones_f = S.sb("ones_f", [128, 128], F32)
        S.op('pool', lambda e: e.memset(ones_f[:], 1.0), [], [ones_f])
        bmb = {}
        for s_ in (4, 8, 16, 32, 64):
            bmb[s_] = S.sb(f"bmb{s_}", [128, 128], BF16)
            S.cp('dve', bmb[s_][:], C[f'bm{s_}'], [cb], [bmb[s_]])
        m2Lb = S.sb("m2Lb", [128, 128], BF16)
        S.cp('dve', m2Lb[:], C['m2L'], [cb], [m2Lb])
        selm = S.sb("selm", [128, 8], F32)
        S.dma('sp', selm[:], selm_in[:, :], writes=[selm], sembuf=selm)
        catT = S.sb("catT", [128, 8, TS], BF16)
        S.op('pool', lambda e: e.memset(catT[:], 0.0), [], [catT])
        mod = S.sb("mod", [128, 48], F32)
        a1 = S.sb("a1", [128, 8], F32)
        a2 = S.sb("a2", [128, 8], F32)
        g12 = S.sb("g12", [128, 2 * D], F32)

        with ExitStack() as es0:
            S.cache_es = es0
            ct = S.sb("ct", [128, 8], F32, es0)
            S.dma('sp', ct[:], cT[:, :], writes=[ct], sembuf=ct)
            sct = S.sb("sct", [128, 8], F32, es0)
            S.act(sct[:], ct[:], AF.Silu, [ct], [sct])
            bad = S.sb("bad", [128, 48], F32, es0)
            S.dma('sp', bad[:], b_adaT[:, :], writes=[bad], sembuf=bad)
            n12t = S.sb("n12t", [128, 16], F32, es0)
            S.dma('sp', n12t[:], n12[:, :], writes=[n12t], sembuf=n12t)
            pm = S.P()
            for j in range(6):
                wa = S.g(f"wada{j % 2}", [128, 8, D], F32)
                S.dma('sp', wa[:], w_ada[:, j * D:(j + 1) * D].rearrange("(c p) n -> p c n", p=128), writes=[wa], sembuf=wa)
                for oc in range(8):
                    col_ = j * 8 + oc
                    for c in range(8):
                        S.mm(pm[:, col_:col_ + 1], wa[:, c, oc * 128:(oc + 1) * 128], sct[:, c:c + 1], [wa, sct], [pm],
                             start=(c == 0), stop=(c == 7))
            S.tt('dve', mod[:], pm[:, 0:48], bad[:], ALU.add, [pm, bad], [mod])
            S.stt(a1[:], mod[:, 8:16], 1.0, n12t[:, 0:8], ALU.add, ALU.mult, [mod, n12t], [a1])
            S.stt(a2[:], mod[:, 32:40], 1.0, n12t[:, 8:16], ALU.add, ALU.mult, [mod, n12t], [a2])
            for gi, base in ((0, 16), (1, 40)):
                for c in range(8):
                    dg = S.g(f"dg{c % 2}", [128, 128], F32)
                    S.ts('dve', dg[:], C['ident'], mod[:, base + c:base + c + 1], ALU.mult, [cb, mod], [dg])
                    pg = S.P()
                    S.mm(pg[:, 0:128], ones_f[:], dg[:], [ones_f, dg], [pg])
                    S.cp('act', g12[:, gi * D + c * 128:gi * D + (c + 1) * 128], pg[:, 0:128], [pg], [g12])
            S.barrier()
        S.cache = {}
        if os.environ.get('KSTOP') == 'pre':
            return nc

        for phase in ('ret', 'gdn'):
            with ExitStack() as esA:
                S.cache_es = esA
                G = S.g
                if phase == 'ret':
                    PC0, PCN = 0, NH * 6 * 128
                else:
                    PC0, PCN = NH * 6 * 128, NH * 4 * 128
                Win = S.sb("Win" + phase, [128, 8, PCN], BF16, esA)
                for c in range(8):
                    S.dma('pool', Win[:, c, :], w_inA[c * 128:(c + 1) * 128, PC0:PC0 + PCN], writes=[Win], sembuf=Win)
                Wab = S.sb("Wab" + phase, [128, 8, 2 * NH], BF16, esA)
                S.dma('pool', Wab[:], w_ab.rearrange("(c p) n -> p c n", p=128), writes=[Wab], sembuf=Wab)
                cw = S.sb("cw" + phase, [128, NH * 12], F32, esA)
                S.dma('sp', cw[:], convw[:, :], writes=[cw], sembuf=cw)
                hvt = S.sb("hvt" + phase, [128, 2 * NH], F32, esA)
                S.dma('sp', hvt[:], hv[:, :], writes=[hvt], sembuf=hvt)
                dnwt = S.sb("dnwt" + phase, [128, 1], F32, esA)
                S.dma('sp', dnwt[:], dnw[:, :], writes=[dnwt], sembuf=dnwt)
                nea = S.sb("nea" + phase, [128, NH], F32, esA)
                S.act(nea[:], hvt[:, 0:NH], AF.Exp, [hvt], [nea])
                S.ts('dve', nea[:], nea[:], -1.0, ALU.mult, [nea], [nea])
                Sr, Srb, Sg, Sgb, cbuf = [], [], [], [], []
                for h in range(NH):
                    for lst, nm, dt in ((Sr, "Sr", F32), (Srb, "Srb", BF16), (Sg, "Sg", F32), (Sgb, "Sgb", BF16)):
                        b = S.sb(f"{nm}{h}{phase}", [128, 128], dt, esA)
                        S.op('pool', lambda e, b=b: e.memset(b[:], 0.0), [], [b])
                        lst.append(b)
                    row = []
                    for w_ in range(3):
                        b = S.sb(f"cbuf{h}_{w_}{phase}", [128, SBW + 3], F32, esA)
                        S.op('pool', lambda e, b=b: e.memset(b[:], 0.0), [], [b])
                        row.append(b)
                    cbuf.append(row)

                def proj(hT, col):
                    ps = S.P()
                    for c in range(8):
                        S.mm(ps[:, 0:SBW], Win[:, c, col - PC0:col - PC0 + 128], hT[:, c, :], [Win, hT], [ps], start=(c == 0), stop=(c == 7))
                    return ps

                for sb in range(NSB):
                    t0 = sb * SBW
                    xt = G("xt", [128, 8, SBW], F32)
                    S.dma('sp', xt[:], xT[:, t0:t0 + SBW].rearrange("(c p) t -> p c t", p=128), writes=[xt], sembuf=xt)
                    xsq = G("xsq", [128, 8, SBW], BF16)
                    S.act(xsq[:], xt[:], AF.Square, [xt], [xsq])
                    pss = S.P()
                    for c in range(8):
                        S.mm(pss[:, 0:SBW], ones_b[:], xsq[:, c, :], [ones_b, xsq], [pss], start=(c == 0), stop=(c == 7))
                    lnt = G("lnt", [128, SBW], F32)
                    rstd = G("rstd", [128, SBW], F32)
                    S.rsqrt((rstd[:], rstd), pss[:, 0:SBW], 1.0 / D, [pss], (lnt[:], lnt))
                    hT = G("hT", [128, 8, SBW], BF16)
                    for c in range(8):
                        tmp = G(f"xn{c % 2}", [128, SBW], F32)
                        S.tt('dve' if c % 2 == 0 else 'pool', tmp[:], xt[:, c, :], rstd[:], ALU.mult, [xt, rstd], [tmp])
                        S.act(hT[:, c, :], tmp[:], AF.Identity, [tmp, a1, mod], [hT], bias=mod[:, c:c + 1], scale=a1[:, c:c + 1])
                    if os.environ.get('KSTOP') == 'ret1':
                        S.barrier(engines=('sp',))
                        return nc
                    if phase == 'ret':
                        posi = G("posi", [128, SBW], I32)
                        S.dma('sp', posi[:], pos[0:1, t0:t0 + SBW].partition_broadcast(128), writes=[posi], sembuf=posi)
                        posf = G("posf", [128, SBW], F32)
                        S.cp('dve', posf[:], posi[:], [posi], [posf])
                        ang = G("ang", [128, SBW], F32)
                        S.ts('dve', ang[:], posf[:], cc[:, 0:1], ALU.mult, [posf, cc], [ang])
                        tabs = []
                        for which in range(2):
                            if which == 1:
                                ang2 = G("ang2", [128, SBW], F32)
                                S.ts('pool', ang2[:], ang[:], math.pi / 2, ALU.add, [ang], [ang2])
                                a_ = ang2
                            else:
                                a_ = ang
                            ki = G(f"ki{which}", [128, SBW], I32)
                            S.ts('dve', ki[:], a_[:], 1.0 / TWO_PI, ALU.mult, [a_], [ki])
                            kf = G(f"kf{which}", [128, SBW], F32)
                            S.cp('pool', kf[:], ki[:], [ki], [kf])
                            rr = G(f"rr{which}", [128, SBW], F32)
                            S.stt(rr[:], kf[:], -TWO_PI, a_[:], ALU.mult, ALU.add, [kf, a_], [rr])
                            tb = G(f"tab{which}", [128, SBW], F32)
                            S.act(tb[:], rr[:], AF.Sin, [rr], [tb])
                            tabs.append(tb)
                        sint, cost = tabs
                        sins = G("sins", [128, SBW], F32)
                        S.ts('pool', sins[:], sint[:], cc[:, 1:2], ALU.mult, [sint, cc], [sins])

                        if os.environ.get('KSTOP') == 'ret2':
                            S.barrier(engines=('sp',))
                            return nc
                        for h in range(NH):
                            base = h * 6 * 128
                            for nm, off in (("q", 0), ("k", 2)):
                                p1 = proj(hT, base + off * 128)
                                p2 = proj(hT, base + (off + 1) * 128)
                                t1 = G("rt1", [128, SBW], F32)
                                t2 = G("rt2", [128, SBW], F32)
                                S.tt('dve', t1[:], p1[:, 0:SBW], cost[:], ALU.mult, [p1, cost], [t1])
                                S.tt('dve', t2[:], p2[:, 0:SBW], sins[:], ALU.mult, [p2, sins], [t2])
                                o_ = G(f"r{nm}T{h}", [128, SBW], BF16)
                                S.tt('pool', o_[:], t1[:], t2[:], ALU.add, [t1, t2], [o_])
                            pv = proj(hT, base + 4 * 128)
                            vT = G(f"rvT{h}", [128, SBW], BF16)
                            S.cp('act', vT[:], pv[:, 0:SBW], [pv], [vT])
                            pg_ = proj(hT, base + 5 * 128)
                            sgT = G(f"rsgT{h}", [128, SBW], F32)
                            S.act(sgT[:], pg_[:, 0:SBW], AF.Silu, [pg_], [sgT])
                        if os.environ.get('KSTOP') == 'ret3':
                            S.barrier(engines=('sp',))
                            return nc
                        for tt in range(NT):
                            sl = slice(tt * 128, (tt + 1) * 128)
                            for h in range(NH):
                                qrT, krT, vT, sgT = (S.cache[f"rqT{h}"], S.cache[f"rkT{h}"], S.cache[f"rvT{h}"], S.cache[f"rsgT{h}"])
                                pk = S.P()
                                pkb = pk.ap[:].bitcast(BF16)
                                S.tr(pkb[:, 0:128], krT[:, sl], ident_b[:], [krT, ident_b], [pk])
                                S.tr(pkb[:, 128:256], vT[:, sl], ident_b[:], [vT, ident_b], [pk])
                                if os.environ.get('KSTOP') == 'r3a0':
                                    S.barrier(engines=('sp',))
                                    return nc
                                kw = G(f"rkw{h}", [128, 128], BF16)
                                S.ts('dve', kw[:], pkb[:, 0:128], cc[:, 4 + h:5 + h], ALU.mult, [pk, cc], [kw])
                                if os.environ.get('KSTOP') == 'r3a1':
                                    S.barrier(engines=('sp',))
                                    return nc
                                vtok = G(f"rvtok{h}", [128, 128], BF16)
                                S.cp('act', vtok[:], pkb[:, 128:256], [pk], [vtok])
                                if os.environ.get('KSTOP') == 'r3a':
                                    S.barrier(engines=('sp',))
                                    return nc
                                psc = S.P()
                                S.mm(psc[:, 0:128], krT[:, sl], qrT[:, sl], [krT, qrT], [psc])
                                sT = G(f"rsT{h}", [128, 128], BF16)
                                S.tt('dve', sT[:], psc[:, 0:128], C[f'decT{h}'], ALU.mult, [psc, cb], [sT])
                                qwT = G(f"rqw{h}", [128, 128], BF16)
                                S.tt('pool', qwT[:], qrT[:, sl], C[f'qdec{h}'], ALU.mult, [qrT, cb], [qwT])
                                if os.environ.get('KSTOP') == 'r3b':
                                    S.barrier(engines=('sp',))
                                    return nc
                                po = S.P()
                                S.mm(po[:, 0:128], sT[:], vtok[:], [sT, vtok], [po], start=True, stop=False)
                                S.mm(po[:, 0:128], qwT[:], Srb[h][:], [qwT, Srb[h]], [po], start=False, stop=True)
                                S.mm(po[:, 128:256], kw[:], vtok[:], [kw, vtok], [po])
                                S.stt(Sr[h][:], Sr[h][:], CD[h], po[:, 128:256], ALU.mult, ALU.add, [Sr[h], po], [Sr[h]])
                                S.cp('act', Srb[h][:], Sr[h][:], [Sr[h]], [Srb[h]])
                                if os.environ.get('KSTOP') == 'r3c':
                                    S.barrier(engines=('sp',))
                                    return nc
                                junk = G(f"rjunk{h}", [128, 128], F32)
                                ssq = G(f"rssq{h}", [128, 4], F32)
                                S.op('pool', lambda e, ssq=ssq: e.memset(ssq[:], 0.0), [], [ssq])
                                S.act(junk[:], po[:, 0:128], AF.Square, [po], [junk, ssq], accum_out=ssq[:, 0:1])
                                S.rsqrt((ssq[:, 2:3], ssq), ssq[:, 0:1], 1.0 / 128, [ssq], (ssq[:, 1:2], ssq))
                                if os.environ.get('KSTOP') == 'r3d':
                                    S.barrier(engines=('sp',))
                                    return nc
                                on = G(f"ron{h}", [128, 128], F32)
                                S.ts('dve', on[:], po[:, 0:128], ssq[:, 2:3], ALU.mult, [po, ssq], [on])
                                pt = S.P()
                                S.tr(pt[:, 0:128], on[:], C['ident'], [on, cb], [pt])
                                cst = G(f"rcst{h}", [128, SBW], BF16)
                                S.tt('dve', cst[:, sl], pt[:, 0:128], sgT[:, sl], ALU.mult, [pt, sgT], [cst])
                        if os.environ.get('KSTOP') == 'ret4':
                            S.barrier(engines=('sp',))
                            return nc
                        for h in range(NH):
                            cst = S.cache[f"rcst{h}"]
                            S.stt(catT[:, 2 * h, t0 % TS:t0 % TS + SBW], cst[:], selm[:, t0 // TS:t0 // TS + 1], catT[:, 2 * h, t0 % TS:t0 % TS + SBW],
                                  ALU.mult, ALU.add, [cst, selm, catT], [catT])

                    if phase == 'gdn':
                        for h in range(NH):
                            base = NH * 6 * 128 + h * 4 * 128
                            for w_, nm in enumerate(("q", "k", "v")):
                                ps = proj(hT, base + w_ * 128)
                                cbf = cbuf[h][w_]
                                S.cp('act', cbf[:, 3:3 + SBW], ps[:, 0:SBW], [ps], [cbf])
                                acc = G(f"gacc{w_}", [128, SBW], F32)
                                wc = h * 12 + w_ * 4
                                S.ts('pool', acc[:], cbf[:, 0:SBW], cw[:, wc:wc + 1], ALU.mult, [cbf, cw], [acc])
                                for j in range(1, 4):
                                    S.stt(acc[:], cbf[:, j:j + SBW], cw[:, wc + j:wc + j + 1], acc[:], ALU.mult, ALU.add, [cbf, cw, acc], [acc])
                                tl = G(f"gtail{w_}", [128, 4], F32)
                                S.cp('pool', tl[:, 0:3], cbf[:, SBW:SBW + 3], [cbf], [tl])
                                S.cp('pool', cbf[:, 0:3], tl[:, 0:3], [tl], [cbf])
                                if nm == "v":
                                    vT = G(f"gvT{h}", [128, SBW], BF16)
                                    S.act(vT[:], acc[:], AF.Silu, [acc], [vT])
                                else:
                                    y = G(f"gy{w_}", [128, SBW], F32)
                                    S.act(y[:], acc[:], AF.Silu, [acc], [y])
                                    sq = G(f"gsq{w_}", [128, SBW], BF16)
                                    S.act(sq[:], y[:], AF.Square, [y], [sq])
                                    pn = S.P()
                                    S.mm(pn[:, 0:SBW], ones_b[:], sq[:], [ones_b, sq], [pn])
                                    lnn = G(f"glnn{w_}", [128, SBW], F32)
                                    rn = G(f"grn{w_}", [128, SBW], F32)
                                    S.rsqrt((rn[:], rn), pn[:, 0:SBW], 1.0, [pn], (lnn[:], lnn))
                                    o_ = G(f"g{nm}nT{h}", [128, SBW], BF16)
                                    if nm == "q":
                                        S.stt(o_[:], y[:], 128 ** -0.5, rn[:], ALU.mult, ALU.mult, [y, rn], [o_])
                                    else:
                                        S.tt('pool', o_[:], y[:], rn[:], ALU.mult, [y, rn], [o_])
                            pz = proj(hT, base + 3 * 128)
                            szT = G(f"gszT{h}", [128, SBW], F32)
                            S.act(szT[:], pz[:, 0:SBW], AF.Silu, [pz], [szT])
                        chains = []
                        for tt in range(NT):
                            sl = slice(tt * 128, (tt + 1) * 128)
                            pab = S.P()
                            for c in range(8):
                                S.mm(pab[:, 0:2 * NH], hT[:, c, sl], Wab[:, c, :], [hT, Wab], [pab], start=(c == 0), stop=(c == 7))
                            sc = G(f"gsc{tt}", [128, 64], F32)
                            S.tt('dve', sc[:, 0:4], pab[:, 0:NH], hvt[:, NH:2 * NH], ALU.add, [pab, hvt], [sc])
                            S.act(sc[:, 4:8], sc[:, 0:4], AF.Exp, [sc], [sc])
                            S.act(sc[:, 8:12], sc[:, 4:8], AF.Ln, [sc], [sc], bias=1.0)
                            S.tt('dve', sc[:, 12:16], sc[:, 8:12], nea[:], ALU.mult, [sc, nea], [sc])
                            S.act(sc[:, 16:20], pab[:, NH:2 * NH], AF.Exp, [pab], [sc], scale=-1.0)
                            S.ts('dve', sc[:, 16:20], sc[:, 16:20], 1.0, ALU.add, [sc], [sc])
                            S.op('dve', lambda e, sc=sc: e.reciprocal(out=sc[:, 20:24], in_=sc[:, 16:20]), [sc], [sc])
                            pgc = S.P()
                            S.mm(pgc[:, 0:NH], C['triU'], sc[:, 12:16], [cb, sc], [pgc])
                            S.mm(pgc[:, 8:8 + NH], ones_f[:], sc[:, 12:16], [ones_f, sc], [pgc])
                            S.cp('dve', sc[:, 24:28], pgc[:, 0:NH], [pgc], [sc])
                            S.cp('dve', sc[:, 28:32], pgc[:, 8:8 + NH], [pgc], [sc])
                            S.act(sc[:, 32:36], sc[:, 24:28], AF.Exp, [sc], [sc])
                            S.act(sc[:, 36:40], sc[:, 28:32], AF.Exp, [sc], [sc])
                            S.tt('dve', sc[:, 40:44], sc[:, 28:32], sc[:, 24:28], ALU.subtract, [sc], [sc])
                            S.act(sc[:, 44:48], sc[:, 40:44], AF.Exp, [sc], [sc])
                            S.tt('dve', sc[:, 48:52], sc[:, 20:24], sc[:, 32:36], ALU.mult, [sc], [sc])
                            for h in range(NH):
                                key = f"{tt}_{h}"
                                ch_i = tt * NH + h
                                qnT, knT, vT = S.cache[f"gqnT{h}"], S.cache[f"gknT{h}"], S.cache[f"gvT{h}"]
                                gb = G(f"ggb{ch_i % 3}", [128, 128], F32)
                                S.ts('pool', gb[:], ones_f[:], sc[:, 12 + h:13 + h], ALU.mult, [ones_f, sc], [gb])
                                pG = S.P()
                                S.mm(pG[:, 0:128], C['triU'], gb[:], [cb, gb], [pG], start=True, stop=False)
                                S.mm(pG[:, 0:128], gb[:], C['negtriU'], [cb, gb], [pG], start=False, stop=True)
                                ex = G(f"gex{ch_i % 3}", [128, 128], F32)
                                S.stt(ex[:], pG[:, 0:128], 0.0, C['negmask'], ALU.min, ALU.add, [pG, cb], [ex])
                                dec_i = G(f"gdi{ch_i % 3}", [128, 128], F32)
                                S.act(dec_i[:], ex[:], AF.Exp, [ex], [dec_i])
                                dec_s = G(f"gds{ch_i % 3}", [128, 128], F32)
                                S.tt('pool', dec_s[:], dec_i[:], C['ident'], ALU.subtract, [dec_i, cb], [dec_s])
                                pK = S.P()
                                S.mm(pK[:, 0:128], knT[:, sl], knT[:, sl], [knT], [pK])
                                S.mm(pK[:, 128:256], qnT[:, sl], knT[:, sl], [qnT, knT], [pK])
                                A = G(f"gA{key}", [128, 128], BF16)
                                S.stt(A[:], pK[:, 0:128], sc[:, 20 + h:21 + h], dec_s[:], ALU.mult, ALU.mult, [pK, sc, dec_s], [A])
                                attn = G(f"gattn{ch_i % 3}", [128, 128], BF16)
                                S.tt('dve', attn[:], pK[:, 128:256], dec_i[:], ALU.mult, [pK, dec_i], [attn])
                                pT = S.P()
                                pTb = pT.ap[:].bitcast(BF16)
                                S.tr(pTb[:, 0:128], attn[:], ident_b[:], [attn, ident_b], [pT])
                                S.tr(pTb[:, 128:256], knT[:, sl], ident_b[:], [knT, ident_b], [pT])
                                S.tr(pTb[:, 256:384], vT[:, sl], ident_b[:], [vT, ident_b], [pT])
                                attnT = G(f"gattnT{key}", [128, 128], BF16)
                                S.cp('act', attnT[:], pTb[:, 0:128], [pT], [attnT])
                                kbg = G(f"gkbg{key}", [128, 128], BF16)
                                S.act(kbg[:], pTb[:, 128:256], AF.Identity, [pT, sc], [kbg], scale=sc[:, 48 + h:49 + h])
                                kt = G(f"gkt{key}", [128, 128], BF16)
                                S.ts('dve', kt[:], pTb[:, 128:256], sc[:, 44 + h:45 + h], ALU.mult, [pT, sc], [kt])
                                vb = G(f"gvb{key}", [128, 128], BF16)
                                S.act(vb[:], pTb[:, 256:384], AF.Identity, [pT, sc], [vb], scale=sc[:, 20 + h:21 + h])
                                tm = G(f"gtm{ch_i % 3}", [128, 128], BF16)
                                S.tt('pool', tm[:], A[:], m2Lb[:], ALU.mult, [A, m2Lb], [tm])
                                T1 = G(f"gT0{key}", [128, 128], BF16)
                                S.tt('pool', T1[:], ident_b[:], tm[:], ALU.subtract, [ident_b, tm], [T1])
                                pU = S.P()
                                pUb = pU.ap[:].bitcast(BF16)
                                S.tr(pUb[:, 0:128], T1[:], ident_b[:], [T1, ident_b], [pU])
                                U = G(f"gU0{key}", [128, 128], BF16)
                                S.cp('act', U[:], pUb[:, 0:128], [pU], [U])
                                chains.append(dict(key=key, tt=tt, h=h, sl=sl, A=A, attnT=attnT, kbg=kbg, kt=kt, vb=vb, U=U, Tk=T1, sc=sc))
                        for lvl, bs in enumerate((4, 8, 16, 32, 64, None)):
                            for ch_i, ch in enumerate(chains):
                                key = ch['key']
                                A, U, Tk = ch['A'], ch['U'], ch['Tk']
                                pW = S.P()
                                S.mm(pW[:, 0:128], A[:], U[:], [A, U], [pW])
                                W = G(f"gW{key}", [128, 128], BF16)
                                S.tt('dve', W[:], pW[:, 0:128], U[:], ALU.add, [pW, U], [W])
                                S.mm(pW[:, 128:256], Tk[:], W[:], [Tk, W], [pW])
                                Un = G(f"gU{(lvl + 1) % 2}{key}", [128, 128], BF16)
                                if bs is not None:
                                    tmpf = G(f"gtf{ch_i % 3}", [128, 128], F32)
                                    S.stt(tmpf[:], U[:], 2.0, pW[:, 128:256], ALU.mult, ALU.subtract, [U, pW], [tmpf])
                                    S.tt('pool', Un[:], tmpf[:], C[f'bm{bs}'], ALU.mult, [tmpf, cb], [Un])
                                    pX = S.P()
                                    pXb = pX.ap[:].bitcast(BF16)
                                    S.tr(pXb[:, 0:128], Un[:], ident_b[:], [Un, ident_b], [pX])
                                    Tn = G(f"gT{(lvl + 1) % 2}{key}", [128, 128], BF16)
                                    S.cp('act', Tn[:], pXb[:, 0:128], [pX], [Tn])
                                    ch['Tk'] = Tn
                                else:
                                    S.stt(Un[:], U[:], 2.0, pW[:, 128:256], ALU.mult, ALU.subtract, [U, pW], [Un])
                                ch['U'] = Un
                        for ch_i, ch in enumerate(chains):
                            key = ch['key']
                            pw = S.P()
                            S.mm(pw[:, 0:128], ch['kbg'][:], ch['U'][:], [ch['kbg'], ch['U']], [pw])
                            S.mm(pw[:, 128:256], ch['U'][:], ch['vb'][:], [ch['U'], ch['vb']], [pw])
                            wT = G(f"gwT{key}", [128, 128], BF16)
                            S.cp('act', wT[:], pw[:, 0:128], [pw], [wT])
                            u = G(f"gu{key}", [128, 128], F32)
                            S.cp('dve', u[:], pw[:, 128:256], [pw], [u])
                            ch['wT'], ch['u'] = wT, u
                        for ch_i, ch in enumerate(chains):
                            key, h, sl, sc = ch['key'], ch['h'], ch['sl'], ch['sc']
                            qnT, szT = S.cache[f"gqnT{h}"], S.cache[f"gszT{h}"]
                            p1 = S.P()
                            S.mm(p1[:, 0:128], ch['wT'][:], Sgb[h][:], [ch['wT'], Sgb[h]], [p1])
                            S.mm(p1[:, 128:256], qnT[:, sl], Sgb[h][:], [qnT, Sgb[h]], [p1])
                            vn = G(f"gvn{ch_i % 3}", [128, 128], BF16)
                            S.tt('dve', vn[:], ch['u'][:], p1[:, 0:128], ALU.subtract, [ch['u'], p1], [vn])
                            o1 = G(f"go1{ch_i % 3}", [128, 128], F32)
                            S.act(o1[:], p1[:, 128:256], AF.Identity, [p1, sc], [o1], scale=sc[:, 32 + h:33 + h])
                            p2 = S.P()
                            S.mm(p2[:, 0:128], ch['kt'][:], vn[:], [ch['kt'], vn], [p2])
                            S.mm(p2[:, 128:256], ch['attnT'][:], vn[:], [ch['attnT'], vn], [p2])
                            S.stt(Sg[h][:], Sg[h][:], sc[:, 36 + h:37 + h], p2[:, 0:128], ALU.mult, ALU.add, [Sg[h], sc, p2], [Sg[h]])
                            S.cp('act', Sgb[h][:], Sg[h][:], [Sg[h]], [Sgb[h]])
                            o = G(f"go{ch_i % 3}", [128, 128], F32)
                            S.tt('dve', o[:], o1[:], p2[:, 128:256], ALU.add, [o1, p2], [o])
                            junk = G(f"gjunk{ch_i % 3}", [128, 128], F32)
                            ssq = G(f"gssq{ch_i % 3}", [128, 4], F32)
                            S.op('pool', lambda e, ssq=ssq: e.memset(ssq[:], 0.0), [], [ssq])
                            S.act(junk[:], o[:], AF.Square, [o], [junk, ssq], accum_out=ssq[:, 0:1])
                            S.rsqrt((ssq[:, 2:3], ssq), ssq[:, 0:1], 1.0 / 128, [ssq], (ssq[:, 1:2], ssq))
                            on = G(f"gon{ch_i % 3}", [128, 128], F32)
                            S.ts('pool', on[:], o[:], ssq[:, 2:3], ALU.mult, [o, ssq], [on])
                            pt = S.P()
                            S.tr(pt[:, 0:128], on[:], C['ident'], [on, cb], [pt])
                            cst = G(f"gcst{h}", [128, SBW], BF16)
                            S.stt(cst[:, sl], pt[:, 0:128], dnwt[:, 0:1], szT[:, sl], ALU.mult, ALU.mult, [pt, dnwt, szT], [cst])
                        for h in range(NH):
                            cst = S.cache[f"gcst{h}"]
                            S.stt(catT[:, 2 * h + 1, t0 % TS:t0 % TS + SBW], cst[:], selm[:, t0 // TS:t0 // TS + 1], catT[:, 2 * h + 1, t0 % TS:t0 % TS + SBW],
                                  ALU.mult, ALU.add, [cst, selm, catT], [catT])
                S.barrier()
            S.cache = {}
            if os.environ.get('KSTOP') == phase:
                return nc

        NTB = TS // 128
        NSL = T // TS
        with ExitStack() as esB:
            fnwt = S.sb("fnwt", [128, D], F32, esB)
            S.dma('sp', fnwt[:], fnw[:, :], writes=[fnwt], sembuf=fnwt)
            h2T = catT
            Gt = S.sb("Gt", [128, NTB, 32], F32, esB)
            with ExitStack() as esB1:
                S.cache_es = esB1
                G = S.g
                Wout = S.sb("Wout", [128, 8, D], BF16, esB1)
                S.dma('pool', Wout[:], w_out.rearrange("(c p) n -> p c n", p=128), writes=[Wout], sembuf=Wout)
                Wr = S.sb("Wr", [128, 8, 36], F32, esB1)
                S.dma('sp', Wr[:], w_r.rearrange("(c p) n -> p c n", p=128), writes=[Wr], sembuf=Wr)
                brt = S.sb("brt", [128, 36], F32, esB1)
                S.dma('sp', brt[:], b_r[:, :], writes=[brt], sembuf=brt)
                for tt in range(NTB):
                    sl = slice(tt * 128, (tt + 1) * 128)
                    xst = G(f"xst{tt % 2}", [128, D], F32)
                    S.dma('sp', xst[:], xs[tt * 128:(tt + 1) * 128, :], writes=[xst], sembuf=xst)
                    x1 = G(f"x1_{tt % 2}", [128, D], F32)
                    for half in range(2):
                        hs = slice(half * 512, (half + 1) * 512)
                        pm_ = S.P()
                        for c in range(8):
                            S.mm(pm_[:, :], catT[:, c, sl], Wout[:, c, hs], [catT, Wout], [pm_], start=(c == 0), stop=(c == 7))
                        tmp = G(f"mixg{half}", [128, 512], F32)
                        S.tt('dve', tmp[:], pm_[:, :], g12[:, half * 512:(half + 1) * 512], ALU.mult, [pm_, g12], [tmp])
                        S.tt('pool', x1[:, hs], tmp[:], xst[:, hs], ALU.add, [tmp, xst], [x1])
                    S.dma('sp', x1_d.ap[tt * 128:(tt + 1) * 128, :], x1[:], reads=[x1], writes=[x1_d], sembuf=x1)
                    junk = G("bjunk", [128, D], F32)
                    ssq = G(f"bssq{tt % 2}", [128, 4], F32)
                    S.op('pool', lambda e, ssq=ssq: e.memset(ssq[:], 0.0), [], [ssq])
                    S.act(junk[:], x1[:], AF.Square, [x1], [junk, ssq], accum_out=ssq[:, 0:1])
                    S.rsqrt((ssq[:, 2:3], ssq), ssq[:, 0:1], 1.0 / D, [ssq], (ssq[:, 1:2], ssq))
                    xn = G("bxn", [128, D], F32)
                    S.ts('dve', xn[:], x1[:], ssq[:, 2:3], ALU.mult, [x1, ssq], [xn])
                    h2f = G("h2f", [128, 8, 128], F32)
                    for c in range(8):
                        ptp = S.P()
                        S.tr(ptp[:, 0:128], xn[:, c * 128:(c + 1) * 128], C['ident'], [xn, cb], [ptp])
                        S.act(h2f[:, c, :], ptp[:, 0:128], AF.Identity, [ptp, a2, mod], [h2f], bias=mod[:, 24 + c:25 + c], scale=a2[:, c:c + 1])
                    S.cp('pool', h2T[:, :, sl], h2f[:], [h2f], [h2T])
                    plg = S.P()
                    for c in range(8):
                        S.mm(plg[:, 0:36], h2f[:, c, :], Wr[:, c, :], [h2f, Wr], [plg], start=(c == 0), stop=(c == 7))
                    r = G("rt", [128, 256], F32)
                    S.tt('dve', r[:, 0:36], plg[:, 0:36], brt[:], ALU.add, [plg, brt], [r])
                    S.op('dve', lambda e, r=r: e.reduce_max(out=r[:, 36:37], in_=r[:, 0:4], axis=AX.X), [r], [r])
                    S.ts('dve', r[:, 40:44], r[:, 0:4], r[:, 36:37], ALU.is_equal, [r], [r])
                    S.ts('dve', r[:, 44:48], r[:, 0:4], r[:, 36:37], ALU.subtract, [r], [r])
                    S.act(r[:, 44:48], r[:, 44:48], AF.Exp, [r], [r])
                    S.op('dve', lambda e, r=r: e.reduce_sum(out=r[:, 48:49], in_=r[:, 44:48], axis=AX.X), [r], [r])
                    S.op('dve', lambda e, r=r: e.reciprocal(out=r[:, 49:50], in_=r[:, 48:49]), [r], [r])
                    S.ts('dve', r[:, 44:48], r[:, 40:44], 1.0, ALU.subtract, [r], [r], s2=1e30, op1=ALU.mult)
                    S.tt('dve', r[:, 64:96].rearrange("p (g e) -> p g e", e=8), r[:, 4:36].rearrange("p (g e) -> p g e", e=8),
                         r[:, 44:48].unsqueeze(2).to_broadcast([128, 4, 8]), ALU.add, [r], [r])
                    S.op('dve', lambda e, r=r: e.reduce_max(out=r[:, 96:97], in_=r[:, 64:96], axis=AX.X), [r], [r])
                    S.ts('dve', r[:, 100:132], r[:, 64:96], r[:, 96:97], ALU.is_equal, [r], [r])
                    S.stt(r[:, 132:164], r[:, 100:132], -1e30, r[:, 64:96], ALU.mult, ALU.add, [r], [r])
                    S.op('dve', lambda e, r=r: e.reduce_max(out=r[:, 164:165], in_=r[:, 132:164], axis=AX.X), [r], [r])
                    S.ts('dve', r[:, 168:200], r[:, 132:164], r[:, 164:165], ALU.is_equal, [r], [r])
                    S.tt('dve', r[:, 200:201], r[:, 164:165], r[:, 96:97], ALU.subtract, [r], [r])
                    S.act(r[:, 200:201], r[:, 200:201], AF.Exp, [r], [r])
                    S.ts('dve', r[:, 201:202], r[:, 200:201], 1.0, ALU.add, [r], [r])
                    S.op('dve', lambda e, r=r: e.reciprocal(out=r[:, 202:203], in_=r[:, 201:202]), [r], [r])
                    S.tt('dve', r[:, 202:203], r[:, 202:203], r[:, 49:50], ALU.mult, [r], [r])
                    S.tt('dve', r[:, 203:204], r[:, 202:203], r[:, 200:201], ALU.mult, [r], [r])
                    S.ts('dve', r[:, 204:236], r[:, 100:132], r[:, 202:203], ALU.mult, [r], [r])
                    S.stt(Gt[:, tt, :], r[:, 168:200], r[:, 203:204], r[:, 204:236], ALU.mult, ALU.add, [r], [Gt])
                S.barrier()
            S.cache = {}
            if os.environ.get('KSTOP') == 'b1':
                return nc
            acc = S.sb("acc", [128, NTB, D], F32, esB)
            S.op('pool', lambda e: e.memset(acc[:], 0.0), [], [acc])
            with ExitStack() as esB2:
                S.cache_es = esB2
                G = S.g
                NB = max(1, TS // 512)
                BW = min(512, TS)
                for ex_ in range(NE):
                    w1t = G(f"w1t{ex_ % 2}", [128, 8, DEXP], BF16)
                    w3t = G(f"w3t{ex_ % 2}", [128, 8, DEXP], BF16)
                    w2t = G(f"w2t{ex_ % 2}", [128, 4, D], BF16)
                    S.dma('pool', w1t[:], w1[ex_].rearrange("(c p) n -> p c n", p=128), writes=[w1t], sembuf=w1t)
                    S.dma('pool', w3t[:], w3[ex_].rearrange("(c p) n -> p c n", p=128), writes=[w3t], sembuf=w3t)
                    S.dma('pool', w2t[:], w2[ex_].rearrange("(c p) n -> p c n", p=128), writes=[w2t], sembuf=w2t)
                    for tb in range(NB):
                        bsl = slice(tb * BW, (tb + 1) * BW)
                        hid = G(f"hid{tb % 2}", [128, 4, BW], BF16)
                        for hc in range(4):
                            p1 = S.P()
                            for c in range(8):
                                S.mm(p1[:, 0:BW], w1t[:, c, hc * 128:(hc + 1) * 128], h2T[:, c, bsl], [w1t, h2T], [p1], start=(c == 0), stop=(c == 7))
                            p3 = S.P()
                            for c in range(8):
                                S.mm(p3[:, 0:BW], w3t[:, c, hc * 128:(hc + 1) * 128], h2T[:, c, bsl], [w3t, h2T], [p3], start=(c == 0), stop=(c == 7))
                            sl_ = G(f"silu{hc % 2}", [128, BW], F32)
                            S.act(sl_[:], p1[:, 0:BW], AF.Silu, [p1], [sl_])
                            S.tt('dve', hid[:, hc, :], sl_[:], p3[:, 0:BW], ALU.mult, [sl_, p3], [hid])
                        for t4 in range(BW // 128):
                            tt = tb * (BW // 128) + t4
                            for half in range(2):
                                py = S.P()
                                for hc in range(4):
                                    S.mm(py[:, :], hid[:, hc, t4 * 128:(t4 + 1) * 128], w2t[:, hc, half * 512:(half + 1) * 512], [hid, w2t], [py],
                                         start=(hc == 0), stop=(hc == 3))
                                S.stt(acc[:, tt, half * 512:(half + 1) * 512], py[:, :], Gt[:, tt, ex_:ex_ + 1], acc[:, tt, half * 512:(half + 1) * 512],
                                      ALU.mult, ALU.add, [py, Gt, acc], [acc])
                S.barrier()
            S.cache = {}
            if os.environ.get('KSTOP') == 'b2':
                return nc
            with ExitStack() as esB3:
                S.cache_es = esB3
                G = S.g
                for tt in range(NTB):
                    x1 = G(f"fx1_{tt % 2}", [128, D], F32)
                    S.dma('sp', x1[:], x1_d.ap[tt * 128:(tt + 1) * 128, :], reads=[x1_d], writes=[x1], sembuf=x1)
                    x2 = G(f"fx2_{tt % 2}", [128, D], F32)
                    S.tt('pool', x2[:], acc[:, tt, :], g12[:, D:2 * D], ALU.mult, [acc, g12], [x2])
                    S.tt('dve', x2[:], x2[:], x1[:], ALU.add, [x2, x1], [x2])
                    junk = G("fjunk", [128, D], F32)
                    ssq = G(f"fssq{tt % 2}", [128, 4], F32)
                    S.op('pool', lambda e, ssq=ssq: e.memset(ssq[:], 0.0), [], [ssq])
                    S.act(junk[:], x2[:], AF.Square, [x2], [junk, ssq], accum_out=ssq[:, 0:1])
                    S.rsqrt((ssq[:, 2:3], ssq), ssq[:, 0:1], 1.0 / D, [ssq], (ssq[:, 1:2], ssq))
                    ot = G(f"fot{tt % 2}", [128, D], F32)
                    S.stt(ot[:], x2[:], ssq[:, 2:3], fnwt[:], ALU.mult, ALU.mult, [x2, ssq, fnwt], [ot])
                    S.dma('sp', out[tt * 128:(tt + 1) * 128, :], ot[:], reads=[ot], sembuf=ot)
                S.barrier()
        print("ninst", S.ninst, "nsem", S.nsem, flush=True)
    return nc


def _host_inputs(inp, T, TS):
    NH = NHEAD
    NE_ = int(os.environ.get('KNEXP', NEXP))
    f = lambda a: np.ascontiguousarray(np.asarray(a), dtype=np.float32)
    x = f(inp['x'])
    B = x.shape[0]
    w_in = f(inp['w_in'])[0]
    swap = np.concatenate([np.arange(64, 128), np.arange(0, 64)])
    cols = []
    for h in range(NH):
        q = np.arange(h * 128, (h + 1) * 128)
        k = 512 + q
        v = 1024 + q
        g = 1536 + q
        cols += [q, q[swap], k, k[swap], v, g]
    for h in range(NH):
        q = 2048 + np.arange(h * 128, (h + 1) * 128)
        cols += [q, q + 512, q + 1024, q + 1536]
    cols = np.concatenate(cols)
    w_inA = np.ascontiguousarray(w_in[:, cols])
    w_ab = np.ascontiguousarray(w_in[:, 4096:4104])
    conv = f(inp['conv_w'])[0]
    convw = np.zeros((128, NH * 12), np.float32)
    for h in range(NH):
        for w_ in range(3):
            ch = w_ * 512 + h * 128 + np.arange(128)
            convw[:, h * 12 + w_ * 4:h * 12 + w_ * 4 + 4] = conv[:, ch].T
    hv = np.broadcast_to(np.concatenate([f(inp['a_log'])[0], f(inp['dt_bias'])[0]])[None, :], (128, 2 * NH)).copy()
    dnw = f(inp['dn_norm_w'])[0].reshape(128, 1).copy()
    w_out = f(inp['w_out'])[0]
    rows = []
    for h in range(NH):
        rows += [np.arange(h * 128, (h + 1) * 128), 512 + np.arange(h * 128, (h + 1) * 128)]
    w_outP = np.ascontiguousarray(w_out[np.concatenate(rows), :])
    w_r = np.ascontiguousarray(np.concatenate([f(inp['w_group'])[0], f(inp['w_expert'])[0]], axis=1))
    b_r = np.broadcast_to(np.concatenate([f(inp['b_group'])[0], f(inp['b_expert'])[0]])[None, :], (128, 36)).copy()
    n12 = np.concatenate([f(inp['norm1_w'])[0].reshape(8, 128).T, f(inp['norm2_w'])[0].reshape(8, 128).T], axis=1).copy()
    b_adaT = np.ascontiguousarray(f(inp['b_ada'])[0].reshape(48, 128).T)
    fnw = np.broadcast_to(f(inp['final_norm_w'])[None, :], (128, D)).copy()
    shared = dict(w_ada=f(inp['w_ada'])[0], b_adaT=b_adaT, n12=n12, w_inA=w_inA, w_ab=w_ab, convw=convw, hv=hv, dnw=dnw,
                  w_out=w_outP, w_r=w_r, b_r=b_r, w1=f(inp['w1'])[0][:NE_], w3=f(inp['w3'])[0][:NE_], w2=f(inp['w2'])[0][:NE_], fnw=fnw,
                  cbig=CBIG, ccol=CCOL)
    pos = np.asarray(inp['positions']).astype(np.int32)
    c = f(inp['c'])
    maps = []
    nsl = T // TS
    for core in range(B * nsl):
        b, s = core // nsl, core % nsl
        m = dict(shared)
        m['xT'] = np.ascontiguousarray(x[b].T)
        m['xs'] = np.ascontiguousarray(x[b, s * TS:(s + 1) * TS, :])
        m['pos'] = np.ascontiguousarray(pos[b][None, :])
        m['cT'] = np.ascontiguousarray(c[b].reshape(8, 128).T)
        sm = np.zeros((128, 8), np.float32)
        sm[:, s] = 1.0
        m['selm'] = sm
        maps.append(m)
    return maps


_NC_CACHE = {}


def _run(inp, T, TS):
    key = (T, TS)
    if key not in _NC_CACHE:
        _NC_CACHE[key] = build(T, TS, SBW=min(256, TS))
    nc = _NC_CACHE[key]
    maps = _host_inputs(inp, T, TS)
    res = run_bass_kernel_spmd(nc, maps, core_ids=list(range(len(maps))))
    B = np.asarray(inp['x']).shape[0]
    nsl = T // TS
    out = np.zeros((B, T, D), np.float32)
    for core in range(B * nsl):
        b, s = core // nsl, core % nsl
        out[b, s * TS:(s + 1) * TS, :] = np.asarray(res.results[core]['out'])
    return out


def kernel(**inputs):
    return _run(inputs, 8192, 2048)
```

```python
import math
import os
from contextlib import ExitStack
import numpy as np
import concourse.bass as bass
import concourse.mybir as mybir
from concourse.bass_utils import run_bass_kernel_spmd

F32 = mybir.dt.float32
BF16 = mybir.dt.bfloat16
I32 = mybir.dt.int32
AF = mybir.ActivationFunctionType
ALU = mybir.AluOpType
AX = mybir.AxisListType

D = 1024
NHEAD = 4
EPS = 1e-6
NEXP = 32
DEXP = 512
TWO_PI = 2.0 * math.pi


class Buf:
    def __init__(self, ap, name):
        self.ap = ap
        self.name = name
        self.ws = {}
        self.rs = {}
        self.dsem = None
        self.dcnt = 0
        self.psum = False

    def __getitem__(self, k):
        return self.ap[k]


class Sched:
    EPOCH = 30000

    def __init__(self, nc, es):
        self.nc = nc
        self.es = es
        self.eng = {'pe': nc.tensor, 'act': nc.scalar, 'dve': nc.vector, 'pool': nc.gpsimd, 'sp': nc.sync}
        self.sem = {}
        self.cnt = {}
        self.waited = {e: {} for e in self.eng}
        self.nsem = 0
        self.allsems = {}
        for e in self.eng:
            self._newsem(e)
        self.bufs = []
        self.ninst = {e: 0 for e in self.eng}
        self.cache = {}
        self.pbanks = []
        self.pi = 0

    def _mksem(self, name):
        self.nsem += 1
        s = self.es.enter_context(self.nc.semaphore(name))
        return s

    def _newsem(self, e):
        self.sem[e] = self._mksem(f"c_{e}_{self.nsem}")
        self.cnt[e] = 0

    def sb(self, name, shape, dt, es=None):
        self.uid = getattr(self, 'uid', 0) + 1
        name = f"s{self.uid}_{name}"
        t = (es or self.es).enter_context(self.nc.sbuf_tensor(name, list(shape), dt))
        b = Buf(t, name)
        self.bufs.append(b)
        return b

    def g(self, name, shape, dt):
        if name not in self.cache:
            self.cache[name] = self.sb(name, shape, dt, es=self.cache_es)
        return self.cache[name]

    def mkpsum(self):
        for i in range(8):
            t = self.es.enter_context(self.nc.psum_tensor(f"pb{i}", [128, 512], F32))
            b = Buf(t, f"pb{i}")
            b.psum = True
            self.bufs.append(b)
            self.pbanks.append(b)

    def P(self):
        b = self.pbanks[self.pi % 8]
        self.pi += 1
        return b

    def dram(self, name, shape, dt, kind="Internal"):
        t = self.nc.dram_tensor(name, list(shape), dt, kind=kind).ap()
        b = Buf(t, name)
        self.bufs.append(b)
        return b

    def _deps(self, reads, writes, e=None):
        deps = {}

        def add(d):
            for k, (s, v) in d.items():
                if k not in deps or deps[k][1] < v:
                    deps[k] = (s, v)
        for b in reads:
            add(b.ws)
            if b.psum:
                own = id(self.sem[e]) if e in self.sem else None
                add({k: v for k, v in b.rs.items() if k != own})
        for b in writes:
            add(b.ws)
            add(b.rs)
        return deps

    def _wait(self, e, deps):
        for k, (s, v) in deps.items():
            if e == 'pe' and s is self.sem['pe']:
                continue
            if self.waited[e].get(k, 0) >= v:
                continue
            self.eng[e].wait_ge(s, v)
            self.ninst[e] += 1
            self.waited[e][k] = v

    def _record(self, ev, reads, writes):
        k = id(ev[0])
        for b in reads:
            if k not in b.rs or b.rs[k][1] < ev[1]:
                b.rs[k] = ev
        for b in writes:
            if k not in b.ws or b.ws[k][1] < ev[1]:
                b.ws[k] = ev

    def op(self, e, fn, reads=(), writes=()):
        if e == 'pool' and os.environ.get('KNOPOOL'):
            e = 'dve'
        self._wait(e, self._deps(reads, writes, e))
        ins = fn(self.eng[e])
        if self.cnt[e] >= self.EPOCH:
            self._newsem(e)
        self.cnt[e] += 1
        ins.then_inc(self.sem[e], 1)
        self.ninst[e] += 1
        self._record((self.sem[e], self.cnt[e]), reads, writes)
        return ins

    def dma(self, q, out_ap, in_ap, reads=(), writes=(), sembuf=None, **kw):
        self._wait(q, self._deps(reads, writes))
        if sembuf.dsem is None:
            sembuf.dsem = self._mksem(f"d_{sembuf.name}")
        ins = self.eng[q].dma_start(out=out_ap, in_=in_ap, **kw)
        sembuf.dcnt += 1
        ins.then_inc(sembuf.dsem, 16)
        self.ninst[q] += 1
        self._record((sembuf.dsem, 16 * sembuf.dcnt), reads, writes)
        return ins

    def barrier(self, engines=('pe', 'act', 'dve', 'pool', 'sp')):
        deps = {}
        for b in self.bufs:
            for d in (b.ws, b.rs):
                for k, (s, v) in d.items():
                    if k not in deps or deps[k][1] < v:
                        deps[k] = (s, v)
        for e in engines:
            self._wait(e, deps)

    def act(self, out, in_, func, reads, writes, **kw):
        return self.op('act', lambda e: e.activation(out=out, in_=in_, func=func, **kw), reads, writes)

    def tt(self, eng, out, a, b, op, reads, writes):
        return self.op(eng, lambda e: e.tensor_tensor(out=out, in0=a, in1=b, op=op), reads, writes)

    def ts(self, eng, out, a, s1, op0, reads, writes, s2=None, op1=None):
        if op1 is None:
            return self.op(eng, lambda e: e.tensor_scalar(out=out, in0=a, scalar1=s1, scalar2=None, op0=op0), reads, writes)
        return self.op(eng, lambda e: e.tensor_scalar(out=out, in0=a, scalar1=s1, scalar2=s2, op0=op0, op1=op1), reads, writes)

    def stt(self, out, a, sc, b, op0, op1, reads, writes):
        return self.op('dve', lambda e: e.scalar_tensor_tensor(out=out, in0=a, scalar=sc, in1=b, op0=op0, op1=op1), reads, writes)

    def cp(self, eng, out, in_, reads, writes):
        if eng == 'act':
            return self.act(out, in_, AF.Copy, reads, writes)
        return self.op(eng, lambda e: e.tensor_copy(out=out, in_=in_), reads, writes)

    def mm(self, out, lhsT, rhs, reads, writes, start=True, stop=True):
        return self.op('pe', lambda e: e.matmul(out, lhsT=lhsT, rhs=rhs, start=start, stop=stop), reads, writes)

    def tr(self, out, in_, ident, reads, writes):
        return self.op('pe', lambda e: e.transpose(out=out, in_=in_, identity=ident), reads, writes)

    def rsqrt(self, out, in_, scale, reads_in, tmp, eps=EPS):
        self.act(tmp[0], in_, AF.Ln, list(reads_in) + [self.epsbuf], [tmp[1]], bias=self.epsc[:in_.shape[0], 0:1], scale=scale)
        self.act(out[0], tmp[0], AF.Exp, [tmp[1]], [out[1]], scale=-0.5)


def rr(gens):
    gens = list(gens)
    while gens:
        nxt = []
        for g_ in gens:
            try:
                next(g_)
                nxt.append(g_)
            except StopIteration:
                pass
        gens = nxt


def _consts():
    i = np.arange(128)
    c = {}
    c['ident'] = np.eye(128, dtype=np.float32)
    c['triU'] = (i[:, None] <= i[None, :]).astype(np.float32)
    c['negtriU'] = -c['triU']
    c['negmask'] = np.where(i[None, :] <= i[:, None], 0.0, -1e30).astype(np.float32)
    c['m2L'] = ((i[:, None] == i[None, :] + 1) & (i[:, None] % 2 == 1)).astype(np.float32)
    for s in (4, 8, 16, 32, 64):
        c[f'bm{s}'] = ((i[:, None] // s) == (i[None, :] // s)).astype(np.float32)
    gam = 1.0 - np.power(2.0, -5.0 - np.arange(NHEAD))
    lg = np.log1p(-np.power(2.0, -5.0 - np.arange(NHEAD, dtype=np.float64)))
    rel = i[None, :] - i[:, None]
    for h in range(NHEAD):
        c[f'decT{h}'] = (np.where(rel >= 0, np.exp(lg[h] * np.maximum(rel, 0)), 0.0) * 128 ** -0.5).astype(np.float32)
        c[f'qdec{h}'] = np.broadcast_to(np.exp(lg[h] * (i + 1.0))[None, :], (128, 128)).astype(np.float32).copy()
    kws = np.stack([np.exp(lg[h] * (127 - i)) * 128 ** -0.5 for h in range(NHEAD)], axis=1)
    cd = [float(np.exp(lg[h] * 128)) for h in range(NHEAD)]
    invf = (10000.0 ** (-(np.arange(0, 128, 2, dtype=np.float32)) / 128.0)).astype(np.float32)
    col = np.zeros((128, 8), np.float32)
    col[:, 0] = np.concatenate([invf, invf])
    col[:, 1] = np.concatenate([-np.ones(64), np.ones(64)])
    col[:, 2] = EPS
    col[:, 3] = math.pi / 2
    col[:, 4:8] = kws
    names = ['ident', 'triU', 'negtriU', 'negmask', 'm2L', 'bm4', 'bm8', 'bm16', 'bm32', 'bm64'] + \
            [f'decT{h}' for h in range(NHEAD)] + [f'qdec{h}' for h in range(NHEAD)]
    big = np.concatenate([c[n] for n in names], axis=1).astype(np.float32)
    return names, big, col, cd


CNAMES, CBIG, CCOL, CD = _consts()


def build(T, TS, SBW=256, dbg=False):
    NH = NHEAD
    NSB = T // SBW
    NT = SBW // 128
    NCOL = NH * 10 * 128
    NE = int(os.environ.get('KNEXP', NEXP))
    nc = bass.Bass("TRN2", target_bir_lowering=False)
    es = ExitStack()
    with es:
        S = Sched(nc, es)
        S.mkpsum()
        dt_in = {}

        def din(name, shape, dt=F32):
            dt_in[name] = nc.dram_tensor(name, list(shape), dt, kind="ExternalInput").ap()
            return dt_in[name]
        xT = din("xT", [D, T])
        xs = din("xs", [TS, D])
        pos = din("pos", [1, T], I32)
        cT = din("cT", [128, 8])
        w_ada = din("w_ada", [D, 6 * D])
        b_adaT = din("b_adaT", [128, 48])
        n12 = din("n12", [128, 16])
        w_inA = din("w_inA", [D, NCOL])
        w_ab = din("w_ab", [D, 2 * NH])
        convw = din("convw", [128, NH * 12])
        hv = din("hv", [128, 2 * NH])
        dnw = din("dnw", [128, 1])
        w_out = din("w_out", [D, D])
        w_r = din("w_r", [D, 36])
        b_r = din("b_r", [128, 36])
        w1 = din("w1", [NE, D, DEXP])
        w3 = din("w3", [NE, D, DEXP])
        w2 = din("w2", [NE, DEXP, D])
        fnw = din("fnw", [128, D])
        cbig = din("cbig", [128, CBIG.shape[1]])
        ccol = din("ccol", [128, 8])
        selm_in = din("selm", [128, 8])
        out = nc.dram_tensor("out", [TS, D], F32, kind="ExternalOutput").ap()
        x1_d = S.dram("x1_d", [TS, D], F32)
        dbg_outs = {}

        cb = S.sb("cbig", [128, CBIG.shape[1]], F32)
        S.dma('sp', cb[:], cbig[:, :], writes=[cb], sembuf=cb)
        cc = S.sb("ccol", [128, 8], F32)
        S.dma('sp', cc[:], ccol[:, :], writes=[cc], sembuf=cc)
        S.epsc = cc.ap[:, 2:3]
        S.epsbuf = cc
        C = {n: cb.ap[:, k * 128:(k + 1) * 128] for k, n in enumerate(CNAMES)}
        ident_b = S.sb("ident_b", [128, 128], BF16)
        S.cp('dve', ident_b[:], C['ident'], [cb], [ident_b])
        ones_b = S.sb("ones_b", [128, 128], BF16)
        S.op('pool', lambda e: e.memset(ones_b[:], 1.0), [], [ones_b])
        ones_f = S.sb("ones_f", [128, 128], F32)
        S.op('pool', lambda e: e.memset(ones_f[:], 1.0), [], [ones_f])
        bmb = {}
        for s_ in (4, 8, 16, 32, 64):
            bmb[s_] = S.sb(f"bmb{s_}", [128, 128], BF16)
            S.cp('dve', bmb[s_][:], C[f'bm{s_}'], [cb], [bmb[s_]])
        m2Lb = S.sb("m2Lb", [128, 128], BF16)
        S.cp('dve', m2Lb[:], C['m2L'], [cb], [m2Lb])
        selm = S.sb("selm", [128, 8], F32)
        S.dma('sp', selm[:], selm_in[:, :], writes=[selm], sembuf=selm)
        catT = S.sb("catT", [128, 8, TS], BF16)
        S.op('pool', lambda e: e.memset(catT[:], 0.0), [], [catT])
        mod = S.sb("mod", [128, 48], F32)
        a1 = S.sb("a1", [128, 8], F32)
        a2 = S.sb("a2", [128, 8], F32)
        g12 = S.sb("g12", [128, 2 * D], F32)

        with ExitStack() as es0:
            S.cache_es = es0
            ct = S.sb("ct", [128, 8], F32, es0)
            S.dma('sp', ct[:], cT[:, :], writes=[ct], sembuf=ct)
            sct = S.sb("sct", [128, 8], F32, es0)
            S.act(sct[:], ct[:], AF.Silu, [ct], [sct])
            bad = S.sb("bad", [128, 48], F32, es0)
            S.dma('sp', bad[:], b_adaT[:, :], writes=[bad], sembuf=bad)
            n12t = S.sb("n12t", [128, 16], F32, es0)
            S.dma('sp', n12t[:], n12[:, :], writes=[n12t], sembuf=n12t)
            pm = S.P()
            for j in range(6):
                wa = S.g(f"wada{j % 2}", [128, 8, D], F32)
                S.dma('sp', wa[:], w_ada[:, j * D:(j + 1) * D].rearrange("(c p) n -> p c n", p=128), writes=[wa], sembuf=wa)
                for oc in range(8):
                    col_ = j * 8 + oc
                    for c in range(8):
                        S.mm(pm[:, col_:col_ + 1], wa[:, c, oc * 128:(oc + 1) * 128], sct[:, c:c + 1], [wa, sct], [pm],
                             start=(c == 0), stop=(c == 7))
            S.tt('dve', mod[:], pm[:, 0:48], bad[:], ALU.add, [pm, bad], [mod])
            S.stt(a1[:], mod[:, 8:16], 1.0, n12t[:, 0:8], ALU.add, ALU.mult, [mod, n12t], [a1])
            S.stt(a2[:], mod[:, 32:40], 1.0, n12t[:, 8:16], ALU.add, ALU.mult, [mod, n12t], [a2])
            for gi, base in ((0, 16), (1, 40)):
                for c in range(8):
                    dg = S.g(f"dg{c % 2}", [128, 128], F32)
                    S.ts('dve', dg[:], C['ident'], mod[:, base + c:base + c + 1], ALU.mult, [cb, mod], [dg])
                    pg = S.P()
                    S.mm(pg[:, 0:128], ones_f[:], dg[:], [ones_f, dg], [pg])
                    S.cp('act', g12[:, gi * D + c * 128:gi * D + (c + 1) * 128], pg[:, 0:128], [pg], [g12])
            S.barrier()
        S.cache = {}
        if os.environ.get('KSTOP') == 'pre':
            return nc

        for phase in ('ret', 'gdn'):
            with ExitStack() as esA:
                S.cache_es = esA
                G = S.g
                if phase == 'ret':
                    PC0, PCN = 0, NH * 6 * 128
                else:
                    PC0, PCN = NH * 6 * 128, NH * 4 * 128
                Win = S.sb("Win" + phase, [128, 8, PCN], BF16, esA)
                for c in range(8):
                    S.dma('pool', Win[:, c, :], w_inA[c * 128:(c + 1) * 128, PC0:PC0 + PCN], writes=[Win], sembuf=Win)
                Wab = S.sb("Wab" + phase, [128, 8, 2 * NH], BF16, esA)
                S.dma('pool', Wab[:], w_ab.rearrange("(c p) n -> p c n", p=128), writes=[Wab], sembuf=Wab)
                cw = S.sb("cw" + phase, [128, NH * 12], F32, esA)
                S.dma('sp', cw[:], convw[:, :], writes=[cw], sembuf=cw)
                hvt = S.sb("hvt" + phase, [128, 2 * NH], F32, esA)
                S.dma('sp', hvt[:], hv[:, :], writes=[hvt], sembuf=hvt)
                dnwt = S.sb("dnwt" + phase, [128, 1], F32, esA)
                S.dma('sp', dnwt[:], dnw[:, :], writes=[dnwt], sembuf=dnwt)
                nea = S.sb("nea" + phase, [128, NH], F32, esA)
                S.act(nea[:], hvt[:, 0:NH], AF.Exp, [hvt], [nea])
                S.ts('dve', nea[:], nea[:], -1.0, ALU.mult, [nea], [nea])
                Sr, Srb, Sg, Sgb, cbuf = [], [], [], [], []
                for h in range(NH):
                    for lst, nm, dt in ((Sr, "Sr", F32), (Srb, "Srb", BF16), (Sg, "Sg", F32), (Sgb, "Sgb", BF16)):
                        b = S.sb(f"{nm}{h}{phase}", [128, 128], dt, esA)
                        S.op('pool', lambda e, b=b: e.memset(b[:], 0.0), [], [b])
                        lst.append(b)
                    row = []
                    for w_ in range(3):
                        b = S.sb(f"cbuf{h}_{w_}{phase}", [128, SBW + 3], F32, esA)
                        S.op('pool', lambda e, b=b: e.memset(b[:], 0.0), [], [b])
                        row.append(b)
                    cbuf.append(row)

                def proj(hT, col):
                    ps = S.P()
                    for c in range(8):
                        S.mm(ps[:, 0:SBW], Win[:, c, col - PC0:col - PC0 + 128], hT[:, c, :], [Win, hT], [ps], start=(c == 0), stop=(c == 7))
                    return ps

                for sb in range(NSB):
                    t0 = sb * SBW
                    xt = G("xt", [128, 8, SBW], F32)
                    S.dma('sp', xt[:], xT[:, t0:t0 + SBW].rearrange("(c p) t -> p c t", p=128), writes=[xt], sembuf=xt)
                    xsq = G("xsq", [128, 8, SBW], BF16)
                    S.act(xsq[:], xt[:], AF.Square, [xt], [xsq])
                    pss = S.P()
                    for c in range(8):
                        S.mm(pss[:, 0:SBW], ones_b[:], xsq[:, c, :], [ones_b, xsq], [pss], start=(c == 0), stop=(c == 7))
                    lnt = G("lnt", [128, SBW], F32)
                    rstd = G("rstd", [128, SBW], F32)
                    S.rsqrt((rstd[:], rstd), pss[:, 0:SBW], 1.0 / D, [pss], (lnt[:], lnt))
                    hT = G("hT", [128, 8, SBW], BF16)
                    for c in range(8):
                        tmp = G(f"xn{c % 2}", [128, SBW], F32)
                        S.tt('dve' if c % 2 == 0 else 'pool', tmp[:], xt[:, c, :], rstd[:], ALU.mult, [xt, rstd], [tmp])
                        S.act(hT[:, c, :], tmp[:], AF.Identity, [tmp, a1, mod], [hT], bias=mod[:, c:c + 1], scale=a1[:, c:c + 1])
                    if os.environ.get('KSTOP') == 'ret1':
                        S.barrier(engines=('sp',))
                        return nc
                    if phase == 'ret':
                        posi = G("posi", [128, SBW], I32)
                        S.dma('sp', posi[:], pos[0:1, t0:t0 + SBW].partition_broadcast(128), writes=[posi], sembuf=posi)
                        posf = G("posf", [128, SBW], F32)
                        S.cp('dve', posf[:], posi[:], [posi], [posf])
                        ang = G("ang", [128, SBW], F32)
                        S.ts('dve', ang[:], posf[:], cc[:, 0:1], ALU.mult, [posf, cc], [ang])
                        tabs = []
                        for which in range(2):
                            if which == 1:
                                ang2 = G("ang2", [128, SBW], F32)
                                S.ts('pool', ang2[:], ang[:], math.pi / 2, ALU.add, [ang], [ang2])
                                a_ = ang2
                            else:
                                a_ = ang
                            ki = G(f"ki{which}", [128, SBW], I32)
                            S.ts('dve', ki[:], a_[:], 1.0 / TWO_PI, ALU.mult, [a_], [ki])
                            kf = G(f"kf{which}", [128, SBW], F32)
                            S.cp('pool', kf[:], ki[:], [ki], [kf])
                            rr_ = G(f"rr{which}", [128, SBW], F32)
                            S.stt(rr_[:], kf[:], -TWO_PI, a_[:], ALU.mult, ALU.add, [kf, a_], [rr_])
                            S.ts('pool', rr_[:], rr_[:], math.pi, ALU.min, [rr_], [rr_], s2=-math.pi, op1=ALU.max)
                            tb = G(f"tab{which}", [128, SBW], F32)
                            S.act(tb[:], rr_[:], AF.Sin, [rr_], [tb])
                            tabs.append(tb)
                        sint, cost = tabs
                        sins = G("sins", [128, SBW], F32)
                        S.ts('pool', sins[:], sint[:], cc[:, 1:2], ALU.mult, [sint, cc], [sins])

                        def ret_prep(h):
                            base = h * 6 * 128
                            for nm, off in (("q", 0), ("k", 2)):
                                t1 = G(f"rt1{nm}{h}", [128, SBW], F32)
                                t2 = G(f"rt2{nm}{h}", [128, SBW], F32)
                                p1 = proj(hT, base + off * 128)
                                S.tt('dve', t1[:], p1[:, 0:SBW], cost[:], ALU.mult, [p1, cost], [t1])
                                yield
                                p2 = proj(hT, base + (off + 1) * 128)
                                S.tt('dve', t2[:], p2[:, 0:SBW], sins[:], ALU.mult, [p2, sins], [t2])
                                yield
                                o_ = G(f"r{nm}T{h}", [128, SBW], BF16)
                                S.tt('pool', o_[:], t1[:], t2[:], ALU.add, [t1, t2], [o_])
                                yield
                            pv = proj(hT, base + 4 * 128)
                            vT = G(f"rvT{h}", [128, SBW], BF16)
                            S.cp('act', vT[:], pv[:, 0:SBW], [pv], [vT])
                            yield
                            pg_ = proj(hT, base + 5 * 128)
                            sgT = G(f"rsgT{h}", [128, SBW], F32)
                            S.act(sgT[:], pg_[:, 0:SBW], AF.Silu, [pg_], [sgT])
                            yield

                        def ret_chain(tt, h):
                            sl = slice(tt * 128, (tt + 1) * 128)
                            qrT, krT, vT, sgT = (S.cache[f"rqT{h}"], S.cache[f"rkT{h}"], S.cache[f"rvT{h}"], S.cache[f"rsgT{h}"])
                            pk = S.P()
                            pkb = pk.ap[:].bitcast(BF16)
                            S.tr(pkb[:, 0:128], krT[:, sl], ident_b[:], [krT, ident_b], [pk])
                            S.tr(pkb[:, 128:256], vT[:, sl], ident_b[:], [vT, ident_b], [pk])
                            kw = G(f"rkw{h}", [128, 128], BF16)
                            S.ts('dve', kw[:], pkb[:, 0:128], cc[:, 4 + h:5 + h], ALU.mult, [pk, cc], [kw])
                            vtok = G(f"rvtok{h}", [128, 128], BF16)
                            S.cp('act', vtok[:], pkb[:, 128:256], [pk], [vtok])
                            qwT = G(f"rqw{h}", [128, 128], BF16)
                            S.tt('pool', qwT[:], qrT[:, sl], C[f'qdec{h}'], ALU.mult, [qrT, cb], [qwT])
                            yield
                            psc = S.P()
                            S.mm(psc[:, 0:128], krT[:, sl], qrT[:, sl], [krT, qrT], [psc])
                            sT = G(f"rsT{h}", [128, 128], BF16)
                            S.tt('dve', sT[:], psc[:, 0:128], C[f'decT{h}'], ALU.mult, [psc, cb], [sT])
                            yield
                            po = S.P()
                            S.mm(po[:, 0:128], sT[:], vtok[:], [sT, vtok], [po], start=True, stop=False)
                            S.mm(po[:, 0:128], qwT[:], Srb[h][:], [qwT, Srb[h]], [po], start=False, stop=True)
                            S.mm(po[:, 128:256], kw[:], vtok[:], [kw, vtok], [po])
                            S.stt(Sr[h][:], Sr[h][:], CD[h], po[:, 128:256], ALU.mult, ALU.add, [Sr[h], po], [Sr[h]])
                            osb = G(f"rosb{h}", [128, 128], F32)
                            S.cp('act', osb[:], po[:, 0:128], [po], [osb])
                            yield
                            S.cp('act', Srb[h][:], Sr[h][:], [Sr[h]], [Srb[h]])
                            junk = G(f"rjunk{h}", [128, 128], F32)
                            ssq = G(f"rssq{h}", [128, 4], F32)
                            S.op('pool', lambda e, ssq=ssq: e.memset(ssq[:], 0.0), [], [ssq])
                            yield
                            S.act(junk[:], osb[:], AF.Square, [osb], [junk, ssq], accum_out=ssq[:, 0:1])
                            yield
                            S.act(ssq[:, 1:2], ssq[:, 0:1], AF.Ln, [ssq, cc], [ssq], bias=cc[:, 2:3], scale=1.0 / 128)
                            yield
                            S.act(ssq[:, 2:3], ssq[:, 1:2], AF.Exp, [ssq], [ssq], scale=-0.5)
                            yield
                            on = G(f"ron{h}", [128, 128], F32)
                            S.ts('dve', on[:], osb[:], ssq[:, 2:3], ALU.mult, [osb, ssq], [on])
                            yield
                            pt = S.P()
                            S.tr(pt[:, 0:128], on[:], C['ident'], [on, cb], [pt])
                            cst = G(f"rcst{h}", [128, SBW], BF16)
                            S.tt('dve', cst[:, sl], pt[:, 0:128], sgT[:, sl], ALU.mult, [pt, sgT], [cst])
                            yield

                        rr([ret_prep(h) for h in range(NH)])
                        for tt in range(NT):
                            rr([ret_chain(tt, h) for h in range(NH)])
                        for h in range(NH):
                            cst = S.cache[f"rcst{h}"]
                            S.stt(catT[:, 2 * h, t0 % TS:t0 % TS + SBW], cst[:], selm[:, t0 // TS:t0 // TS + 1], catT[:, 2 * h, t0 % TS:t0 % TS + SBW],
                                  ALU.mult, ALU.add, [cst, selm, catT], [catT])

                    if phase == 'gdn':
                        def gdn_prep(h, w_):
                            nm = ("q", "k", "v")[w_]
                            base = NH * 6 * 128 + h * 4 * 128
                            cbf = cbuf[h][w_]
                            ps = proj(hT, base + w_ * 128)
                            S.cp('act', cbf[:, 3:3 + SBW], ps[:, 0:SBW], [ps], [cbf])
                            yield
                            acc = G(f"gacc{h}_{w_}", [128, SBW], F32)
                            wc = h * 12 + w_ * 4
                            S.ts('pool', acc[:], cbf[:, 0:SBW], cw[:, wc:wc + 1], ALU.mult, [cbf, cw], [acc])
                            yield
                            for j in range(1, 4):
                                S.stt(acc[:], cbf[:, j:j + SBW], cw[:, wc + j:wc + j + 1], acc[:], ALU.mult, ALU.add, [cbf, cw, acc], [acc])
                                yield
                            tl = G(f"gtail{h}_{w_}", [128, 4], F32)
                            S.cp('pool', tl[:, 0:3], cbf[:, SBW:SBW + 3], [cbf], [tl])
                            yield
                            S.cp('pool', cbf[:, 0:3], tl[:, 0:3], [tl], [cbf])
                            yield
                            if nm == "v":
                                vT = G(f"gvT{h}", [128, SBW], BF16)
                                S.act(vT[:], acc[:], AF.Silu, [acc], [vT])
                                yield
                            else:
                                y = acc
                                S.act(y[:], acc[:], AF.Silu, [acc], [y])
                                yield
                                sq = G(f"gsq{h}_{w_}", [128, SBW], BF16)
                                S.act(sq[:], y[:], AF.Square, [y], [sq])
                                yield
                                pn = S.P()
                                S.mm(pn[:, 0:SBW], ones_b[:], sq[:], [ones_b, sq], [pn])
                                rn = G(f"grn{h}_{w_}", [128, SBW], F32)
                                S.act(rn[:], pn[:, 0:SBW], AF.Ln, [pn, cc], [rn], bias=cc[:, 2:3], scale=1.0)
                                yield
                                S.act(rn[:], rn[:], AF.Exp, [rn], [rn], scale=-0.5)
                                yield
                                o_ = G(f"g{nm}nT{h}", [128, SBW], BF16)
                                if nm == "q":
                                    S.stt(o_[:], y[:], 128 ** -0.5, rn[:], ALU.mult, ALU.mult, [y, rn], [o_])
                                else:
                                    S.tt('pool', o_[:], y[:], rn[:], ALU.mult, [y, rn], [o_])
                                yield

                        def gdn_prep_z(h):
                            base = NH * 6 * 128 + h * 4 * 128
                            pz = proj(hT, base + 3 * 128)
                            szT = G(f"gszT{h}", [128, SBW], F32)
                            S.act(szT[:], pz[:, 0:SBW], AF.Silu, [pz], [szT])
                            yield

                        def gdn_scal(tt):
                            sl = slice(tt * 128, (tt + 1) * 128)
                            sc = G(f"gsc{tt}", [128, 64], F32)
                            pab = S.P()
                            for c in range(8):
                                S.mm(pab[:, 0:2 * NH], hT[:, c, sl], Wab[:, c, :], [hT, Wab], [pab], start=(c == 0), stop=(c == 7))
                            S.tt('dve', sc[:, 0:4], pab[:, 0:NH], hvt[:, NH:2 * NH], ALU.add, [pab, hvt], [sc])
                            S.act(sc[:, 16:20], pab[:, NH:2 * NH], AF.Exp, [pab], [sc], scale=-1.0)
                            yield
                            S.act(sc[:, 4:8], sc[:, 0:4], AF.Exp, [sc], [sc])
                            yield
                            S.act(sc[:, 8:12], sc[:, 4:8], AF.Ln, [sc], [sc], bias=1.0)
                            yield
                            S.tt('dve', sc[:, 12:16], sc[:, 8:12], nea[:], ALU.mult, [sc, nea], [sc])
                            yield
                            S.ts('dve', sc[:, 16:20], sc[:, 16:20], 1.0, ALU.add, [sc], [sc])
                            yield
                            S.op('dve', lambda e, sc=sc: e.reciprocal(out=sc[:, 20:24], in_=sc[:, 16:20]), [sc], [sc])
                            yield
                            pgc = S.P()
                            S.mm(pgc[:, 0:NH], C['triU'], sc[:, 12:16], [cb, sc], [pgc])
                            S.mm(pgc[:, 8:8 + NH], ones_f[:], sc[:, 12:16], [ones_f, sc], [pgc])
                            S.cp('dve', sc[:, 24:28], pgc[:, 0:NH], [pgc], [sc])
                            S.cp('dve', sc[:, 28:32], pgc[:, 8:8 + NH], [pgc], [sc])
                            yield
                            S.act(sc[:, 32:40], sc[:, 24:32], AF.Exp, [sc], [sc])
                            yield
                            S.tt('dve', sc[:, 40:44], sc[:, 28:32], sc[:, 24:28], ALU.subtract, [sc], [sc])
                            yield
                            S.act(sc[:, 44:48], sc[:, 40:44], AF.Exp, [sc], [sc])
                            yield
                            S.tt('dve', sc[:, 48:52], sc[:, 20:24], sc[:, 32:36], ALU.mult, [sc], [sc])
                            yield

                        def chain_pre(ch):
                            key, tt, h, sl = ch['key'], ch['tt'], ch['h'], ch['sl']
                            sc = S.cache[f"gsc{tt}"]
                            ch['sc'] = sc
                            qnT, knT, vT = S.cache[f"gqnT{h}"], S.cache[f"gknT{h}"], S.cache[f"gvT{h}"]
                            F = [G(f"gF{i}_{key}", [128, 128], F32) for i in range(4)]
                            Bq = [G(f"gB{i}_{key}", [128, 128], BF16) for i in range(2)]
                            gb = F[0]
                            S.ts('pool', gb[:], ones_f[:], sc[:, 12 + h:13 + h], ALU.mult, [ones_f, sc], [gb])
                            pT = S.P()
                            pTb = pT.ap[:].bitcast(BF16)
                            S.tr(pTb[:, 128:256], knT[:, sl], ident_b[:], [knT, ident_b], [pT])
                            S.tr(pTb[:, 256:384], vT[:, sl], ident_b[:], [vT, ident_b], [pT])
                            kbg = G(f"gkbg{key}", [128, 128], BF16)
                            S.act(kbg[:], pTb[:, 128:256], AF.Identity, [pT, sc], [kbg], scale=sc[:, 48 + h:49 + h])
                            kt = G(f"gkt{key}", [128, 128], BF16)
                            S.ts('dve', kt[:], pTb[:, 128:256], sc[:, 44 + h:45 + h], ALU.mult, [pT, sc], [kt])
                            vb = G(f"gvb{key}", [128, 128], BF16)
                            S.act(vb[:], pTb[:, 256:384], AF.Identity, [pT, sc], [vb], scale=sc[:, 20 + h:21 + h])
                            yield
                            pG = S.P()
                            S.mm(pG[:, 0:128], C['triU'], gb[:], [cb, gb], [pG], start=True, stop=False)
                            S.mm(pG[:, 0:128], gb[:], C['negtriU'], [cb, gb], [pG], start=False, stop=True)
                            ex = F[1]
                            S.stt(ex[:], pG[:, 0:128], 0.0, C['negmask'], ALU.min, ALU.add, [pG, cb], [ex])
                            yield
                            dec_i = F[1]
                            S.act(dec_i[:], ex[:], AF.Exp, [ex], [dec_i])
                            yield
                            dec_s = F[2]
                            S.tt('pool', dec_s[:], dec_i[:], C['ident'], ALU.subtract, [dec_i, cb], [dec_s])
                            yield
                            pK = S.P()
                            S.mm(pK[:, 0:128], knT[:, sl], knT[:, sl], [knT], [pK])
                            S.mm(pK[:, 128:256], qnT[:, sl], knT[:, sl], [qnT, knT], [pK])
                            A = G(f"gA{key}", [128, 128], BF16)
                            S.stt(A[:], pK[:, 0:128], sc[:, 20 + h:21 + h], dec_s[:], ALU.mult, ALU.mult, [pK, sc, dec_s], [A])
                            attn = Bq[0]
                            S.tt('dve', attn[:], pK[:, 128:256], dec_i[:], ALU.mult, [pK, dec_i], [attn])
                            yield
                            tm = Bq[1]
                            S.tt('pool', tm[:], A[:], m2Lb[:], ALU.mult, [A, m2Lb], [tm])
                            pT2 = S.P()
                            pT2b = pT2.ap[:].bitcast(BF16)
                            S.tr(pT2b[:, 0:128], attn[:], ident_b[:], [attn, ident_b], [pT2])
                            attnT = G(f"gattnT{key}", [128, 128], BF16)
                            S.cp('act', attnT[:], pT2b[:, 0:128], [pT2], [attnT])
                            yield
                            T1 = G(f"gT0{key}", [128, 128], BF16)
                            S.tt('pool', T1[:], ident_b[:], tm[:], ALU.subtract, [ident_b, tm], [T1])
                            yield
                            pU = S.P()
                            pUb = pU.ap[:].bitcast(BF16)
                            S.tr(pUb[:, 0:128], T1[:], ident_b[:], [T1, ident_b], [pU])
                            U = G(f"gU0{key}", [128, 128], BF16)
                            S.cp('act', U[:], pUb[:, 0:128], [pU], [U])
                            yield
                            Tk = T1
                            for lvl, bs in enumerate((4, 8, 16, 32, 64, None)):
                                pW = S.P()
                                S.mm(pW[:, 0:128], A[:], U[:], [A, U], [pW])
                                W = Bq[0]
                                S.tt('dve', W[:], pW[:, 0:128], U[:], ALU.add, [pW, U], [W])
                                yield
                                pW2 = S.P()
                                S.mm(pW2[:, 0:128], Tk[:], W[:], [Tk, W], [pW2])
                                Un = G(f"gU{(lvl + 1) % 2}{key}", [128, 128], BF16)
                                if bs is not None:
                                    tmpf = F[0]
                                    S.stt(tmpf[:], U[:], 2.0, pW2[:, 0:128], ALU.mult, ALU.subtract, [U, pW2], [tmpf])
                                    yield
                                    S.tt('pool', Un[:], tmpf[:], C[f'bm{bs}'], ALU.mult, [tmpf, cb], [Un])
                                    yield
                                    pX = S.P()
                                    pXb = pX.ap[:].bitcast(BF16)
                                    S.tr(pXb[:, 0:128], Un[:], ident_b[:], [Un, ident_b], [pX])
                                    Tn = G(f"gT{(lvl + 1) % 2}{key}", [128, 128], BF16)
                                    S.cp('act', Tn[:], pXb[:, 0:128], [pX], [Tn])
                                    yield
                                    Tk = Tn
                                else:
                                    S.stt(Un[:], U[:], 2.0, pW2[:, 0:128], ALU.mult, ALU.subtract, [U, pW2], [Un])
                                    yield
                                U = Un
                            pw = S.P()
                            S.mm(pw[:, 0:128], kbg[:], U[:], [kbg, U], [pw])
                            S.mm(pw[:, 128:256], U[:], vb[:], [U, vb], [pw])
                            wT = G(f"gwT{key}", [128, 128], BF16)
                            S.cp('act', wT[:], pw[:, 0:128], [pw], [wT])
                            u = F[3]
                            S.cp('dve', u[:], pw[:, 128:256], [pw], [u])
                            yield
                            ch.update(kt=kt, attnT=attnT, wT=wT, u=u, F=F, Bq=Bq)

                        def chain_scan(ch):
                            key, h, sl, sc, F, Bq = ch['key'], ch['h'], ch['sl'], ch['sc'], ch['F'], ch['Bq']
                            qnT, szT = S.cache[f"gqnT{h}"], S.cache[f"gszT{h}"]
                            p1 = S.P()
                            S.mm(p1[:, 0:128], ch['wT'][:], Sgb[h][:], [ch['wT'], Sgb[h]], [p1])
                            S.mm(p1[:, 128:256], qnT[:, sl], Sgb[h][:], [qnT, Sgb[h]], [p1])
                            vn = Bq[1]
                            S.tt('dve', vn[:], ch['u'][:], p1[:, 0:128], ALU.subtract, [ch['u'], p1], [vn])
                            o1 = F[1]
                            S.act(o1[:], p1[:, 128:256], AF.Identity, [p1, sc], [o1], scale=sc[:, 32 + h:33 + h])
                            yield
                            p2 = S.P()
                            S.mm(p2[:, 0:128], ch['kt'][:], vn[:], [ch['kt'], vn], [p2])
                            S.mm(p2[:, 128:256], ch['attnT'][:], vn[:], [ch['attnT'], vn], [p2])
                            S.stt(Sg[h][:], Sg[h][:], sc[:, 36 + h:37 + h], p2[:, 0:128], ALU.mult, ALU.add, [Sg[h], sc, p2], [Sg[h]])
                            o = F[2]
                            S.tt('dve', o[:], o1[:], p2[:, 128:256], ALU.add, [o1, p2], [o])
                            yield
                            S.cp('act', Sgb[h][:], Sg[h][:], [Sg[h]], [Sgb[h]])
                            junk = F[0]
                            ssq = G(f"gssq{key}", [128, 4], F32)
                            S.op('pool', lambda e, ssq=ssq: e.memset(ssq[:], 0.0), [], [ssq])
                            yield
                            S.act(junk[:], o[:], AF.Square, [o], [junk, ssq], accum_out=ssq[:, 0:1])
                            yield
                            S.act(ssq[:, 1:2], ssq[:, 0:1], AF.Ln, [ssq, cc], [ssq], bias=cc[:, 2:3], scale=1.0 / 128)
                            yield
                            S.act(ssq[:, 2:3], ssq[:, 1:2], AF.Exp, [ssq], [ssq], scale=-0.5)
                            yield
                            on = F[1]
                            S.ts('pool', on[:], o[:], ssq[:, 2:3], ALU.mult, [o, ssq], [on])
                            yield
                            pt = S.P()
                            S.tr(pt[:, 0:128], on[:], C['ident'], [on, cb], [pt])
                            cst = G(f"gcst{h}", [128, SBW], BF16)
                            S.stt(cst[:, sl], pt[:, 0:128], dnwt[:, 0:1], szT[:, sl], ALU.mult, ALU.mult, [pt, dnwt, szT], [cst])
                            yield

                        rr([gdn_prep(h, w_) for h in range(NH) for w_ in range(3)] + [gdn_prep_z(h) for h in range(NH)]
                           + [gdn_scal(tt) for tt in range(NT)])
                        chains = [dict(key=f"{tt}_{h}", tt=tt, h=h, sl=slice(tt * 128, (tt + 1) * 128)) for tt in range(NT) for h in range(NH)]
                        rr([chain_pre(ch) for ch in chains])
                        for tt in range(NT):
                            rr([chain_scan(ch) for ch in chains if ch['tt'] == tt])
                        for h in range(NH):
                            cst = S.cache[f"gcst{h}"]
                            S.stt(catT[:, 2 * h + 1, t0 % TS:t0 % TS + SBW], cst[:], selm[:, t0 // TS:t0 // TS + 1], catT[:, 2 * h + 1, t0 % TS:t0 % TS + SBW],
                                  ALU.mult, ALU.add, [cst, selm, catT], [catT])
                S.barrier()
            S.cache = {}
            if os.environ.get('KSTOP') == phase:
                return nc

        NTB = TS // 128
        NSL = T // TS
        with ExitStack() as esB:
            fnwt = S.sb("fnwt", [128, D], F32, esB)
            S.dma('sp', fnwt[:], fnw[:, :], writes=[fnwt], sembuf=fnwt)
            h2T = catT
            Gt = S.sb("Gt", [128, NTB, 32], F32, esB)
            with ExitStack() as esB1:
                S.cache_es = esB1
                G = S.g
                Wout = S.sb("Wout", [128, 8, D], BF16, esB1)
                S.dma('pool', Wout[:], w_out.rearrange("(c p) n -> p c n", p=128), writes=[Wout], sembuf=Wout)
                Wr = S.sb("Wr", [128, 8, 36], F32, esB1)
                S.dma('sp', Wr[:], w_r.rearrange("(c p) n -> p c n", p=128), writes=[Wr], sembuf=Wr)
                brt = S.sb("brt", [128, 36], F32, esB1)
                S.dma('sp', brt[:], b_r[:, :], writes=[brt], sembuf=brt)
                for tt in range(NTB):
                    sl = slice(tt * 128, (tt + 1) * 128)
                    xst = G(f"xst{tt % 2}", [128, D], F32)
                    S.dma('sp', xst[:], xs[tt * 128:(tt + 1) * 128, :], writes=[xst], sembuf=xst)
                    x1 = G(f"x1_{tt % 2}", [128, D], F32)
                    for half in range(2):
                        hs = slice(half * 512, (half + 1) * 512)
                        pm_ = S.P()
                        for c in range(8):
                            S.mm(pm_[:, :], catT[:, c, sl], Wout[:, c, hs], [catT, Wout], [pm_], start=(c == 0), stop=(c == 7))
                        tmp = G(f"mixg{half}", [128, 512], F32)
                        S.tt('dve', tmp[:], pm_[:, :], g12[:, half * 512:(half + 1) * 512], ALU.mult, [pm_, g12], [tmp])
                        S.tt('pool', x1[:, hs], tmp[:], xst[:, hs], ALU.add, [tmp, xst], [x1])
                    S.dma('sp', x1_d.ap[tt * 128:(tt + 1) * 128, :], x1[:], reads=[x1], writes=[x1_d], sembuf=x1)
                    junk = G("bjunk", [128, D], F32)
                    ssq = G(f"bssq{tt % 2}", [128, 4], F32)
                    S.op('pool', lambda e, ssq=ssq: e.memset(ssq[:], 0.0), [], [ssq])
                    S.act(junk[:], x1[:], AF.Square, [x1], [junk, ssq], accum_out=ssq[:, 0:1])
                    S.rsqrt((ssq[:, 2:3], ssq), ssq[:, 0:1], 1.0 / D, [ssq], (ssq[:, 1:2], ssq))
                    xn = G("bxn", [128, D], F32)
                    S.ts('dve', xn[:], x1[:], ssq[:, 2:3], ALU.mult, [x1, ssq], [xn])
                    h2f = G("h2f", [128, 8, 128], F32)
                    for c in range(8):
                        ptp = S.P()
                        S.tr(ptp[:, 0:128], xn[:, c * 128:(c + 1) * 128], C['ident'], [xn, cb], [ptp])
                        S.act(h2f[:, c, :], ptp[:, 0:128], AF.Identity, [ptp, a2, mod], [h2f], bias=mod[:, 24 + c:25 + c], scale=a2[:, c:c + 1])
                    S.cp('pool', h2T[:, :, sl], h2f[:], [h2f], [h2T])
                    plg = S.P()
                    for c in range(8):
                        S.mm(plg[:, 0:36], h2f[:, c, :], Wr[:, c, :], [h2f, Wr], [plg], start=(c == 0), stop=(c == 7))
                    r = G("rt", [128, 256], F32)
                    S.tt('dve', r[:, 0:36], plg[:, 0:36], brt[:], ALU.add, [plg, brt], [r])
                    S.op('dve', lambda e, r=r: e.reduce_max(out=r[:, 36:37], in_=r[:, 0:4], axis=AX.X), [r], [r])
                    S.ts('dve', r[:, 40:44], r[:, 0:4], r[:, 36:37], ALU.is_equal, [r], [r])
                    S.ts('dve', r[:, 44:48], r[:, 0:4], r[:, 36:37], ALU.subtract, [r], [r])
                    S.act(r[:, 44:48], r[:, 44:48], AF.Exp, [r], [r])
                    S.op('dve', lambda e, r=r: e.reduce_sum(out=r[:, 48:49], in_=r[:, 44:48], axis=AX.X), [r], [r])
                    S.op('dve', lambda e, r=r: e.reciprocal(out=r[:, 49:50], in_=r[:, 48:49]), [r], [r])
                    S.ts('dve', r[:, 44:48], r[:, 40:44], 1.0, ALU.subtract, [r], [r], s2=1e30, op1=ALU.mult)
                    S.tt('dve', r[:, 64:96].rearrange("p (g e) -> p g e", e=8), r[:, 4:36].rearrange("p (g e) -> p g e", e=8),
                         r[:, 44:48].unsqueeze(2).to_broadcast([128, 4, 8]), ALU.add, [r], [r])
                    S.op('dve', lambda e, r=r: e.reduce_max(out=r[:, 96:97], in_=r[:, 64:96], axis=AX.X), [r], [r])
                    S.ts('dve', r[:, 100:132], r[:, 64:96], r[:, 96:97], ALU.is_equal, [r], [r])
                    S.stt(r[:, 132:164], r[:, 100:132], -1e30, r[:, 64:96], ALU.mult, ALU.add, [r], [r])
                    S.op('dve', lambda e, r=r: e.reduce_max(out=r[:, 164:165], in_=r[:, 132:164], axis=AX.X), [r], [r])
                    S.ts('dve', r[:, 168:200], r[:, 132:164], r[:, 164:165], ALU.is_equal, [r], [r])
                    S.tt('dve', r[:, 200:201], r[:, 164:165], r[:, 96:97], ALU.subtract, [r], [r])
                    S.act(r[:, 200:201], r[:, 200:201], AF.Exp, [r], [r])
                    S.ts('dve', r[:, 201:202], r[:, 200:201], 1.0, ALU.add, [r], [r])
                    S.op('dve', lambda e, r=r: e.reciprocal(out=r[:, 202:203], in_=r[:, 201:202]), [r], [r])
                    S.tt('dve', r[:, 202:203], r[:, 202:203], r[:, 49:50], ALU.mult, [r], [r])
                    S.tt('dve', r[:, 203:204], r[:, 202:203], r[:, 200:201], ALU.mult, [r], [r])
                    S.ts('dve', r[:, 204:236], r[:, 100:132], r[:, 202:203], ALU.mult, [r], [r])
                    S.stt(Gt[:, tt, :], r[:, 168:200], r[:, 203:204], r[:, 204:236], ALU.mult, ALU.add, [r], [Gt])
                S.barrier()
            S.cache = {}
            if os.environ.get('KSTOP') == 'b1':
                return nc
            acc = S.sb("acc", [128, NTB, D], F32, esB)
            S.op('pool', lambda e: e.memset(acc[:], 0.0), [], [acc])
            with ExitStack() as esB2:
                S.cache_es = esB2
                G = S.g
                NB = max(1, TS // 512)
                BW = min(512, TS)
                for ex_ in range(NE):
                    w1t = G(f"w1t{ex_ % 2}", [128, 8, DEXP], BF16)
                    w3t = G(f"w3t{ex_ % 2}", [128, 8, DEXP], BF16)
                    w2t = G(f"w2t{ex_ % 2}", [128, 4, D], BF16)
                    S.dma('pool', w1t[:], w1[ex_].rearrange("(c p) n -> p c n", p=128), writes=[w1t], sembuf=w1t)
                    S.dma('pool', w3t[:], w3[ex_].rearrange("(c p) n -> p c n", p=128), writes=[w3t], sembuf=w3t)
                    S.dma('pool', w2t[:], w2[ex_].rearrange("(c p) n -> p c n", p=128), writes=[w2t], sembuf=w2t)
                    for tb in range(NB):
                        bsl = slice(tb * BW, (tb + 1) * BW)
                        hid = G(f"hid{tb % 2}", [128, 4, BW], BF16)
                        for hc in range(4):
                            p1 = S.P()
                            for c in range(8):
                                S.mm(p1[:, 0:BW], w1t[:, c, hc * 128:(hc + 1) * 128], h2T[:, c, bsl], [w1t, h2T], [p1], start=(c == 0), stop=(c == 7))
                            p3 = S.P()
                            for c in range(8):
                                S.mm(p3[:, 0:BW], w3t[:, c, hc * 128:(hc + 1) * 128], h2T[:, c, bsl], [w3t, h2T], [p3], start=(c == 0), stop=(c == 7))
                            sl_ = G(f"silu{hc % 2}", [128, BW], F32)
                            S.act(sl_[:], p1[:, 0:BW], AF.Silu, [p1], [sl_])
                            S.tt('dve', hid[:, hc, :], sl_[:], p3[:, 0:BW], ALU.mult, [sl_, p3], [hid])
                        for t4 in range(BW // 128):
                            tt = tb * (BW // 128) + t4
                            for half in range(2):
                                py = S.P()
                                for hc in range(4):
                                    S.mm(py[:, :], hid[:, hc, t4 * 128:(t4 + 1) * 128], w2t[:, hc, half * 512:(half + 1) * 512], [hid, w2t], [py],
                                         start=(hc == 0), stop=(hc == 3))
                                S.stt(acc[:, tt, half * 512:(half + 1) * 512], py[:, :], Gt[:, tt, ex_:ex_ + 1], acc[:, tt, half * 512:(half + 1) * 512],
                                      ALU.mult, ALU.add, [py, Gt, acc], [acc])
                S.barrier()
            S.cache = {}
            if os.environ.get('KSTOP') == 'b2':
                return nc
            with ExitStack() as esB3:
                S.cache_es = esB3
                G = S.g
                for tt in range(NTB):
                    x1 = G(f"fx1_{tt % 2}", [128, D], F32)
                    S.dma('sp', x1[:], x1_d.ap[tt * 128:(tt + 1) * 128, :], reads=[x1_d], writes=[x1], sembuf=x1)
                    x2 = G(f"fx2_{tt % 2}", [128, D], F32)
                    S.tt('pool', x2[:], acc[:, tt, :], g12[:, D:2 * D], ALU.mult, [acc, g12], [x2])
                    S.tt('dve', x2[:], x2[:], x1[:], ALU.add, [x2, x1], [x2])
                    junk = G("fjunk", [128, D], F32)
                    ssq = G(f"fssq{tt % 2}", [128, 4], F32)
                    S.op('pool', lambda e, ssq=ssq: e.memset(ssq[:], 0.0), [], [ssq])
                    S.act(junk[:], x2[:], AF.Square, [x2], [junk, ssq], accum_out=ssq[:, 0:1])
                    S.rsqrt((ssq[:, 2:3], ssq), ssq[:, 0:1], 1.0 / D, [ssq], (ssq[:, 1:2], ssq))
                    ot = G(f"fot{tt % 2}", [128, D], F32)
                    S.stt(ot[:], x2[:], ssq[:, 2:3], fnwt[:], ALU.mult, ALU.mult, [x2, ssq, fnwt], [ot])
                    S.dma('sp', out[tt * 128:(tt + 1) * 128, :], ot[:], reads=[ot], sembuf=ot)
                S.barrier()
        print("ninst", S.ninst, "nsem", S.nsem, flush=True)
    return nc


def _host_inputs(inp, T, TS):
    NH = NHEAD
    NE_ = int(os.environ.get('KNEXP', NEXP))
    f = lambda a: np.ascontiguousarray(np.asarray(a), dtype=np.float32)
    x = f(inp['x'])
    B = x.shape[0]
    w_in = f(inp['w_in'])[0]
    swap = np.concatenate([np.arange(64, 128), np.arange(0, 64)])
    cols = []
    for h in range(NH):
        q = np.arange(h * 128, (h + 1) * 128)
        k = 512 + q
        v = 1024 + q
        g = 1536 + q
        cols += [q, q[swap], k, k[swap], v, g]
    for h in range(NH):
        q = 2048 + np.arange(h * 128, (h + 1) * 128)
        cols += [q, q + 512, q + 1024, q + 1536]
    cols = np.concatenate(cols)
    w_inA = np.ascontiguousarray(w_in[:, cols])
    w_ab = np.ascontiguousarray(w_in[:, 4096:4104])
    conv = f(inp['conv_w'])[0]
    convw = np.zeros((128, NH * 12), np.float32)
    for h in range(NH):
        for w_ in range(3):
            ch = w_ * 512 + h * 128 + np.arange(128)
            convw[:, h * 12 + w_ * 4:h * 12 + w_ * 4 + 4] = conv[:, ch].T
    hv = np.broadcast_to(np.concatenate([f(inp['a_log'])[0], f(inp['dt_bias'])[0]])[None, :], (128, 2 * NH)).copy()
    dnw = f(inp['dn_norm_w'])[0].reshape(128, 1).copy()
    w_out = f(inp['w_out'])[0]
    rows = []
    for h in range(NH):
        rows += [np.arange(h * 128, (h + 1) * 128), 512 + np.arange(h * 128, (h + 1) * 128)]
    w_outP = np.ascontiguousarray(w_out[np.concatenate(rows), :])
    w_r = np.ascontiguousarray(np.concatenate([f(inp['w_group'])[0], f(inp['w_expert'])[0]], axis=1))
    b_r = np.broadcast_to(np.concatenate([f(inp['b_group'])[0], f(inp['b_expert'])[0]])[None, :], (128, 36)).copy()
    n12 = np.concatenate([f(inp['norm1_w'])[0].reshape(8, 128).T, f(inp['norm2_w'])[0].reshape(8, 128).T], axis=1).copy()
    b_adaT = np.ascontiguousarray(f(inp['b_ada'])[0].reshape(48, 128).T)
    fnw = np.broadcast_to(f(inp['final_norm_w'])[None, :], (128, D)).copy()
    shared = dict(w_ada=f(inp['w_ada'])[0], b_adaT=b_adaT, n12=n12, w_inA=w_inA, w_ab=w_ab, convw=convw, hv=hv, dnw=dnw,
                  w_out=w_outP, w_r=w_r, b_r=b_r, w1=f(inp['w1'])[0][:NE_], w3=f(inp['w3'])[0][:NE_], w2=f(inp['w2'])[0][:NE_], fnw=fnw,
                  cbig=CBIG, ccol=CCOL)
    pos = np.asarray(inp['positions']).astype(np.int32)
    c = f(inp['c'])
    maps = []
    nsl = T // TS
    for core in range(B * nsl):
        b, s = core // nsl, core % nsl
        m = dict(shared)
        m['xT'] = np.ascontiguousarray(x[b].T)
        m['xs'] = np.ascontiguousarray(x[b, s * TS:(s + 1) * TS, :])
        m['pos'] = np.ascontiguousarray(pos[b][None, :])
        m['cT'] = np.ascontiguousarray(c[b].reshape(8, 128).T)
        sm = np.zeros((128, 8), np.float32)
        sm[:, s] = 1.0
        m['selm'] = sm
        maps.append(m)
    return maps


_NC_CACHE = {}


def _run(inp, T, TS):
    key = (T, TS)
    if key not in _NC_CACHE:
        _NC_CACHE[key] = build(T, TS, SBW=min(256, TS))
    nc = _NC_CACHE[key]
    maps = _host_inputs(inp, T, TS)
    res = run_bass_kernel_spmd(nc, maps, core_ids=list(range(len(maps))))
    B = np.asarray(inp['x']).shape[0]
    nsl = T // TS
    out = np.zeros((B, T, D), np.float32)
    for core in range(B * nsl):
        b, s = core // nsl, core % nsl
        out[b, s * TS:(s + 1) * TS, :] = np.asarray(res.results[core]['out'])
    return out


def kernel(**inputs):
    return _run(inputs, 8192, 2048)
```

```python
import math
import os
from contextlib import ExitStack
import numpy as np
import concourse.bass as bass
import concourse.mybir as mybir
from concourse.bass_utils import run_bass_kernel_spmd

F32 = mybir.dt.float32
BF16 = mybir.dt.bfloat16
I32 = mybir.dt.int32
AF = mybir.ActivationFunctionType
ALU = mybir.AluOpType
AX = mybir.AxisListType

D = 1024
NHEAD = 4
EPS = 1e-6
NEXP = 32
DEXP = 512
TWO_PI = 2.0 * math.pi


class Buf:
    def __init__(self, ap, name):
        self.ap = ap
        self.name = name
        self.ws = {}
        self.rs = {}
        self.dsem = None
        self.dcnt = 0
        self.psum = False

    def __getitem__(self, k):
        return self.ap[k]


class Sched:
    EPOCH = 30000

    def __init__(self, nc, es):
        self.nc = nc
        self.es = es
        self.eng = {'pe': nc.tensor, 'act': nc.scalar, 'dve': nc.vector, 'pool': nc.gpsimd, 'sp': nc.sync}
        self.sem = {}
        self.cnt = {}
        self.waited = {e: {} for e in self.eng}
        self.nsem = 0
        self.allsems = {}
        for e in self.eng:
            self._newsem(e)
        self.bufs = []
        self.ninst = {e: 0 for e in self.eng}
        self.cache = {}
        self.pbanks = []
        self.pi = 0

    def _mksem(self, name):
        self.nsem += 1
        s = self.es.enter_context(self.nc.semaphore(name))
        return s

    def _newsem(self, e):
        self.sem[e] = self._mksem(f"c_{e}_{self.nsem}")
        self.cnt[e] = 0

    def sb(self, name, shape, dt, es=None):
        self.uid = getattr(self, 'uid', 0) + 1
        name = f"s{self.uid}_{name}"
        t = (es or self.es).enter_context(self.nc.sbuf_tensor(name, list(shape), dt))
        b = Buf(t, name)
        self.bufs.append(b)
        return b

    def g(self, name, shape, dt):
        if name not in self.cache:
            self.cache[name] = self.sb(name, shape, dt, es=self.cache_es)
        return self.cache[name]

    def mkpsum(self):
        for i in range(8):
            t = self.es.enter_context(self.nc.psum_tensor(f"pb{i}", [128, 512], F32))
            b = Buf(t, f"pb{i}")
            b.psum = True
            self.bufs.append(b)
            self.pbanks.append(b)

    def P(self):
        b = self.pbanks[self.pi % 8]
        self.pi += 1
        return b

    def dram(self, name, shape, dt, kind="Internal"):
        t = self.nc.dram_tensor(name, list(shape), dt, kind=kind).ap()
        b = Buf(t, name)
        self.bufs.append(b)
        return b

    def _deps(self, reads, writes, e=None):
        deps = {}

        def add(d):
            for k, (s, v) in d.items():
                if k not in deps or deps[k][1] < v:
                    deps[k] = (s, v)
        for b in reads:
            add(b.ws)
            if b.psum:
                own = id(self.sem[e]) if e in self.sem else None
                add({k: v for k, v in b.rs.items() if k != own})
        for b in writes:
            add(b.ws)
            add(b.rs)
        return deps

    def _wait(self, e, deps):
        for k, (s, v) in deps.items():
            if e == 'pe' and s is self.sem['pe']:
                continue
            if self.waited[e].get(k, 0) >= v:
                continue
            self.eng[e].wait_ge(s, v)
            self.ninst[e] += 1
            self.waited[e][k] = v

    def _record(self, ev, reads, writes):
        k = id(ev[0])
        for b in reads:
            if k not in b.rs or b.rs[k][1] < ev[1]:
                b.rs[k] = ev
        for b in writes:
            if k not in b.ws or b.ws[k][1] < ev[1]:
                b.ws[k] = ev

    def op(self, e, fn, reads=(), writes=()):
        if e == 'pool' and os.environ.get('KNOPOOL'):
            e = 'dve'
        self._wait(e, self._deps(reads, writes, e))
        ins = fn(self.eng[e])
        if self.cnt[e] >= self.EPOCH:
            self._newsem(e)
        self.cnt[e] += 1
        ins.then_inc(self.sem[e], 1)
        self.ninst[e] += 1
        self._record((self.sem[e], self.cnt[e]), reads, writes)
        return ins

    def dma(self, q, out_ap, in_ap, reads=(), writes=(), sembuf=None, **kw):
        self._wait(q, self._deps(reads, writes))
        if sembuf.dsem is None:
            sembuf.dsem = self._mksem(f"d_{sembuf.name}")
        ins = self.eng[q].dma_start(out=out_ap, in_=in_ap, **kw)
        sembuf.dcnt += 1
        ins.then_inc(sembuf.dsem, 16)
        self.ninst[q] += 1
        self._record((sembuf.dsem, 16 * sembuf.dcnt), reads, writes)
        return ins

    def barrier(self, engines=('pe', 'act', 'dve', 'pool', 'sp')):
        deps = {}
        for b in self.bufs:
            for d in (b.ws, b.rs):
                for k, (s, v) in d.items():
                    if k not in deps or deps[k][1] < v:
                        deps[k] = (s, v)
        for e in engines:
            self._wait(e, deps)

    def act(self, out, in_, func, reads, writes, **kw):
        return self.op('act', lambda e: e.activation(out=out, in_=in_, func=func, **kw), reads, writes)

    def tt(self, eng, out, a, b, op, reads, writes):
        return self.op(eng, lambda e: e.tensor_tensor(out=out, in0=a, in1=b, op=op), reads, writes)

    def ts(self, eng, out, a, s1, op0, reads, writes, s2=None, op1=None):
        if op1 is None:
            return self.op(eng, lambda e: e.tensor_scalar(out=out, in0=a, scalar1=s1, scalar2=None, op0=op0), reads, writes)
        return self.op(eng, lambda e: e.tensor_scalar(out=out, in0=a, scalar1=s1, scalar2=s2, op0=op0, op1=op1), reads, writes)

    def stt(self, out, a, sc, b, op0, op1, reads, writes):
        return self.op('dve', lambda e: e.scalar_tensor_tensor(out=out, in0=a, scalar=sc, in1=b, op0=op0, op1=op1), reads, writes)

    def cp(self, eng, out, in_, reads, writes):
        if eng == 'act':
            return self.act(out, in_, AF.Copy, reads, writes)
        return self.op(eng, lambda e: e.tensor_copy(out=out, in_=in_), reads, writes)

    def mm(self, out, lhsT, rhs, reads, writes, start=True, stop=True):
        return self.op('pe', lambda e: e.matmul(out, lhsT=lhsT, rhs=rhs, start=start, stop=stop), reads, writes)

    def tr(self, out, in_, ident, reads, writes):
        return self.op('pe', lambda e: e.transpose(out=out, in_=in_, identity=ident), reads, writes)

    def rsqrt(self, out, in_, scale, reads_in, tmp, eps=EPS):
        self.act(tmp[0], in_, AF.Ln, list(reads_in) + [self.epsbuf], [tmp[1]], bias=self.epsc[:in_.shape[0], 0:1], scale=scale)
        self.act(out[0], tmp[0], AF.Exp, [tmp[1]], [out[1]], scale=-0.5)


def rr(gens):
    gens = list(gens)
    while gens:
        nxt = []
        for g_ in gens:
            try:
                next(g_)
                nxt.append(g_)
            except StopIteration:
                pass
        gens = nxt


def _consts():
    i = np.arange(128)
    c = {}
    c['ident'] = np.eye(128, dtype=np.float32)
    c['triU'] = (i[:, None] <= i[None, :]).astype(np.float32)
    c['negtriU'] = -c['triU']
    c['negmask'] = np.where(i[None, :] <= i[:, None], 0.0, -1e30).astype(np.float32)
    c['m2L'] = ((i[:, None] == i[None, :] + 1) & (i[:, None] % 2 == 1)).astype(np.float32)
    for s in (4, 8, 16, 32, 64):
        c[f'bm{s}'] = ((i[:, None] // s) == (i[None, :] // s)).astype(np.float32)
    gam = 1.0 - np.power(2.0, -5.0 - np.arange(NHEAD))
    lg = np.log1p(-np.power(2.0, -5.0 - np.arange(NHEAD, dtype=np.float64)))
    rel = i[None, :] - i[:, None]
    for h in range(NHEAD):
        c[f'decT{h}'] = (np.where(rel >= 0, np.exp(lg[h] * np.maximum(rel, 0)), 0.0) * 128 ** -0.5).astype(np.float32)
        c[f'qdec{h}'] = np.broadcast_to(np.exp(lg[h] * (i + 1.0))[None, :], (128, 128)).astype(np.float32).copy()
    kws = np.stack([np.exp(lg[h] * (127 - i)) * 128 ** -0.5 for h in range(NHEAD)], axis=1)
    cd = [float(np.exp(lg[h] * 128)) for h in range(NHEAD)]
    invf = (10000.0 ** (-(np.arange(0, 128, 2, dtype=np.float32)) / 128.0)).astype(np.float32)
    col = np.zeros((128, 8), np.float32)
    col[:, 0] = np.concatenate([invf, invf])
    col[:, 1] = np.concatenate([-np.ones(64), np.ones(64)])
    col[:, 2] = EPS
    col[:, 3] = math.pi / 2
    col[:, 4:8] = kws
    names = ['ident', 'triU', 'negtriU', 'negmask', 'm2L', 'bm4', 'bm8', 'bm16', 'bm32', 'bm64'] + \
            [f'decT{h}' for h in range(NHEAD)] + [f'qdec{h}' for h in range(NHEAD)]
    big = np.concatenate([c[n] for n in names], axis=1).astype(np.float32)
    return names, big, col, cd


CNAMES, CBIG, CCOL, CD = _consts()


def build(T, TS, SBW=256, dbg=False):
    NH = NHEAD
    NSB = T // SBW
    NT = SBW // 128
    NCOL = NH * 10 * 128
    NE = int(os.environ.get('KNEXP', NEXP))
    nc = bass.Bass("TRN2", target_bir_lowering=False)
    es = ExitStack()
    with es:
        S = Sched(nc, es)
        S.mkpsum()
        dt_in = {}

        def din(name, shape, dt=F32):
            dt_in[name] = nc.dram_tensor(name, list(shape), dt, kind="ExternalInput").ap()
            return dt_in[name]
        xT = din("xT", [D, T])
        xs = din("xs", [TS, D])
        pos = din("pos", [1, T], I32)
        cT = din("cT", [128, 8])
        w_ada = din("w_ada", [D, 6 * D])
        b_adaT = din("b_adaT", [128, 48])
        n12 = din("n12", [128, 16])
        w_inA = din("w_inA", [D, NCOL])
        w_ab = din("w_ab", [D, 2 * NH])
        convw = din("convw", [128, NH * 12])
        hv = din("hv", [128, 2 * NH])
        dnw = din("dnw", [128, 1])
        w_out = din("w_out", [D, D])
        w_r = din("w_r", [D, 36])
        b_r = din("b_r", [128, 36])
        w1 = din("w1", [NE, D, DEXP])
        w3 = din("w3", [NE, D, DEXP])
        w2 = din("w2", [NE, DEXP, D])
        fnw = din("fnw", [128, D])
        cbig = din("cbig", [128, CBIG.shape[1]])
        ccol = din("ccol", [128, 8])
        selm_in = din("selm", [128, 8])
        out = nc.dram_tensor("out", [TS, D], F32, kind="ExternalOutput").ap()
        x1_d = S.dram("x1_d", [TS, D], F32)
        dbg_outs = {}

        cb = S.sb("cbig", [128, CBIG.shape[1]], F32)
        S.dma('sp', cb[:], cbig[:, :], writes=[cb], sembuf=cb)
        cc = S.sb("ccol", [128, 8], F32)
        S.dma('sp', cc[:], ccol[:, :], writes=[cc], sembuf=cc)
        S.epsc = cc.ap[:, 2:3]
        S.epsbuf = cc
        C = {n: cb.ap[:, k * 128:(k + 1) * 128] for k, n in enumerate(CNAMES)}
        ident_b = S.sb("ident_b", [128, 128], BF16)
        S.cp('dve', ident_b[:], C['ident'], [cb], [ident_b])
        ones_b = S.sb("ones_b", [128, 128], BF16)
        S.op('pool', lambda e: e.memset(ones_b[:], 1.0), [], [ones_b])
        ones_f = S.sb("ones_f", [128, 128], F32)
        S.op('pool', lambda e: e.memset(ones_f[:], 1.0), [], [ones_f])
        bmb = {}
        for s_ in (4, 8, 16, 32, 64):
            bmb[s_] = S.sb(f"bmb{s_}", [128, 128], BF16)
            S.cp('dve', bmb[s_][:], C[f'bm{s_}'], [cb], [bmb[s_]])
        m2Lb = S.sb("m2Lb", [128, 128], BF16)
        S.cp('dve', m2Lb[:], C['m2L'], [cb], [m2Lb])
        selm = S.sb("selm", [128, 8], F32)
        S.dma('sp', selm[:], selm_in[:, :], writes=[selm], sembuf=selm)
        catT = S.sb("catT", [128, 8, TS], BF16)
        S.op('pool', lambda e: e.memset(catT[:], 0.0), [], [catT])
        mod = S.sb("mod", [128, 48], F32)
        a1 = S.sb("a1", [128, 8], F32)
        a2 = S.sb("a2", [128, 8], F32)
        g12 = S.sb("g12", [128, 2 * D], F32)

        with ExitStack() as es0:
            S.cache_es = es0
            ct = S.sb("ct", [128, 8], F32, es0)
            S.dma('sp', ct[:], cT[:, :], writes=[ct], sembuf=ct)
            sct = S.sb("sct", [128, 8], F32, es0)
            S.act(sct[:], ct[:], AF.Silu, [ct], [sct])
            bad = S.sb("bad", [128, 48], F32, es0)
            S.dma('sp', bad[:], b_adaT[:, :], writes=[bad], sembuf=bad)
            n12t = S.sb("n12t", [128, 16], F32, es0)
            S.dma('sp', n12t[:], n12[:, :], writes=[n12t], sembuf=n12t)
            pm = S.P()
            for j in range(6):
                wa = S.g(f"wada{j % 2}", [128, 8, D], F32)
                S.dma('sp', wa[:], w_ada[:, j * D:(j + 1) * D].rearrange("(c p) n -> p c n", p=128), writes=[wa], sembuf=wa)
                for oc in range(8):
                    col_ = j * 8 + oc
                    for c in range(8):
                        S.mm(pm[:, col_:col_ + 1], wa[:, c, oc * 128:(oc + 1) * 128], sct[:, c:c + 1], [wa, sct], [pm],
                             start=(c == 0), stop=(c == 7))
            S.tt('dve', mod[:], pm[:, 0:48], bad[:], ALU.add, [pm, bad], [mod])
            S.stt(a1[:], mod[:, 8:16], 1.0, n12t[:, 0:8], ALU.add, ALU.mult, [mod, n12t], [a1])
            S.stt(a2[:], mod[:, 32:40], 1.0, n12t[:, 8:16], ALU.add, ALU.mult, [mod, n12t], [a2])
            for gi, base in ((0, 16), (1, 40)):
                for c in range(8):
                    dg = S.g(f"dg{c % 2}", [128, 128], F32)
                    S.ts('dve', dg[:], C['ident'], mod[:, base + c:base + c + 1], ALU.mult, [cb, mod], [dg])
                    pg = S.P()
                    S.mm(pg[:, 0:128], ones_f[:], dg[:], [ones_f, dg], [pg])
                    S.cp('act', g12[:, gi * D + c * 128:gi * D + (c + 1) * 128], pg[:, 0:128], [pg], [g12])
            S.barrier()
        S.cache = {}
        if os.environ.get('KSTOP') == 'pre':
            return nc

        for phase in ('ret', 'gdn'):
            with ExitStack() as esA:
                S.cache_es = esA
                G = S.g
                if phase == 'ret':
                    PC0, PCN = 0, NH * 6 * 128
                else:
                    PC0, PCN = NH * 6 * 128, NH * 4 * 128
                Win = S.sb("Win" + phase, [128, 8, PCN], BF16, esA)
                for c in range(8):
                    S.dma('pool', Win[:, c, :], w_inA[c * 128:(c + 1) * 128, PC0:PC0 + PCN], writes=[Win], sembuf=Win)
                Wab = S.sb("Wab" + phase, [128, 8, 2 * NH], BF16, esA)
                S.dma('pool', Wab[:], w_ab.rearrange("(c p) n -> p c n", p=128), writes=[Wab], sembuf=Wab)
                cw = S.sb("cw" + phase, [128, NH * 12], F32, esA)
                S.dma('sp', cw[:], convw[:, :], writes=[cw], sembuf=cw)
                hvt = S.sb("hvt" + phase, [128, 2 * NH], F32, esA)
                S.dma('sp', hvt[:], hv[:, :], writes=[hvt], sembuf=hvt)
                dnwt = S.sb("dnwt" + phase, [128, 1], F32, esA)
                S.dma('sp', dnwt[:], dnw[:, :], writes=[dnwt], sembuf=dnwt)
                nea = S.sb("nea" + phase, [128, NH], F32, esA)
                S.act(nea[:], hvt[:, 0:NH], AF.Exp, [hvt], [nea])
                S.ts('dve', nea[:], nea[:], -1.0, ALU.mult, [nea], [nea])
                Sr, Srb, Sg, Sgb, cbuf = [], [], [], [], []
                for h in range(NH):
                    for lst, nm, dt in ((Sr, "Sr", F32), (Srb, "Srb", BF16), (Sg, "Sg", F32), (Sgb, "Sgb", BF16)):
                        b = S.sb(f"{nm}{h}{phase}", [128, 128], dt, esA)
                        S.op('pool', lambda e, b=b: e.memset(b[:], 0.0), [], [b])
                        lst.append(b)
                    row = []
                    for w_ in range(3):
                        b = S.sb(f"cbuf{h}_{w_}{phase}", [128, SBW + 3], F32, esA)
                        S.op('pool', lambda e, b=b: e.memset(b[:], 0.0), [], [b])
                        row.append(b)
                    cbuf.append(row)

                def proj(hT, col):
                    ps = S.P()
                    for c in range(8):
                        S.mm(ps[:, 0:SBW], Win[:, c, col - PC0:col - PC0 + 128], hT[:, c, :], [Win, hT], [ps], start=(c == 0), stop=(c == 7))
                    return ps

                for sb in range(NSB):
                    t0 = sb * SBW
                    xt = G("xt", [128, 8, SBW], F32)
                    S.dma('sp', xt[:], xT[:, t0:t0 + SBW].rearrange("(c p) t -> p c t", p=128), writes=[xt], sembuf=xt)
                    xsq = G("xsq", [128, 8, SBW], BF16)
                    S.act(xsq[:], xt[:], AF.Square, [xt], [xsq])
                    pss = S.P()
                    for c in range(8):
                        S.mm(pss[:, 0:SBW], ones_b[:], xsq[:, c, :], [ones_b, xsq], [pss], start=(c == 0), stop=(c == 7))
                    lnt = G("lnt", [128, SBW], F32)
                    rstd = G("rstd", [128, SBW], F32)
                    S.rsqrt((rstd[:], rstd), pss[:, 0:SBW], 1.0 / D, [pss], (lnt[:], lnt))
                    hT = G("hT", [128, 8, SBW], BF16)
                    for c in range(8):
                        tmp = G(f"xn{c % 2}", [128, SBW], F32)
                        S.tt('dve' if c % 2 == 0 else 'pool', tmp[:], xt[:, c, :], rstd[:], ALU.mult, [xt, rstd], [tmp])
                        S.act(hT[:, c, :], tmp[:], AF.Identity, [tmp, a1, mod], [hT], bias=mod[:, c:c + 1], scale=a1[:, c:c + 1])
                    if os.environ.get('KSTOP') == 'ret1':
                        S.barrier(engines=('sp',))
                        return nc
                    if phase == 'ret':
                        posi = G("posi", [128, SBW], I32)
                        S.dma('sp', posi[:], pos[0:1, t0:t0 + SBW].partition_broadcast(128), writes=[posi], sembuf=posi)
                        posf = G("posf", [128, SBW], F32)
                        S.cp('dve', posf[:], posi[:], [posi], [posf])
                        ang = G("ang", [128, SBW], F32)
                        S.ts('dve', ang[:], posf[:], cc[:, 0:1], ALU.mult, [posf, cc], [ang])
                        tabs = []
                        for which in range(2):
                            if which == 1:
                                ang2 = G("ang2", [128, SBW], F32)
                                S.ts('pool', ang2[:], ang[:], math.pi / 2, ALU.add, [ang], [ang2])
                                a_ = ang2
                            else:
                                a_ = ang
                            ki = G(f"ki{which}", [128, SBW], I32)
                            S.ts('dve', ki[:], a_[:], 1.0 / TWO_PI, ALU.mult, [a_], [ki])
                            kf = G(f"kf{which}", [128, SBW], F32)
                            S.cp('pool', kf[:], ki[:], [ki], [kf])
                            rr_ = G(f"rr{which}", [128, SBW], F32)
                            S.stt(rr_[:], kf[:], -TWO_PI, a_[:], ALU.mult, ALU.add, [kf, a_], [rr_])
                            S.ts('pool', rr_[:], rr_[:], math.pi, ALU.min, [rr_], [rr_], s2=-math.pi, op1=ALU.max)
                            tb = G(f"tab{which}", [128, SBW], F32)
                            S.act(tb[:], rr_[:], AF.Sin, [rr_], [tb])
                            tabs.append(tb)
                        sint, cost = tabs
                        sins = G("sins", [128, SBW], F32)
                        S.act(sins[:], sint[:], AF.Identity, [sint, cc], [sins], scale=cc[:, 1:2])

                        def ret_prep(h):
                            base = h * 6 * 128
                            for nm, off in (("q", 0), ("k", 2)):
                                t1 = G(f"rt1{nm}{h}", [128, SBW], F32)
                                t2 = G(f"rt2{nm}{h}", [128, SBW], F32)
                                p1 = proj(hT, base + off * 128)
                                S.tt('dve', t1[:], p1[:, 0:SBW], cost[:], ALU.mult, [p1, cost], [t1])
                                yield
                                p2 = proj(hT, base + (off + 1) * 128)
                                S.tt('dve', t2[:], p2[:, 0:SBW], sins[:], ALU.mult, [p2, sins], [t2])
                                yield
                                o_ = G(f"r{nm}T{h}", [128, SBW], BF16)
                                S.tt('pool', o_[:], t1[:], t2[:], ALU.add, [t1, t2], [o_])
                                yield
                            pv = proj(hT, base + 4 * 128)
                            vT = G(f"rvT{h}", [128, SBW], BF16)
                            S.cp('act', vT[:], pv[:, 0:SBW], [pv], [vT])
                            yield
                            pg_ = proj(hT, base + 5 * 128)
                            sgT = G(f"rsgT{h}", [128, SBW], F32)
                            S.act(sgT[:], pg_[:, 0:SBW], AF.Silu, [pg_], [sgT])
                            yield

                        def ret_chain(tt, h):
                            sl = slice(tt * 128, (tt + 1) * 128)
                            qrT, krT, vT, sgT = (S.cache[f"rqT{h}"], S.cache[f"rkT{h}"], S.cache[f"rvT{h}"], S.cache[f"rsgT{h}"])
                            pk = S.P()
                            pkb = pk.ap[:].bitcast(BF16)
                            S.tr(pkb[:, 0:128], krT[:, sl], ident_b[:], [krT, ident_b], [pk])
                            S.tr(pkb[:, 128:256], vT[:, sl], ident_b[:], [vT, ident_b], [pk])
                            kw = G(f"rkw{h}", [128, 128], BF16)
                            S.ts('dve', kw[:], pkb[:, 0:128], cc[:, 4 + h:5 + h], ALU.mult, [pk, cc], [kw])
                            vtok = G(f"rvtok{h}", [128, 128], BF16)
                            S.cp('act', vtok[:], pkb[:, 128:256], [pk], [vtok])
                            qwT = G(f"rqw{h}", [128, 128], BF16)
                            S.tt('pool', qwT[:], qrT[:, sl], C[f'qdec{h}'], ALU.mult, [qrT, cb], [qwT])
                            yield
                            psc = S.P()
                            S.mm(psc[:, 0:128], krT[:, sl], qrT[:, sl], [krT, qrT], [psc])
                            sT = G(f"rsT{h}", [128, 128], BF16)
                            S.tt('dve', sT[:], psc[:, 0:128], C[f'decT{h}'], ALU.mult, [psc, cb], [sT])
                            yield
                            po = S.P()
                            S.mm(po[:, 0:128], sT[:], vtok[:], [sT, vtok], [po], start=True, stop=False)
                            S.mm(po[:, 0:128], qwT[:], Srb[h][:], [qwT, Srb[h]], [po], start=False, stop=True)
                            S.mm(po[:, 128:256], kw[:], vtok[:], [kw, vtok], [po])
                            S.stt(Sr[h][:], Sr[h][:], CD[h], po[:, 128:256], ALU.mult, ALU.add, [Sr[h], po], [Sr[h]])
                            osb = G(f"rosb{h}", [128, 128], F32)
                            S.cp('act', osb[:], po[:, 0:128], [po], [osb])
                            yield
                            S.cp('act', Srb[h][:], Sr[h][:], [Sr[h]], [Srb[h]])
                            junk = G(f"rjunk{h}", [128, 128], F32)
                            ssq = G(f"rssq{h}", [128, 4], F32)
                            S.op('pool', lambda e, ssq=ssq: e.memset(ssq[:], 0.0), [], [ssq])
                            yield
                            S.act(junk[:], osb[:], AF.Square, [osb], [junk, ssq], accum_out=ssq[:, 0:1])
                            yield
                            S.act(ssq[:, 1:2], ssq[:, 0:1], AF.Ln, [ssq, cc], [ssq], bias=cc[:, 2:3], scale=1.0 / 128)
                            yield
                            S.act(ssq[:, 2:3], ssq[:, 1:2], AF.Exp, [ssq], [ssq], scale=-0.5)
                            yield
                            on = G(f"ron{h}", [128, 128], F32)
                            S.ts('dve', on[:], osb[:], ssq[:, 2:3], ALU.mult, [osb, ssq], [on])
                            yield
                            pt = S.P()
                            S.tr(pt[:, 0:128], on[:], C['ident'], [on, cb], [pt])
                            cst = G(f"rcst{h}", [128, SBW], BF16)
                            S.tt('dve', cst[:, sl], pt[:, 0:128], sgT[:, sl], ALU.mult, [pt, sgT], [cst])
                            yield

                        rr([ret_prep(h) for h in range(NH)])
                        for tt in range(NT):
                            rr([ret_chain(tt, h) for h in range(NH)])
                        for h in range(NH):
                            cst = S.cache[f"rcst{h}"]
                            S.stt(catT[:, 2 * h, t0 % TS:t0 % TS + SBW], cst[:], selm[:, t0 // TS:t0 // TS + 1], catT[:, 2 * h, t0 % TS:t0 % TS + SBW],
                                  ALU.mult, ALU.add, [cst, selm, catT], [catT])

                    if phase == 'gdn':
                        def gdn_prep(h, w_):
                            nm = ("q", "k", "v")[w_]
                            base = NH * 6 * 128 + h * 4 * 128
                            cbf = cbuf[h][w_]
                            ps = proj(hT, base + w_ * 128)
                            S.cp('act', cbf[:, 3:3 + SBW], ps[:, 0:SBW], [ps], [cbf])
                            yield
                            acc = G(f"gacc{h}_{w_}", [128, SBW], F32)
                            wc = h * 12 + w_ * 4
                            S.act(acc[:], cbf[:, 0:SBW], AF.Identity, [cbf, cw], [acc], scale=cw[:, wc:wc + 1])
                            yield
                            for j in range(1, 4):
                                S.stt(acc[:], cbf[:, j:j + SBW], cw[:, wc + j:wc + j + 1], acc[:], ALU.mult, ALU.add, [cbf, cw, acc], [acc])
                                yield
                            tl = G(f"gtail{h}_{w_}", [128, 4], F32)
                            S.cp('pool', tl[:, 0:3], cbf[:, SBW:SBW + 3], [cbf], [tl])
                            yield
                            S.cp('pool', cbf[:, 0:3], tl[:, 0:3], [tl], [cbf])
                            yield
                            if nm == "v":
                                vT = G(f"gvT{h}", [128, SBW], BF16)
                                S.act(vT[:], acc[:], AF.Silu, [acc], [vT])
                                yield
                            else:
                                y = acc
                                S.act(y[:], acc[:], AF.Silu, [acc], [y])
                                yield
                                sq = G(f"gsq{h}_{w_}", [128, SBW], BF16)
                                S.act(sq[:], y[:], AF.Square, [y], [sq])
                                yield
                                pn = S.P()
                                S.mm(pn[:, 0:SBW], ones_b[:], sq[:], [ones_b, sq], [pn])
                                rn = G(f"grn{h}_{w_}", [128, SBW], F32)
                                S.act(rn[:], pn[:, 0:SBW], AF.Ln, [pn, cc], [rn], bias=cc[:, 2:3], scale=1.0)
                                yield
                                S.act(rn[:], rn[:], AF.Exp, [rn], [rn], scale=-0.5)
                                yield
                                o_ = G(f"g{nm}nT{h}", [128, SBW], BF16)
                                if nm == "q":
                                    S.stt(o_[:], y[:], 128 ** -0.5, rn[:], ALU.mult, ALU.mult, [y, rn], [o_])
                                else:
                                    S.tt('pool', o_[:], y[:], rn[:], ALU.mult, [y, rn], [o_])
                                yield

                        def gdn_prep_z(h):
                            base = NH * 6 * 128 + h * 4 * 128
                            pz = proj(hT, base + 3 * 128)
                            szT = G(f"gszT{h}", [128, SBW], F32)
                            S.act(szT[:], pz[:, 0:SBW], AF.Silu, [pz], [szT])
                            yield

                        def gdn_scal(tt):
                            sl = slice(tt * 128, (tt + 1) * 128)
                            sc = G(f"gsc{tt}", [128, 64], F32)
                            pab = S.P()
                            for c in range(8):
                                S.mm(pab[:, 0:2 * NH], hT[:, c, sl], Wab[:, c, :], [hT, Wab], [pab], start=(c == 0), stop=(c == 7))
                            S.tt('dve', sc[:, 0:4], pab[:, 0:NH], hvt[:, NH:2 * NH], ALU.add, [pab, hvt], [sc])
                            S.act(sc[:, 16:20], pab[:, NH:2 * NH], AF.Exp, [pab], [sc], scale=-1.0)
                            yield
                            S.act(sc[:, 4:8], sc[:, 0:4], AF.Exp, [sc], [sc])
                            yield
                            S.act(sc[:, 8:12], sc[:, 4:8], AF.Ln, [sc], [sc], bias=1.0)
                            yield
                            S.tt('dve', sc[:, 12:16], sc[:, 8:12], nea[:], ALU.mult, [sc, nea], [sc])
                            yield
                            S.ts('dve', sc[:, 16:20], sc[:, 16:20], 1.0, ALU.add, [sc], [sc])
                            yield
                            S.op('dve', lambda e, sc=sc: e.reciprocal(out=sc[:, 20:24], in_=sc[:, 16:20]), [sc], [sc])
                            yield
                            pgc = S.P()
                            S.mm(pgc[:, 0:NH], C['triU'], sc[:, 12:16], [cb, sc], [pgc])
                            S.mm(pgc[:, 8:8 + NH], ones_f[:], sc[:, 12:16], [ones_f, sc], [pgc])
                            S.cp('dve', sc[:, 24:28], pgc[:, 0:NH], [pgc], [sc])
                            S.cp('dve', sc[:, 28:32], pgc[:, 8:8 + NH], [pgc], [sc])
                            yield
                            S.act(sc[:, 32:40], sc[:, 24:32], AF.Exp, [sc], [sc])
                            yield
                            S.tt('dve', sc[:, 40:44], sc[:, 28:32], sc[:, 24:28], ALU.subtract, [sc], [sc])
                            yield
                            S.act(sc[:, 44:48], sc[:, 40:44], AF.Exp, [sc], [sc])
                            yield
                            S.tt('dve', sc[:, 48:52], sc[:, 20:24], sc[:, 32:36], ALU.mult, [sc], [sc])
                            yield

                        def chain_pre(ch):
                            key, tt, h, sl = ch['key'], ch['tt'], ch['h'], ch['sl']
                            sc = S.cache[f"gsc{tt}"]
                            ch['sc'] = sc
                            qnT, knT, vT = S.cache[f"gqnT{h}"], S.cache[f"gknT{h}"], S.cache[f"gvT{h}"]
                            F = [G(f"gF{i}_{key}", [128, 128], F32) for i in range(4)]
                            Bq = [G(f"gB{i}_{key}", [128, 128], BF16) for i in range(2)]
                            gb = F[0]
                            S.act(gb[:], ones_f[:], AF.Identity, [ones_f, sc], [gb], scale=sc[:, 12 + h:13 + h])
                            pT = S.P()
                            pTb = pT.ap[:].bitcast(BF16)
                            S.tr(pTb[:, 128:256], knT[:, sl], ident_b[:], [knT, ident_b], [pT])
                            S.tr(pTb[:, 256:384], vT[:, sl], ident_b[:], [vT, ident_b], [pT])
                            kbg = G(f"gkbg{key}", [128, 128], BF16)
                            S.act(kbg[:], pTb[:, 128:256], AF.Identity, [pT, sc], [kbg], scale=sc[:, 48 + h:49 + h])
                            kt = G(f"gkt{key}", [128, 128], BF16)
                            S.ts('dve', kt[:], pTb[:, 128:256], sc[:, 44 + h:45 + h], ALU.mult, [pT, sc], [kt])
                            vb = G(f"gvb{key}", [128, 128], BF16)
                            S.act(vb[:], pTb[:, 256:384], AF.Identity, [pT, sc], [vb], scale=sc[:, 20 + h:21 + h])
                            yield
                            pG = S.P()
                            S.mm(pG[:, 0:128], C['triU'], gb[:], [cb, gb], [pG], start=True, stop=False)
                            S.mm(pG[:, 0:128], gb[:], C['negtriU'], [cb, gb], [pG], start=False, stop=True)
                            ex = F[1]
                            S.stt(ex[:], pG[:, 0:128], 0.0, C['negmask'], ALU.min, ALU.add, [pG, cb], [ex])
                            yield
                            dec_i = F[1]
                            S.act(dec_i[:], ex[:], AF.Exp, [ex], [dec_i])
                            yield
                            dec_s = F[2]
                            S.tt('pool', dec_s[:], dec_i[:], C['ident'], ALU.subtract, [dec_i, cb], [dec_s])
                            yield
                            pK = S.P()
                            S.mm(pK[:, 0:128], knT[:, sl], knT[:, sl], [knT], [pK])
                            S.mm(pK[:, 128:256], qnT[:, sl], knT[:, sl], [qnT, knT], [pK])
                            A = G(f"gA{key}", [128, 128], BF16)
                            S.stt(A[:], pK[:, 0:128], sc[:, 20 + h:21 + h], dec_s[:], ALU.mult, ALU.mult, [pK, sc, dec_s], [A])
                            attn = Bq[0]
                            S.tt('dve', attn[:], pK[:, 128:256], dec_i[:], ALU.mult, [pK, dec_i], [attn])
                            yield
                            tm = Bq[1]
                            S.tt('pool', tm[:], A[:], m2Lb[:], ALU.mult, [A, m2Lb], [tm])
                            pT2 = S.P()
                            pT2b = pT2.ap[:].bitcast(BF16)
                            S.tr(pT2b[:, 0:128], attn[:], ident_b[:], [attn, ident_b], [pT2])
                            attnT = G(f"gattnT{key}", [128, 128], BF16)
                            S.cp('act', attnT[:], pT2b[:, 0:128], [pT2], [attnT])
                            yield
                            T1 = G(f"gT0{key}", [128, 128], BF16)
                            S.tt('pool', T1[:], ident_b[:], tm[:], ALU.subtract, [ident_b, tm], [T1])
                            yield
                            pU = S.P()
                            pUb = pU.ap[:].bitcast(BF16)
                            S.tr(pUb[:, 0:128], T1[:], ident_b[:], [T1, ident_b], [pU])
                            U = G(f"gU0{key}", [128, 128], BF16)
                            S.cp('act', U[:], pUb[:, 0:128], [pU], [U])
                            yield
                            Tk = T1
                            for lvl, bs in enumerate((4, 8, 16, 32, 64, None)):
                                pW = S.P()
                                S.mm(pW[:, 0:128], A[:], U[:], [A, U], [pW])
                                W = Bq[0]
                                S.tt('dve', W[:], pW[:, 0:128], U[:], ALU.add, [pW, U], [W])
                                yield
                                pW2 = S.P()
                                S.mm(pW2[:, 0:128], Tk[:], W[:], [Tk, W], [pW2])
                                Un = G(f"gU{(lvl + 1) % 2}{key}", [128, 128], BF16)
                                if bs is not None:
                                    tmpf = F[0]
                                    S.stt(tmpf[:], U[:], 2.0, pW2[:, 0:128], ALU.mult, ALU.subtract, [U, pW2], [tmpf])
                                    yield
                                    S.tt('pool', Un[:], tmpf[:], C[f'bm{bs}'], ALU.mult, [tmpf, cb], [Un])
                                    yield
                                    pX = S.P()
                                    pXb = pX.ap[:].bitcast(BF16)
                                    S.tr(pXb[:, 0:128], Un[:], ident_b[:], [Un, ident_b], [pX])
                                    Tn = G(f"gT{(lvl + 1) % 2}{key}", [128, 128], BF16)
                                    S.cp('act', Tn[:], pXb[:, 0:128], [pX], [Tn])
                                    yield
                                    Tk = Tn
                                else:
                                    S.stt(Un[:], U[:], 2.0, pW2[:, 0:128], ALU.mult, ALU.subtract, [U, pW2], [Un])
                                    yield
                                U = Un
                            pw = S.P()
                            S.mm(pw[:, 0:128], kbg[:], U[:], [kbg, U], [pw])
                            S.mm(pw[:, 128:256], U[:], vb[:], [U, vb], [pw])
                            wT = G(f"gwT{key}", [128, 128], BF16)
                            S.cp('act', wT[:], pw[:, 0:128], [pw], [wT])
                            u = F[3]
                            S.cp('dve', u[:], pw[:, 128:256], [pw], [u])
                            yield
                            ch.update(kt=kt, attnT=attnT, wT=wT, u=u, F=F, Bq=Bq)

                        def chain_scan(ch):
                            key, h, sl, sc, F, Bq = ch['key'], ch['h'], ch['sl'], ch['sc'], ch['F'], ch['Bq']
                            qnT, szT = S.cache[f"gqnT{h}"], S.cache[f"gszT{h}"]
                            p1 = S.P()
                            S.mm(p1[:, 0:128], ch['wT'][:], Sgb[h][:], [ch['wT'], Sgb[h]], [p1])
                            S.mm(p1[:, 128:256], qnT[:, sl], Sgb[h][:], [qnT, Sgb[h]], [p1])
                            vn = Bq[1]
                            S.tt('dve', vn[:], ch['u'][:], p1[:, 0:128], ALU.subtract, [ch['u'], p1], [vn])
                            o1 = F[1]
                            S.act(o1[:], p1[:, 128:256], AF.Identity, [p1, sc], [o1], scale=sc[:, 32 + h:33 + h])
                            yield
                            p2 = S.P()
                            S.mm(p2[:, 0:128], ch['kt'][:], vn[:], [ch['kt'], vn], [p2])
                            S.mm(p2[:, 128:256], ch['attnT'][:], vn[:], [ch['attnT'], vn], [p2])
                            S.stt(Sg[h][:], Sg[h][:], sc[:, 36 + h:37 + h], p2[:, 0:128], ALU.mult, ALU.add, [Sg[h], sc, p2], [Sg[h]])
                            o = F[2]
                            S.tt('dve', o[:], o1[:], p2[:, 128:256], ALU.add, [o1, p2], [o])
                            yield
                            S.cp('act', Sgb[h][:], Sg[h][:], [Sg[h]], [Sgb[h]])
                            junk = F[0]
                            ssq = G(f"gssq{key}", [128, 4], F32)
                            S.op('pool', lambda e, ssq=ssq: e.memset(ssq[:], 0.0), [], [ssq])
                            yield
                            S.act(junk[:], o[:], AF.Square, [o], [junk, ssq], accum_out=ssq[:, 0:1])
                            yield
                            S.act(ssq[:, 1:2], ssq[:, 0:1], AF.Ln, [ssq, cc], [ssq], bias=cc[:, 2:3], scale=1.0 / 128)
                            yield
                            S.act(ssq[:, 2:3], ssq[:, 1:2], AF.Exp, [ssq], [ssq], scale=-0.5)
                            yield
                            on = F[1]
                            S.act(on[:], o[:], AF.Identity, [o, ssq], [on], scale=ssq[:, 2:3])
                            yield
                            pt = S.P()
                            S.tr(pt[:, 0:128], on[:], C['ident'], [on, cb], [pt])
                            cst = G(f"gcst{h}", [128, SBW], BF16)
                            S.stt(cst[:, sl], pt[:, 0:128], dnwt[:, 0:1], szT[:, sl], ALU.mult, ALU.mult, [pt, dnwt, szT], [cst])
                            yield

                        rr([gdn_prep(h, w_) for h in range(NH) for w_ in range(3)] + [gdn_prep_z(h) for h in range(NH)]
                           + [gdn_scal(tt) for tt in range(NT)])
                        chains = [dict(key=f"{tt}_{h}", tt=tt, h=h, sl=slice(tt * 128, (tt + 1) * 128)) for tt in range(NT) for h in range(NH)]
                        rr([chain_pre(ch) for ch in chains])
                        for tt in range(NT):
                            rr([chain_scan(ch) for ch in chains if ch['tt'] == tt])
                        for h in range(NH):
                            cst = S.cache[f"gcst{h}"]
                            S.stt(catT[:, 2 * h + 1, t0 % TS:t0 % TS + SBW], cst[:], selm[:, t0 // TS:t0 // TS + 1], catT[:, 2 * h + 1, t0 % TS:t0 % TS + SBW],
                                  ALU.mult, ALU.add, [cst, selm, catT], [catT])
                print('SBUF remaining in phase', phase, nc.sbuf_bytes_remaining, flush=True)
                S.barrier()
            S.cache = {}
            print('SBUF remaining after phase', phase, nc.sbuf_bytes_remaining, flush=True) if False else None
            if os.environ.get('KSTOP') == phase:
                return nc

        NTB = TS // 128
        NSL = T // TS
        with ExitStack() as esB:
            fnwt = S.sb("fnwt", [128, D], F32, esB)
            S.dma('sp', fnwt[:], fnw[:, :], writes=[fnwt], sembuf=fnwt)
            h2T = catT
            Gt = S.sb("Gt", [128, NTB, 32], F32, esB)
            with ExitStack() as esB1:
                S.cache_es = esB1
                G = S.g
                Wout = S.sb("Wout", [128, 8, D], BF16, esB1)
                S.dma('pool', Wout[:], w_out.rearrange("(c p) n -> p c n", p=128), writes=[Wout], sembuf=Wout)
                Wr = S.sb("Wr", [128, 8, 36], F32, esB1)
                S.dma('sp', Wr[:], w_r.rearrange("(c p) n -> p c n", p=128), writes=[Wr], sembuf=Wr)
                brt = S.sb("brt", [128, 36], F32, esB1)
                S.dma('sp', brt[:], b_r[:, :], writes=[brt], sembuf=brt)
                for tt in range(NTB):
                    sl = slice(tt * 128, (tt + 1) * 128)
                    xst = G(f"xst{tt % 2}", [128, D], F32)
                    S.dma('sp', xst[:], xs[tt * 128:(tt + 1) * 128, :], writes=[xst], sembuf=xst)
                    x1 = G(f"x1_{tt % 2}", [128, D], F32)
                    for half in range(2):
                        hs = slice(half * 512, (half + 1) * 512)
                        pm_ = S.P()
                        for c in range(8):
                            S.mm(pm_[:, :], catT[:, c, sl], Wout[:, c, hs], [catT, Wout], [pm_], start=(c == 0), stop=(c == 7))
                        tmp = G(f"mixg{half}", [128, 512], F32)
                        S.tt('dve', tmp[:], pm_[:, :], g12[:, half * 512:(half + 1) * 512], ALU.mult, [pm_, g12], [tmp])
                        S.tt('pool', x1[:, hs], tmp[:], xst[:, hs], ALU.add, [tmp, xst], [x1])
                    S.dma('sp', x1_d.ap[tt * 128:(tt + 1) * 128, :], x1[:], reads=[x1], writes=[x1_d], sembuf=x1)
                    junk = G("bjunk", [128, D], F32)
                    ssq = G(f"bssq{tt % 2}", [128, 4], F32)
                    S.op('pool', lambda e, ssq=ssq: e.memset(ssq[:], 0.0), [], [ssq])
                    S.act(junk[:], x1[:], AF.Square, [x1], [junk, ssq], accum_out=ssq[:, 0:1])
                    S.rsqrt((ssq[:, 2:3], ssq), ssq[:, 0:1], 1.0 / D, [ssq], (ssq[:, 1:2], ssq))
                    xn = G("bxn", [128, D], F32)
                    S.ts('dve', xn[:], x1[:], ssq[:, 2:3], ALU.mult, [x1, ssq], [xn])
                    h2f = G("h2f", [128, 8, 128], F32)
                    for c in range(8):
                        ptp = S.P()
                        S.tr(ptp[:, 0:128], xn[:, c * 128:(c + 1) * 128], C['ident'], [xn, cb], [ptp])
                        S.act(h2f[:, c, :], ptp[:, 0:128], AF.Identity, [ptp, a2, mod], [h2f], bias=mod[:, 24 + c:25 + c], scale=a2[:, c:c + 1])
                    S.cp('pool', h2T[:, :, sl], h2f[:], [h2f], [h2T])
                    plg = S.P()
                    for c in range(8):
                        S.mm(plg[:, 0:36], h2f[:, c, :], Wr[:, c, :], [h2f, Wr], [plg], start=(c == 0), stop=(c == 7))
                    r = G("rt", [128, 256], F32)
                    S.tt('dve', r[:, 0:36], plg[:, 0:36], brt[:], ALU.add, [plg, brt], [r])
                    S.op('dve', lambda e, r=r: e.reduce_max(out=r[:, 36:37], in_=r[:, 0:4], axis=AX.X), [r], [r])
                    S.ts('dve', r[:, 40:44], r[:, 0:4], r[:, 36:37], ALU.is_equal, [r], [r])
                    S.ts('dve', r[:, 44:48], r[:, 0:4], r[:, 36:37], ALU.subtract, [r], [r])
                    S.act(r[:, 44:48], r[:, 44:48], AF.Exp, [r], [r])
                    S.op('dve', lambda e, r=r: e.reduce_sum(out=r[:, 48:49], in_=r[:, 44:48], axis=AX.X), [r], [r])
                    S.op('dve', lambda e, r=r: e.reciprocal(out=r[:, 49:50], in_=r[:, 48:49]), [r], [r])
                    S.ts('dve', r[:, 44:48], r[:, 40:44], 1.0, ALU.subtract, [r], [r], s2=1e30, op1=ALU.mult)
                    S.tt('dve', r[:, 64:96].rearrange("p (g e) -> p g e", e=8), r[:, 4:36].rearrange("p (g e) -> p g e", e=8),
                         r[:, 44:48].unsqueeze(2).to_broadcast([128, 4, 8]), ALU.add, [r], [r])
                    S.op('dve', lambda e, r=r: e.reduce_max(out=r[:, 96:97], in_=r[:, 64:96], axis=AX.X), [r], [r])
                    S.ts('dve', r[:, 100:132], r[:, 64:96], r[:, 96:97], ALU.is_equal, [r], [r])
                    S.stt(r[:, 132:164], r[:, 100:132], -1e30, r[:, 64:96], ALU.mult, ALU.add, [r], [r])
                    S.op('dve', lambda e, r=r: e.reduce_max(out=r[:, 164:165], in_=r[:, 132:164], axis=AX.X), [r], [r])
                    S.ts('dve', r[:, 168:200], r[:, 132:164], r[:, 164:165], ALU.is_equal, [r], [r])
                    S.tt('dve', r[:, 200:201], r[:, 164:165], r[:, 96:97], ALU.subtract, [r], [r])
                    S.act(r[:, 200:201], r[:, 200:201], AF.Exp, [r], [r])
                    S.ts('dve', r[:, 201:202], r[:, 200:201], 1.0, ALU.add, [r], [r])
                    S.op('dve', lambda e, r=r: e.reciprocal(out=r[:, 202:203], in_=r[:, 201:202]), [r], [r])
                    S.tt('dve', r[:, 202:203], r[:, 202:203], r[:, 49:50], ALU.mult, [r], [r])
                    S.tt('dve', r[:, 203:204], r[:, 202:203], r[:, 200:201], ALU.mult, [r], [r])
                    S.ts('dve', r[:, 204:236], r[:, 100:132], r[:, 202:203], ALU.mult, [r], [r])
                    S.stt(Gt[:, tt, :], r[:, 168:200], r[:, 203:204], r[:, 204:236], ALU.mult, ALU.add, [r], [Gt])
                S.barrier()
            S.cache = {}
            if os.environ.get('KSTOP') == 'b1':
                return nc
            acc = S.sb("acc", [128, NTB, D], F32, esB)
            S.op('pool', lambda e: e.memset(acc[:], 0.0), [], [acc])
            with ExitStack() as esB2:
                S.cache_es = esB2
                G = S.g
                NB = max(1, TS // 512)
                BW = min(512, TS)
                for ex_ in range(NE):
                    w1t = G(f"w1t{ex_ % 2}", [128, 8, DEXP], BF16)
                    w3t = G(f"w3t{ex_ % 2}", [128, 8, DEXP], BF16)
                    w2t = G(f"w2t{ex_ % 2}", [128, 4, D], BF16)
                    S.dma('pool', w1t[:], w1[ex_].rearrange("(c p) n -> p c n", p=128), writes=[w1t], sembuf=w1t)
                    S.dma('pool', w3t[:], w3[ex_].rearrange("(c p) n -> p c n", p=128), writes=[w3t], sembuf=w3t)
                    S.dma('pool', w2t[:], w2[ex_].rearrange("(c p) n -> p c n", p=128), writes=[w2t], sembuf=w2t)
                    for tb in range(NB):
                        bsl = slice(tb * BW, (tb + 1) * BW)
                        hid = G(f"hid{tb % 2}", [128, 4, BW], BF16)
                        for hc in range(4):
                            p1 = S.P()
                            for c in range(8):
                                S.mm(p1[:, 0:BW], w1t[:, c, hc * 128:(hc + 1) * 128], h2T[:, c, bsl], [w1t, h2T], [p1], start=(c == 0), stop=(c == 7))
                            p3 = S.P()
                            for c in range(8):
                                S.mm(p3[:, 0:BW], w3t[:, c, hc * 128:(hc + 1) * 128], h2T[:, c, bsl], [w3t, h2T], [p3], start=(c == 0), stop=(c == 7))
                            sl_ = G(f"silu{hc % 2}", [128, BW], F32)
                            S.act(sl_[:], p1[:, 0:BW], AF.Silu, [p1], [sl_])
                            S.tt('dve', hid[:, hc, :], sl_[:], p3[:, 0:BW], ALU.mult, [sl_, p3], [hid])
                        for t4 in range(BW // 128):
                            tt = tb * (BW // 128) + t4
                            for half in range(2):
                                py = S.P()
                                for hc in range(4):
                                    S.mm(py[:, :], hid[:, hc, t4 * 128:(t4 + 1) * 128], w2t[:, hc, half * 512:(half + 1) * 512], [hid, w2t], [py],
                                         start=(hc == 0), stop=(hc == 3))
                                S.stt(acc[:, tt, half * 512:(half + 1) * 512], py[:, :], Gt[:, tt, ex_:ex_ + 1], acc[:, tt, half * 512:(half + 1) * 512],
                                      ALU.mult, ALU.add, [py, Gt, acc], [acc])
                S.barrier()
            S.cache = {}
            if os.environ.get('KSTOP') == 'b2':
                return nc
            with ExitStack() as esB3:
                S.cache_es = esB3
                G = S.g
                for tt in range(NTB):
                    x1 = G(f"fx1_{tt % 2}", [128, D], F32)
                    S.dma('sp', x1[:], x1_d.ap[tt * 128:(tt + 1) * 128, :], reads=[x1_d], writes=[x1], sembuf=x1)
                    x2 = G(f"fx2_{tt % 2}", [128, D], F32)
                    S.tt('pool', x2[:], acc[:, tt, :], g12[:, D:2 * D], ALU.mult, [acc, g12], [x2])
                    S.tt('dve', x2[:], x2[:], x1[:], ALU.add, [x2, x1], [x2])
                    junk = G("fjunk", [128, D], F32)
                    ssq = G(f"fssq{tt % 2}", [128, 4], F32)
                    S.op('pool', lambda e, ssq=ssq: e.memset(ssq[:], 0.0), [], [ssq])
                    S.act(junk[:], x2[:], AF.Square, [x2], [junk, ssq], accum_out=ssq[:, 0:1])
                    S.rsqrt((ssq[:, 2:3], ssq), ssq[:, 0:1], 1.0 / D, [ssq], (ssq[:, 1:2], ssq))
                    ot = G(f"fot{tt % 2}", [128, D], F32)
                    S.stt(ot[:], x2[:], ssq[:, 2:3], fnwt[:], ALU.mult, ALU.mult, [x2, ssq, fnwt], [ot])
                    S.dma('sp', out[tt * 128:(tt + 1) * 128, :], ot[:], reads=[ot], sembuf=ot)
                S.barrier()
        print("ninst", S.ninst, "nsem", S.nsem, flush=True)
    return nc


def _host_inputs(inp, T, TS):
    NH = NHEAD
    NE_ = int(os.environ.get('KNEXP', NEXP))
    f = lambda a: np.ascontiguousarray(np.asarray(a), dtype=np.float32)
    x = f(inp['x'])
    B = x.shape[0]
    w_in = f(inp['w_in'])[0]
    swap = np.concatenate([np.arange(64, 128), np.arange(0, 64)])
    cols = []
    for h in range(NH):
        q = np.arange(h * 128, (h + 1) * 128)
        k = 512 + q
        v = 1024 + q
        g = 1536 + q
        cols += [q, q[swap], k, k[swap], v, g]
    for h in range(NH):
        q = 2048 + np.arange(h * 128, (h + 1) * 128)
        cols += [q, q + 512, q + 1024, q + 1536]
    cols = np.concatenate(cols)
    w_inA = np.ascontiguousarray(w_in[:, cols])
    w_ab = np.ascontiguousarray(w_in[:, 4096:4104])
    conv = f(inp['conv_w'])[0]
    convw = np.zeros((128, NH * 12), np.float32)
    for h in range(NH):
        for w_ in range(3):
            ch = w_ * 512 + h * 128 + np.arange(128)
            convw[:, h * 12 + w_ * 4:h * 12 + w_ * 4 + 4] = conv[:, ch].T
    hv = np.broadcast_to(np.concatenate([f(inp['a_log'])[0], f(inp['dt_bias'])[0]])[None, :], (128, 2 * NH)).copy()
    dnw = f(inp['dn_norm_w'])[0].reshape(128, 1).copy()
    w_out = f(inp['w_out'])[0]
    rows = []
    for h in range(NH):
        rows += [np.arange(h * 128, (h + 1) * 128), 512 + np.arange(h * 128, (h + 1) * 128)]
    w_outP = np.ascontiguousarray(w_out[np.concatenate(rows), :])
    w_r = np.ascontiguousarray(np.concatenate([f(inp['w_group'])[0], f(inp['w_expert'])[0]], axis=1))
    b_r = np.broadcast_to(np.concatenate([f(inp['b_group'])[0], f(inp['b_expert'])[0]])[None, :], (128, 36)).copy()
    n12 = np.concatenate([f(inp['norm1_w'])[0].reshape(8, 128).T, f(inp['norm2_w'])[0].reshape(8, 128).T], axis=1).copy()
    b_adaT = np.ascontiguousarray(f(inp['b_ada'])[0].reshape(48, 128).T)
    fnw = np.broadcast_to(f(inp['final_norm_w'])[None, :], (128, D)).copy()
    shared = dict(w_ada=f(inp['w_ada'])[0], b_adaT=b_adaT, n12=n12, w_inA=w_inA, w_ab=w_ab, convw=convw, hv=hv, dnw=dnw,
                  w_out=w_outP, w_r=w_r, b_r=b_r, w1=f(inp['w1'])[0][:NE_], w3=f(inp['w3'])[0][:NE_], w2=f(inp['w2'])[0][:NE_], fnw=fnw,
                  cbig=CBIG, ccol=CCOL)
    pos = np.asarray(inp['positions']).astype(np.int32)
    c = f(inp['c'])
    maps = []
    nsl = T // TS
    for core in range(B * nsl):
        b, s = core // nsl, core % nsl
        m = dict(shared)
        m['xT'] = np.ascontiguousarray(x[b].T)
        m['xs'] = np.ascontiguousarray(x[b, s * TS:(s + 1) * TS, :])
        m['pos'] = np.ascontiguousarray(pos[b][None, :])
        m['cT'] = np.ascontiguousarray(c[b].reshape(8, 128).T)
        sm = np.zeros((128, 8), np.float32)
        sm[:, s] = 1.0
        m['selm'] = sm
        maps.append(m)
    return maps


_NC_CACHE = {}


def _run(inp, T, TS):
    key = (T, TS)
    if key not in _NC_CACHE:
        _NC_CACHE[key] = build(T, TS, SBW=min(256, TS))
    nc = _NC_CACHE[key]
    maps = _host_inputs(inp, T, TS)
    res = run_bass_kernel_spmd(nc, maps, core_ids=list(range(len(maps))))
    B = np.asarray(inp['x']).shape[0]
    nsl = T // TS
    out = np.zeros((B, T, D), np.float32)
    for core in range(B * nsl):
        b, s = core // nsl, core % nsl
        out[b, s * TS:(s + 1) * TS, :] = np.asarray(res.results[core]['out'])
    return out


def kernel(**inputs):
    return _run(inputs, 8192, 2048)
```

```python
import math
import os
from contextlib import ExitStack
import numpy as np
import concourse.bass as bass
import concourse.mybir as mybir
from concourse.bass_utils import run_bass_kernel_spmd

F32 = mybir.dt.float32
BF16 = mybir.dt.bfloat16
I32 = mybir.dt.int32
AF = mybir.ActivationFunctionType
ALU = mybir.AluOpType
AX = mybir.AxisListType

D = 1024
NHEAD = 4
EPS = 1e-6
NEXP = 32
DEXP = 512
TWO_PI = 2.0 * math.pi


class Buf:
    def __init__(self, ap, name):
        self.ap = ap
        self.name = name
        self.ws = {}
        self.rs = {}
        self.dsem = None
        self.dcnt = 0
        self.psum = False

    def __getitem__(self, k):
        return self.ap[k]


class Sched:
    EPOCH = 30000

    def __init__(self, nc, es):
        self.nc = nc
        self.es = es
        self.eng = {'pe': nc.tensor, 'act': nc.scalar, 'dve': nc.vector, 'pool': nc.gpsimd, 'sp': nc.sync}
        self.sem = {}
        self.cnt = {}
        self.waited = {e: {} for e in self.eng}
        self.nsem = 0
        self.allsems = {}
        for e in self.eng:
            self._newsem(e)
        self.bufs = []
        self.ninst = {e: 0 for e in self.eng}
        self.cache = {}
        self.pbanks = []
        self.pi = 0

    def _mksem(self, name):
        self.nsem += 1
        s = self.es.enter_context(self.nc.semaphore(name))
        return s

    def _newsem(self, e):
        self.sem[e] = self._mksem(f"c_{e}_{self.nsem}")
        self.cnt[e] = 0

    def sb(self, name, shape, dt, es=None):
        self.uid = getattr(self, 'uid', 0) + 1
        name = f"s{self.uid}_{name}"
        t = (es or self.es).enter_context(self.nc.sbuf_tensor(name, list(shape), dt))
        b = Buf(t, name)
        self.bufs.append(b)
        return b

    def g(self, name, shape, dt):
        if name not in self.cache:
            self.cache[name] = self.sb(name, shape, dt, es=self.cache_es)
        return self.cache[name]

    def mkpsum(self):
        for i in range(8):
            t = self.es.enter_context(self.nc.psum_tensor(f"pb{i}", [128, 512], F32))
            b = Buf(t, f"pb{i}")
            b.psum = True
            self.bufs.append(b)
            self.pbanks.append(b)

    def P(self):
        b = self.pbanks[self.pi % 8]
        self.pi += 1
        return b

    def dram(self, name, shape, dt, kind="Internal"):
        t = self.nc.dram_tensor(name, list(shape), dt, kind=kind).ap()
        b = Buf(t, name)
        self.bufs.append(b)
        return b

    def _deps(self, reads, writes, e=None):
        deps = {}

        def add(d):
            for k, (s, v) in d.items():
                if k not in deps or deps[k][1] < v:
                    deps[k] = (s, v)
        for b in reads:
            add(b.ws)
            if b.psum:
                own = id(self.sem[e]) if e in self.sem else None
                add({k: v for k, v in b.rs.items() if k != own})
        for b in writes:
            add(b.ws)
            add(b.rs)
        return deps

    def _wait(self, e, deps):
        for k, (s, v) in deps.items():
            if e == 'pe' and s is self.sem['pe']:
                continue
            if self.waited[e].get(k, 0) >= v:
                continue
            self.eng[e].wait_ge(s, v)
            self.ninst[e] += 1
            self.waited[e][k] = v

    def _record(self, ev, reads, writes):
        k = id(ev[0])
        for b in reads:
            if k not in b.rs or b.rs[k][1] < ev[1]:
                b.rs[k] = ev
        for b in writes:
            if k not in b.ws or b.ws[k][1] < ev[1]:
                b.ws[k] = ev

    def op(self, e, fn, reads=(), writes=()):
        if e == 'pool' and os.environ.get('KNOPOOL'):
            e = 'dve'
        self._wait(e, self._deps(reads, writes, e))
        ins = fn(self.eng[e])
        if self.cnt[e] >= self.EPOCH:
            self._newsem(e)
        self.cnt[e] += 1
        ins.then_inc(self.sem[e], 1)
        self.ninst[e] += 1
        self._record((self.sem[e], self.cnt[e]), reads, writes)
        return ins

    def dma(self, q, out_ap, in_ap, reads=(), writes=(), sembuf=None, **kw):
        self._wait(q, self._deps(reads, writes))
        if sembuf.dsem is None:
            sembuf.dsem = self._mksem(f"d_{sembuf.name}")
        ins = self.eng[q].dma_start(out=out_ap, in_=in_ap, **kw)
        sembuf.dcnt += 1
        ins.then_inc(sembuf.dsem, 16)
        self.ninst[q] += 1
        self._record((sembuf.dsem, 16 * sembuf.dcnt), reads, writes)
        return ins

    def barrier(self, engines=('pe', 'act', 'dve', 'pool', 'sp')):
        deps = {}
        for b in self.bufs:
            for d in (b.ws, b.rs):
                for k, (s, v) in d.items():
                    if k not in deps or deps[k][1] < v:
                        deps[k] = (s, v)
        for e in engines:
            self._wait(e, deps)

    def act(self, out, in_, func, reads, writes, **kw):
        return self.op('act', lambda e: e.activation(out=out, in_=in_, func=func, **kw), reads, writes)

    def tt(self, eng, out, a, b, op, reads, writes):
        return self.op(eng, lambda e: e.tensor_tensor(out=out, in0=a, in1=b, op=op), reads, writes)

    def ts(self, eng, out, a, s1, op0, reads, writes, s2=None, op1=None):
        if op1 is None:
            return self.op(eng, lambda e: e.tensor_scalar(out=out, in0=a, scalar1=s1, scalar2=None, op0=op0), reads, writes)
        return self.op(eng, lambda e: e.tensor_scalar(out=out, in0=a, scalar1=s1, scalar2=s2, op0=op0, op1=op1), reads, writes)

    def stt(self, out, a, sc, b, op0, op1, reads, writes):
        return self.op('dve', lambda e: e.scalar_tensor_tensor(out=out, in0=a, scalar=sc, in1=b, op0=op0, op1=op1), reads, writes)

    def cp(self, eng, out, in_, reads, writes):
        if eng == 'act':
            return self.act(out, in_, AF.Copy, reads, writes)
        return self.op(eng, lambda e: e.tensor_copy(out=out, in_=in_), reads, writes)

    def mm(self, out, lhsT, rhs, reads, writes, start=True, stop=True):
        return self.op('pe', lambda e: e.matmul(out, lhsT=lhsT, rhs=rhs, start=start, stop=stop), reads, writes)

    def tr(self, out, in_, ident, reads, writes):
        return self.op('pe', lambda e: e.transpose(out=out, in_=in_, identity=ident), reads, writes)

    def rsqrt(self, out, in_, scale, reads_in, tmp, eps=EPS):
        self.act(tmp[0], in_, AF.Ln, list(reads_in) + [self.epsbuf], [tmp[1]], bias=self.epsc[:in_.shape[0], 0:1], scale=scale)
        self.act(out[0], tmp[0], AF.Exp, [tmp[1]], [out[1]], scale=-0.5)


def rr(gens):
    gens = list(gens)
    while gens:
        nxt = []
        for g_ in gens:
            try:
                next(g_)
                nxt.append(g_)
            except StopIteration:
                pass
        gens = nxt


def _consts():
    i = np.arange(128)
    c = {}
    c['ident'] = np.eye(128, dtype=np.float32)
    c['triU'] = (i[:, None] <= i[None, :]).astype(np.float32)
    c['negtriU'] = -c['triU']
    c['negmask'] = np.where(i[None, :] <= i[:, None], 0.0, -1e30).astype(np.float32)
    c['m2L'] = ((i[:, None] == i[None, :] + 1) & (i[:, None] % 2 == 1)).astype(np.float32)
    for s in (4, 8, 16, 32, 64):
        c[f'bm{s}'] = ((i[:, None] // s) == (i[None, :] // s)).astype(np.float32)
    gam = 1.0 - np.power(2.0, -5.0 - np.arange(NHEAD))
    lg = np.log1p(-np.power(2.0, -5.0 - np.arange(NHEAD, dtype=np.float64)))
    rel = i[None, :] - i[:, None]
    for h in range(NHEAD):
        c[f'decT{h}'] = (np.where(rel >= 0, np.exp(lg[h] * np.maximum(rel, 0)), 0.0) * 128 ** -0.5).astype(np.float32)
        c[f'qdec{h}'] = np.broadcast_to(np.exp(lg[h] * (i + 1.0))[None, :], (128, 128)).astype(np.float32).copy()
    kws = np.stack([np.exp(lg[h] * (127 - i)) * 128 ** -0.5 for h in range(NHEAD)], axis=1)
    cd = [float(np.exp(lg[h] * 128)) for h in range(NHEAD)]
    invf = (10000.0 ** (-(np.arange(0, 128, 2, dtype=np.float32)) / 128.0)).astype(np.float32)
    col = np.zeros((128, 8), np.float32)
    col[:, 0] = np.concatenate([invf, invf])
    col[:, 1] = np.concatenate([-np.ones(64), np.ones(64)])
    col[:, 2] = EPS
    col[:, 3] = math.pi / 2
    col[:, 4:8] = kws
    names = ['ident', 'triU', 'negtriU', 'negmask', 'm2L', 'bm4', 'bm8', 'bm16', 'bm32', 'bm64'] + \
            [f'decT{h}' for h in range(NHEAD)] + [f'qdec{h}' for h in range(NHEAD)]
    big = np.concatenate([c[n] for n in names], axis=1).astype(np.float32)
    return names, big, col, cd


CNAMES, CBIG, CCOL, CD = _consts()


def build(T, TS, SBW=256, dbg=False):
    NH = NHEAD
    NSB = T // SBW
    NT = SBW // 128
    NCOL = NH * 10 * 128
    NE = int(os.environ.get('KNEXP', NEXP))
    nc = bass.Bass("TRN2", target_bir_lowering=False)
    es = ExitStack()
    with es:
        S = Sched(nc, es)
        S.mkpsum()
        dt_in = {}

        def din(name, shape, dt=F32):
            dt_in[name] = nc.dram_tensor(name, list(shape), dt, kind="ExternalInput").ap()
            return dt_in[name]
        xT = din("xT", [D, T])
        xs = din("xs", [TS, D])
        pos = din("pos", [1, T], I32)
        cT = din("cT", [128, 8])
        w_ada = din("w_ada", [D, 6 * D])
        b_adaT = din("b_adaT", [128, 48])
        n12 = din("n12", [128, 16])
        w_inA = din("w_inA", [D, NCOL])
        w_ab = din("w_ab", [D, 2 * NH])
        convw = din("convw", [128, NH * 12])
        hv = din("hv", [128, 2 * NH])
        dnw = din("dnw", [128, 1])
        w_out = din("w_out", [D, D])
        w_r = din("w_r", [D, 36])
        b_r = din("b_r", [128, 36])
        w1 = din("w1", [NE, D, DEXP])
        w3 = din("w3", [NE, D, DEXP])
        w2 = din("w2", [NE, DEXP, D])
        fnw = din("fnw", [128, D])
        cbig = din("cbig", [128, CBIG.shape[1]])
        ccol = din("ccol", [128, 8])
        selm_in = din("selm", [128, 8])
        vmask_in = din("vmask", [128, NSB])
        out = nc.dram_tensor("out", [TS, D], F32, kind="ExternalOutput").ap()
        x1_d = S.dram("x1_d", [TS, D], F32)
        dbg_outs = {}

        cb = S.sb("cbig", [128, CBIG.shape[1]], F32)
        S.dma('sp', cb[:], cbig[:, :], writes=[cb], sembuf=cb)
        cc = S.sb("ccol", [128, 8], F32)
        S.dma('sp', cc[:], ccol[:, :], writes=[cc], sembuf=cc)
        S.epsc = cc.ap[:, 2:3]
        S.epsbuf = cc
        C = {n: cb.ap[:, k * 128:(k + 1) * 128] for k, n in enumerate(CNAMES)}
        ident_b = S.sb("ident_b", [128, 128], BF16)
        S.cp('dve', ident_b[:], C['ident'], [cb], [ident_b])
        ones_b = S.sb("ones_b", [128, 128], BF16)
        S.op('pool', lambda e: e.memset(ones_b[:], 1.0), [], [ones_b])
        ones_f = S.sb("ones_f", [128, 128], F32)
        S.op('pool', lambda e: e.memset(ones_f[:], 1.0), [], [ones_f])
        bmb = {}
        for s_ in (4, 8, 16, 32, 64):
            bmb[s_] = S.sb(f"bmb{s_}", [128, 128], BF16)
            S.cp('dve', bmb[s_][:], C[f'bm{s_}'], [cb], [bmb[s_]])
        m2Lb = S.sb("m2Lb", [128, 128], BF16)
        S.cp('dve', m2Lb[:], C['m2L'], [cb], [m2Lb])
        selm = S.sb("selm", [128, 8], F32)
        S.dma('sp', selm[:], selm_in[:, :], writes=[selm], sembuf=selm)
        vm = S.sb("vmask", [128, NSB], F32)
        S.dma('sp', vm[:], vmask_in[:, :], writes=[vm], sembuf=vm)
        NFULL0 = NSB - TS // SBW
        catT = S.sb("catT", [128, 8, TS], BF16)
        S.op('pool', lambda e: e.memset(catT[:], 0.0), [], [catT])
        mod = S.sb("mod", [128, 48], F32)
        a1 = S.sb("a1", [128, 8], F32)
        a2 = S.sb("a2", [128, 8], F32)

        with ExitStack() as es0:
            S.cache_es = es0
            ct = S.sb("ct", [128, 8], F32, es0)
            S.dma('sp', ct[:], cT[:, :], writes=[ct], sembuf=ct)
            sct = S.sb("sct", [128, 8], F32, es0)
            S.act(sct[:], ct[:], AF.Silu, [ct], [sct])
            bad = S.sb("bad", [128, 48], F32, es0)
            S.dma('sp', bad[:], b_adaT[:, :], writes=[bad], sembuf=bad)
            n12t = S.sb("n12t", [128, 16], F32, es0)
            S.dma('sp', n12t[:], n12[:, :], writes=[n12t], sembuf=n12t)
            pm = S.P()
            for j in range(6):
                wa = S.g(f"wada{j % 2}", [128, 8, D], F32)
                S.dma('sp', wa[:], w_ada[:, j * D:(j + 1) * D].rearrange("(c p) n -> p c n", p=128), writes=[wa], sembuf=wa)
                for oc in range(8):
                    col_ = j * 8 + oc
                    for c in range(8):
                        S.mm(pm[:, col_:col_ + 1], wa[:, c, oc * 128:(oc + 1) * 128], sct[:, c:c + 1], [wa, sct], [pm],
                             start=(c == 0), stop=(c == 7))
            S.tt('dve', mod[:], pm[:, 0:48], bad[:], ALU.add, [pm, bad], [mod])
            S.stt(a1[:], mod[:, 8:16], 1.0, n12t[:, 0:8], ALU.add, ALU.mult, [mod, n12t], [a1])
            S.stt(a2[:], mod[:, 32:40], 1.0, n12t[:, 8:16], ALU.add, ALU.mult, [mod, n12t], [a2])
            S.barrier()
        S.cache = {}
        if os.environ.get('KSTOP') == 'pre':
            return nc

        for phase in ('ret', 'gdn'):
            with ExitStack() as esA:
                S.cache_es = esA
                G = S.g
                if phase == 'ret':
                    PC0, PCN = 0, NH * 6 * 128
                else:
                    PC0, PCN = NH * 6 * 128, NH * 4 * 128
                Win = S.sb("Win" + phase, [128, 8, PCN], BF16, esA)
                for c in range(8):
                    S.dma('pool', Win[:, c, :], w_inA[c * 128:(c + 1) * 128, PC0:PC0 + PCN], writes=[Win], sembuf=Win)
                Wab = S.sb("Wab" + phase, [128, 8, 2 * NH], BF16, esA)
                S.dma('pool', Wab[:], w_ab.rearrange("(c p) n -> p c n", p=128), writes=[Wab], sembuf=Wab)
                cw = S.sb("cw" + phase, [128, NH * 12], F32, esA)
                S.dma('sp', cw[:], convw[:, :], writes=[cw], sembuf=cw)
                hvt = S.sb("hvt" + phase, [128, 2 * NH], F32, esA)
                S.dma('sp', hvt[:], hv[:, :], writes=[hvt], sembuf=hvt)
                dnwt = S.sb("dnwt" + phase, [128, 1], F32, esA)
                S.dma('sp', dnwt[:], dnw[:, :], writes=[dnwt], sembuf=dnwt)
                nea = S.sb("nea" + phase, [128, NH], F32, esA)
                S.act(nea[:], hvt[:, 0:NH], AF.Exp, [hvt], [nea])
                S.ts('dve', nea[:], nea[:], -1.0, ALU.mult, [nea], [nea])
                Sr, Srb, Sg, Sgb, cbuf = [], [], [], [], []
                for h in range(NH):
                    for lst, nm, dt in ((Sr, "Sr", F32), (Srb, "Srb", BF16), (Sg, "Sg", F32), (Sgb, "Sgb", BF16)):
                        b = S.sb(f"{nm}{h}{phase}", [128, 128], dt, esA)
                        S.op('pool', lambda e, b=b: e.memset(b[:], 0.0), [], [b])
                        lst.append(b)
                    row = []
                    for w_ in range(3):
                        b = S.sb(f"cbuf{h}_{w_}{phase}", [128, SBW + 3], F32, esA)
                        S.op('pool', lambda e, b=b: e.memset(b[:], 0.0), [], [b])
                        row.append(b)
                    cbuf.append(row)

                def proj(hT, col):
                    ps = S.P()
                    for c in range(8):
                        S.mm(ps[:, 0:SBW], Win[:, c, col - PC0:col - PC0 + 128], hT[:, c, :], [Win, hT], [ps], start=(c == 0), stop=(c == 7))
                    return ps

                def emitA(sb_):
                    t0 = sb_ * SBW
                    xt = G("xt", [128, 8, SBW], F32)
                    S.dma('sp', xt[:], xT[:, t0:t0 + SBW].rearrange("(c p) t -> p c t", p=128), writes=[xt], sembuf=xt)
                    xsq = G("xsq", [128, 8, SBW], BF16)
                    S.act(xsq[:], xt[:], AF.Square, [xt], [xsq])
                    pss = S.P()
                    for c in range(8):
                        S.mm(pss[:, 0:SBW], ones_b[:], xsq[:, c, :], [ones_b, xsq], [pss], start=(c == 0), stop=(c == 7))
                    lnt = G("lnt", [128, SBW], F32)
                    rstd = G("rstd", [128, SBW], F32)
                    S.rsqrt((rstd[:], rstd), pss[:, 0:SBW], 1.0 / D, [pss], (lnt[:], lnt))
                    hT = G("hT", [128, 8, SBW], BF16)
                    for c in range(8):
                        tmp = G(f"xn{c % 2}", [128, SBW], F32)
                        S.tt('dve' if c % 2 == 0 else 'pool', tmp[:], xt[:, c, :], rstd[:], ALU.mult, [xt, rstd], [tmp])
                        S.act(hT[:, c, :], tmp[:], AF.Identity, [tmp, a1, mod], [hT], bias=mod[:, c:c + 1], scale=a1[:, c:c + 1])
                    return hT

                for sb in range(NSB):
                    t0 = sb * SBW
                    if phase == 'ret' or sb == 0:
                        hT = emitA(sb)
                    if os.environ.get('KSTOP') == 'ret1':
                        S.barrier(engines=('sp',))
                        return nc
                    if phase == 'ret':
                        posi = G("posi", [128, SBW], I32)
                        S.dma('sp', posi[:], pos[0:1, t0:t0 + SBW].partition_broadcast(128), writes=[posi], sembuf=posi)
                        posf = G("posf", [128, SBW], F32)
                        S.cp('dve', posf[:], posi[:], [posi], [posf])
                        ang = G("ang", [128, SBW], F32)
                        S.ts('dve', ang[:], posf[:], cc[:, 0:1], ALU.mult, [posf, cc], [ang])
                        tabs = []
                        for which in range(2):
                            if which == 1:
                                ang2 = G("ang2", [128, SBW], F32)
                                S.ts('pool', ang2[:], ang[:], math.pi / 2, ALU.add, [ang], [ang2])
                                a_ = ang2
                            else:
                                a_ = ang
                            ki = G(f"ki{which}", [128, SBW], I32)
                            S.ts('dve', ki[:], a_[:], 1.0 / TWO_PI, ALU.mult, [a_], [ki])
                            kf = G(f"kf{which}", [128, SBW], F32)
                            S.cp('pool', kf[:], ki[:], [ki], [kf])
                            rr_ = G(f"rr{which}", [128, SBW], F32)
                            S.stt(rr_[:], kf[:], -TWO_PI, a_[:], ALU.mult, ALU.add, [kf, a_], [rr_])
                            S.ts('pool', rr_[:], rr_[:], math.pi, ALU.min, [rr_], [rr_], s2=-math.pi, op1=ALU.max)
                            tb = G(f"tab{which}", [128, SBW], F32)
                            S.act(tb[:], rr_[:], AF.Sin, [rr_], [tb])
                            tabs.append(tb)
                        sint, cost = tabs
                        sins = G("sins", [128, SBW], F32)
                        S.act(sins[:], sint[:], AF.Identity, [sint, cc], [sins], scale=cc[:, 1:2])

                        def ret_prep(h, full):
                            base = h * 6 * 128
                            for nm, off in ((("q", 0), ("k", 2)) if full else (("k", 2),)):
                                t1 = G(f"rt1{nm}{h}", [128, SBW], F32)
                                t2 = G(f"rt2{nm}{h}", [128, SBW], F32)
                                p1 = proj(hT, base + off * 128)
                                S.tt('dve', t1[:], p1[:, 0:SBW], cost[:], ALU.mult, [p1, cost], [t1])
                                yield
                                p2 = proj(hT, base + (off + 1) * 128)
                                S.tt('dve', t2[:], p2[:, 0:SBW], sins[:], ALU.mult, [p2, sins], [t2])
                                yield
                                o_ = G(f"r{nm}T{h}", [128, SBW], BF16)
                                S.tt('pool', o_[:], t1[:], t2[:], ALU.add, [t1, t2], [o_])
                                yield
                            pv = proj(hT, base + 4 * 128)
                            vT = G(f"rvT{h}", [128, SBW], BF16)
                            S.cp('act', vT[:], pv[:, 0:SBW], [pv], [vT])
                            yield
                            if full:
                                pg_ = proj(hT, base + 5 * 128)
                                sgT = G(f"rsgT{h}", [128, SBW], F32)
                                S.act(sgT[:], pg_[:, 0:SBW], AF.Silu, [pg_], [sgT])
                                yield

                        def ret_chain(tt, h, sb_, full):
                            sl = slice(tt * 128, (tt + 1) * 128)
                            krT, vT = S.cache[f"rkT{h}"], S.cache[f"rvT{h}"]
                            if full:
                                qrT, sgT = S.cache[f"rqT{h}"], S.cache[f"rsgT{h}"]
                            pk = S.P()
                            pkb = pk.ap[:].bitcast(BF16)
                            S.tr(pkb[:, 0:128], krT[:, sl], ident_b[:], [krT, ident_b], [pk])
                            S.tr(pkb[:, 128:256], vT[:, sl], ident_b[:], [vT, ident_b], [pk])
                            kw = G(f"rkw{h}", [128, 128], BF16)
                            S.ts('dve', kw[:], pkb[:, 0:128], cc[:, 4 + h:5 + h], ALU.mult, [pk, cc], [kw])
                            vtok = G(f"rvtok{h}", [128, 128], BF16)
                            S.act(vtok[:], pkb[:, 128:256], AF.Identity, [pk, vm], [vtok], scale=vm[:, sb_:sb_ + 1])
                            if not full:
                                yield
                                po = S.P()
                                S.mm(po[:, 128:256], kw[:], vtok[:], [kw, vtok], [po])
                                S.stt(Sr[h][:], Sr[h][:], CD[h], po[:, 128:256], ALU.mult, ALU.add, [Sr[h], po], [Sr[h]])
                                yield
                                S.cp('act', Srb[h][:], Sr[h][:], [Sr[h]], [Srb[h]])
                                yield
                                return
                            qwT = G(f"rqw{h}", [128, 128], BF16)
                            S.tt('pool', qwT[:], qrT[:, sl], C[f'qdec{h}'], ALU.mult, [qrT, cb], [qwT])
                            yield
                            psc = S.P()
                            S.mm(psc[:, 0:128], krT[:, sl], qrT[:, sl], [krT, qrT], [psc])
                            sT = G(f"rsT{h}", [128, 128], BF16)
                            S.tt('dve', sT[:], psc[:, 0:128], C[f'decT{h}'], ALU.mult, [psc, cb], [sT])
                            yield
                            po = S.P()
                            S.mm(po[:, 0:128], sT[:], vtok[:], [sT, vtok], [po], start=True, stop=False)
                            S.mm(po[:, 0:128], qwT[:], Srb[h][:], [qwT, Srb[h]], [po], start=False, stop=True)
                            S.mm(po[:, 128:256], kw[:], vtok[:], [kw, vtok], [po])
                            S.stt(Sr[h][:], Sr[h][:], CD[h], po[:, 128:256], ALU.mult, ALU.add, [Sr[h], po], [Sr[h]])
                            osb = G(f"rosb{h}", [128, 128], F32)
                            S.cp('act', osb[:], po[:, 0:128], [po], [osb])
                            yield
                            S.cp('act', Srb[h][:], Sr[h][:], [Sr[h]], [Srb[h]])
                            junk = G(f"rjunk{h}", [128, 128], F32)
                            ssq = G(f"rssq{h}", [128, 4], F32)
                            S.op('pool', lambda e, ssq=ssq: e.memset(ssq[:], 0.0), [], [ssq])
                            yield
                            S.act(junk[:], osb[:], AF.Square, [osb], [junk, ssq], accum_out=ssq[:, 0:1])
                            yield
                            S.act(ssq[:, 1:2], ssq[:, 0:1], AF.Ln, [ssq, cc], [ssq], bias=cc[:, 2:3], scale=1.0 / 128)
                            yield
                            S.act(ssq[:, 2:3], ssq[:, 1:2], AF.Exp, [ssq], [ssq], scale=-0.5)
                            yield
                            on = G(f"ron{h}", [128, 128], F32)
                            S.ts('dve', on[:], osb[:], ssq[:, 2:3], ALU.mult, [osb, ssq], [on])
                            yield
                            pt = S.P()
                            S.tr(pt[:, 0:128], on[:], C['ident'], [on, cb], [pt])
                            loc = (sb_ - NFULL0) * SBW + tt * 128
                            S.tt('dve', catT[:, 2 * h, loc:loc + 128], pt[:, 0:128], sgT[:, sl], ALU.mult, [pt, sgT], [catT])
                            yield

                        full = sb >= NFULL0
                        rr([ret_prep(h, full) for h in range(NH)])
                        for tt in range(NT):
                            rr([ret_chain(tt, h, sb, full) for h in range(NH)])

                    if phase == 'gdn':
                        def gdn_prep(h, w_, p, sb_):
                            nm = ("q", "k", "v")[w_]
                            base = NH * 6 * 128 + h * 4 * 128
                            cbf = cbuf[h][w_]
                            ps = proj(hT, base + w_ * 128)
                            S.act(cbf[:, 3:3 + SBW], ps[:, 0:SBW], AF.Identity, [ps, vm], [cbf], scale=vm[:, sb_:sb_ + 1])
                            yield
                            acc = G(f"gacc{h}_{w_}", [128, SBW], F32)
                            wc = h * 12 + w_ * 4
                            S.act(acc[:], cbf[:, 0:SBW], AF.Identity, [cbf, cw], [acc], scale=cw[:, wc:wc + 1])
                            yield
                            for j in range(1, 4):
                                S.stt(acc[:], cbf[:, j:j + SBW], cw[:, wc + j:wc + j + 1], acc[:], ALU.mult, ALU.add, [cbf, cw, acc], [acc])
                                yield
                            tl = G(f"gtail{h}_{w_}", [128, 4], F32)
                            S.cp('pool', tl[:, 0:3], cbf[:, SBW:SBW + 3], [cbf], [tl])
                            yield
                            S.cp('pool', cbf[:, 0:3], tl[:, 0:3], [tl], [cbf])
                            yield
                            if nm == "v":
                                vT = G(f"gvT{h}_{p}", [128, SBW], BF16)
                                S.act(vT[:], acc[:], AF.Silu, [acc], [vT])
                                yield
                            else:
                                y = acc
                                S.act(y[:], acc[:], AF.Silu, [acc], [y])
                                yield
                                sq = G(f"gsq{h}_{w_}", [128, SBW], BF16)
                                S.act(sq[:], y[:], AF.Square, [y], [sq])
                                yield
                                pn = S.P()
                                S.mm(pn[:, 0:SBW], ones_b[:], sq[:], [ones_b, sq], [pn])
                                rn = G(f"grn{h}_{w_}", [128, SBW], F32)
                                S.act(rn[:], pn[:, 0:SBW], AF.Ln, [pn, cc], [rn], bias=cc[:, 2:3], scale=1.0)
                                yield
                                S.act(rn[:], rn[:], AF.Exp, [rn], [rn], scale=-0.5)
                                yield
                                o_ = G(f"g{nm}nT{h}_{p}", [128, SBW], BF16)
                                if nm == "q":
                                    S.stt(o_[:], y[:], 128 ** -0.5, rn[:], ALU.mult, ALU.mult, [y, rn], [o_])
                                else:
                                    S.tt('pool', o_[:], y[:], rn[:], ALU.mult, [y, rn], [o_])
                                yield

                        def gdn_prep_z(h, p):
                            base = NH * 6 * 128 + h * 4 * 128
                            pz = proj(hT, base + 3 * 128)
                            szT = G(f"gszT{h}_{p}", [128, SBW], F32)
                            S.act(szT[:], pz[:, 0:SBW], AF.Silu, [pz], [szT])
                            yield

                        def gdn_scal(tt, p, sb_):
                            sl = slice(tt * 128, (tt + 1) * 128)
                            sc = G(f"gsc{tt}_{p}", [128, 64], F32)
                            pab = S.P()
                            for c in range(8):
                                S.mm(pab[:, 0:2 * NH], hT[:, c, sl], Wab[:, c, :], [hT, Wab], [pab], start=(c == 0), stop=(c == 7))
                            S.tt('dve', sc[:, 0:4], pab[:, 0:NH], hvt[:, NH:2 * NH], ALU.add, [pab, hvt], [sc])
                            S.act(sc[:, 16:20], pab[:, NH:2 * NH], AF.Exp, [pab], [sc], scale=-1.0)
                            yield
                            S.act(sc[:, 4:8], sc[:, 0:4], AF.Exp, [sc], [sc])
                            yield
                            S.act(sc[:, 8:12], sc[:, 4:8], AF.Ln, [sc], [sc], bias=1.0)
                            yield
                            S.tt('dve', sc[:, 12:16], sc[:, 8:12], nea[:], ALU.mult, [sc, nea], [sc])
                            yield
                            S.ts('dve', sc[:, 16:20], sc[:, 16:20], 1.0, ALU.add, [sc], [sc])
                            yield
                            S.op('dve', lambda e, sc=sc: e.reciprocal(out=sc[:, 20:24], in_=sc[:, 16:20]), [sc], [sc])
                            yield
                            S.ts('dve', sc[:, 20:24], sc[:, 20:24], vm[:, sb_:sb_ + 1], ALU.mult, [sc, vm], [sc])
                            yield
                            pgc = S.P()
                            S.mm(pgc[:, 0:NH], C['triU'], sc[:, 12:16], [cb, sc], [pgc])
                            S.mm(pgc[:, 8:8 + NH], ones_f[:], sc[:, 12:16], [ones_f, sc], [pgc])
                            S.cp('dve', sc[:, 24:28], pgc[:, 0:NH], [pgc], [sc])
                            S.cp('dve', sc[:, 28:32], pgc[:, 8:8 + NH], [pgc], [sc])
                            yield
                            S.act(sc[:, 32:40], sc[:, 24:32], AF.Exp, [sc], [sc])
                            yield
                            S.tt('dve', sc[:, 40:44], sc[:, 28:32], sc[:, 24:28], ALU.subtract, [sc], [sc])
                            yield
                            S.act(sc[:, 44:48], sc[:, 40:44], AF.Exp, [sc], [sc])
                            yield
                            S.tt('dve', sc[:, 48:52], sc[:, 20:24], sc[:, 32:36], ALU.mult, [sc], [sc])
                            yield

                        def chain_pre(ch):
                            key, tt, h, sl, p = ch['key'], ch['tt'], ch['h'], ch['sl'], ch['p']
                            sc = S.cache[f"gsc{tt}_{p}"]
                            ch['sc'] = sc
                            full = ch['full']
                            knT, vT = S.cache[f"gknT{h}_{p}"], S.cache[f"gvT{h}_{p}"]
                            qnT = S.cache[f"gqnT{h}_{p}"] if full else None
                            F = [G(f"gF{i}_{key}", [128, 128], F32) for i in range(4)]
                            Bq = [G(f"gB{i}_{key}", [128, 128], BF16) for i in range(2)]
                            gb = F[0]
                            S.act(gb[:], ones_f[:], AF.Identity, [ones_f, sc], [gb], scale=sc[:, 12 + h:13 + h])
                            pT = S.P()
                            pTb = pT.ap[:].bitcast(BF16)
                            S.tr(pTb[:, 128:256], knT[:, sl], ident_b[:], [knT, ident_b], [pT])
                            S.tr(pTb[:, 256:384], vT[:, sl], ident_b[:], [vT, ident_b], [pT])
                            kbg = G(f"gkbg{key}", [128, 128], BF16)
                            S.act(kbg[:], pTb[:, 128:256], AF.Identity, [pT, sc], [kbg], scale=sc[:, 48 + h:49 + h])
                            kt = G(f"gkt{key}", [128, 128], BF16)
                            S.ts('dve', kt[:], pTb[:, 128:256], sc[:, 44 + h:45 + h], ALU.mult, [pT, sc], [kt])
                            vb = G(f"gvb{key}", [128, 128], BF16)
                            S.act(vb[:], pTb[:, 256:384], AF.Identity, [pT, sc], [vb], scale=sc[:, 20 + h:21 + h])
                            yield
                            pG = S.P()
                            S.mm(pG[:, 0:128], C['triU'], gb[:], [cb, gb], [pG], start=True, stop=False)
                            S.mm(pG[:, 0:128], gb[:], C['negtriU'], [cb, gb], [pG], start=False, stop=True)
                            ex = F[1]
                            S.stt(ex[:], pG[:, 0:128], 0.0, C['negmask'], ALU.min, ALU.add, [pG, cb], [ex])
                            yield
                            dec_i = F[1]
                            S.act(dec_i[:], ex[:], AF.Exp, [ex], [dec_i])
                            yield
                            dec_s = F[2]
                            S.tt('pool', dec_s[:], dec_i[:], C['ident'], ALU.subtract, [dec_i, cb], [dec_s])
                            yield
                            pK = S.P()
                            S.mm(pK[:, 0:128], knT[:, sl], knT[:, sl], [knT], [pK])
                            if full:
                                S.mm(pK[:, 128:256], qnT[:, sl], knT[:, sl], [qnT, knT], [pK])
                            A = G(f"gA{key}", [128, 128], BF16)
                            S.stt(A[:], pK[:, 0:128], sc[:, 20 + h:21 + h], dec_s[:], ALU.mult, ALU.mult, [pK, sc, dec_s], [A])
                            attn = Bq[0]
                            if full:
                                S.tt('dve', attn[:], pK[:, 128:256], dec_i[:], ALU.mult, [pK, dec_i], [attn])
                            yield
                            tm = Bq[1]
                            S.tt('pool', tm[:], A[:], m2Lb[:], ALU.mult, [A, m2Lb], [tm])
                            attnT = G(f"gattnT{key}", [128, 128], BF16)
                            if full:
                                pT2 = S.P()
                                pT2b = pT2.ap[:].bitcast(BF16)
                                S.tr(pT2b[:, 0:128], attn[:], ident_b[:], [attn, ident_b], [pT2])
                                S.cp('act', attnT[:], pT2b[:, 0:128], [pT2], [attnT])
                            yield
                            T1 = G(f"gT0{key}", [128, 128], BF16)
                            S.tt('pool', T1[:], ident_b[:], tm[:], ALU.subtract, [ident_b, tm], [T1])
                            yield
                            pU = S.P()
                            pUb = pU.ap[:].bitcast(BF16)
                            S.tr(pUb[:, 0:128], T1[:], ident_b[:], [T1, ident_b], [pU])
                            U = G(f"gU0{key}", [128, 128], BF16)
                            S.cp('act', U[:], pUb[:, 0:128], [pU], [U])
                            yield
                            Tk = T1
                            for lvl, bs in enumerate((4, 8, 16, 32, 64, None)):
                                pW = S.P()
                                S.mm(pW[:, 0:128], A[:], U[:], [A, U], [pW])
                                W = Bq[0]
                                S.tt('dve', W[:], pW[:, 0:128], U[:], ALU.add, [pW, U], [W])
                                yield
                                pW2 = S.P()
                                S.mm(pW2[:, 0:128], Tk[:], W[:], [Tk, W], [pW2])
                                Un = G(f"gU{(lvl + 1) % 2}{key}", [128, 128], BF16)
                                if bs is not None:
                                    tmpf = F[0]
                                    S.stt(tmpf[:], U[:], 2.0, pW2[:, 0:128], ALU.mult, ALU.subtract, [U, pW2], [tmpf])
                                    yield
                                    S.tt('pool', Un[:], tmpf[:], C[f'bm{bs}'], ALU.mult, [tmpf, cb], [Un])
                                    yield
                                    pX = S.P()
                                    pXb = pX.ap[:].bitcast(BF16)
                                    S.tr(pXb[:, 0:128], Un[:], ident_b[:], [Un, ident_b], [pX])
                                    Tn = G(f"gT{(lvl + 1) % 2}{key}", [128, 128], BF16)
                                    S.cp('act', Tn[:], pXb[:, 0:128], [pX], [Tn])
                                    yield
                                    Tk = Tn
                                else:
                                    S.stt(Un[:], U[:], 2.0, pW2[:, 0:128], ALU.mult, ALU.subtract, [U, pW2], [Un])
                                    yield
                                U = Un
                            pw = S.P()
                            S.mm(pw[:, 0:128], kbg[:], U[:], [kbg, U], [pw])
                            S.mm(pw[:, 128:256], U[:], vb[:], [U, vb], [pw])
                            wT = G(f"gwT{key}", [128, 128], BF16)
                            S.cp('act', wT[:], pw[:, 0:128], [pw], [wT])
                            u = F[3]
                            S.cp('dve', u[:], pw[:, 128:256], [pw], [u])
                            yield
                            ch.update(kt=kt, attnT=attnT, wT=wT, u=u, F=F, Bq=Bq)

                        def chain_scan(ch):
                            key, h, sl, sc, F, Bq = ch['key'], ch['h'], ch['sl'], ch['sc'], ch['F'], ch['Bq']
                            full = ch['full']
                            if not full:
                                p1 = S.P()
                                S.mm(p1[:, 0:128], ch['wT'][:], Sgb[h][:], [ch['wT'], Sgb[h]], [p1])
                                vn = Bq[1]
                                S.tt('dve', vn[:], ch['u'][:], p1[:, 0:128], ALU.subtract, [ch['u'], p1], [vn])
                                yield
                                p2 = S.P()
                                S.mm(p2[:, 0:128], ch['kt'][:], vn[:], [ch['kt'], vn], [p2])
                                S.stt(Sg[h][:], Sg[h][:], sc[:, 36 + h:37 + h], p2[:, 0:128], ALU.mult, ALU.add, [Sg[h], sc, p2], [Sg[h]])
                                yield
                                S.cp('act', Sgb[h][:], Sg[h][:], [Sg[h]], [Sgb[h]])
                                yield
                                return
                            qnT, szT = S.cache[f"gqnT{h}_{ch['p']}"], S.cache[f"gszT{h}_{ch['p']}"]
                            p1 = S.P()
                            S.mm(p1[:, 0:128], ch['wT'][:], Sgb[h][:], [ch['wT'], Sgb[h]], [p1])
                            S.mm(p1[:, 128:256], qnT[:, sl], Sgb[h][:], [qnT, Sgb[h]], [p1])
                            vn = Bq[1]
                            S.tt('dve', vn[:], ch['u'][:], p1[:, 0:128], ALU.subtract, [ch['u'], p1], [vn])
                            o1 = F[1]
                            S.act(o1[:], p1[:, 128:256], AF.Identity, [p1, sc], [o1], scale=sc[:, 32 + h:33 + h])
                            yield
                            p2 = S.P()
                            S.mm(p2[:, 0:128], ch['kt'][:], vn[:], [ch['kt'], vn], [p2])
                            S.mm(p2[:, 128:256], ch['attnT'][:], vn[:], [ch['attnT'], vn], [p2])
                            S.stt(Sg[h][:], Sg[h][:], sc[:, 36 + h:37 + h], p2[:, 0:128], ALU.mult, ALU.add, [Sg[h], sc, p2], [Sg[h]])
                            o = F[2]
                            S.tt('dve', o[:], o1[:], p2[:, 128:256], ALU.add, [o1, p2], [o])
                            yield
                            S.cp('act', Sgb[h][:], Sg[h][:], [Sg[h]], [Sgb[h]])
                            junk = F[0]
                            ssq = G(f"gssq{key}", [128, 4], F32)
                            S.op('pool', lambda e, ssq=ssq: e.memset(ssq[:], 0.0), [], [ssq])
                            yield
                            S.act(junk[:], o[:], AF.Square, [o], [junk, ssq], accum_out=ssq[:, 0:1])
                            yield
                            S.act(ssq[:, 1:2], ssq[:, 0:1], AF.Ln, [ssq, cc], [ssq], bias=cc[:, 2:3], scale=1.0 / 128)
                            yield
                            S.act(ssq[:, 2:3], ssq[:, 1:2], AF.Exp, [ssq], [ssq], scale=-0.5)
                            yield
                            on = F[1]
                            S.act(on[:], o[:], AF.Identity, [o, ssq], [on], scale=ssq[:, 2:3])
                            yield
                            pt = S.P()
                            S.tr(pt[:, 0:128], on[:], C['ident'], [on, cb], [pt])
                            loc = (ch['sb'] - NFULL0) * SBW + ch['tt'] * 128
                            S.stt(catT[:, 2 * h + 1, loc:loc + 128], pt[:, 0:128], dnwt[:, 0:1], szT[:, sl], ALU.mult, ALU.mult, [pt, dnwt, szT], [catT])
                            yield

                        def Bgens(p, sb_):
                            full_ = sb_ >= NFULL0
                            return ([gdn_prep(h, w_, p, sb_) for h in range(NH) for w_ in range(3) if (full_ or w_ > 0 or sb_ == NFULL0 - 1)]
                                    + ([gdn_prep_z(h, p) for h in range(NH)] if full_ else [])
                                    + [gdn_scal(tt, p, sb_) for tt in range(NT)])
                        p = sb % 2
                        if sb == 0:
                            rr(Bgens(0, 0))
                        chains = [dict(key=f"{tt}_{h}", tt=tt, h=h, p=p, sb=sb, full=(sb >= NFULL0), sl=slice(tt * 128, (tt + 1) * 128))
                                  for tt in range(NT) for h in range(NH)]
                        gens = [chain_pre(ch) for ch in chains]
                        if sb + 1 < NSB:
                            hT = emitA(sb + 1)
                            gens = gens + Bgens(1 - p, sb + 1)
                        rr(gens)
                        for tt in range(NT):
                            rr([chain_scan(ch) for ch in chains if ch['tt'] == tt])
                print('SBUF remaining in phase', phase, nc.sbuf_bytes_remaining, flush=True)
                S.barrier()
            S.cache = {}
            print('SBUF remaining after phase', phase, nc.sbuf_bytes_remaining, flush=True) if False else None
            if os.environ.get('KSTOP') == phase:
                return nc

        NTB = TS // 128
        NSL = T // TS
        with ExitStack() as esB:
            fnwt = S.sb("fnwt", [128, D], F32, esB)
            S.dma('sp', fnwt[:], fnw[:, :], writes=[fnwt], sembuf=fnwt)
            h2T = catT
            Gt = S.sb("Gt", [128, NTB, 32], F32, esB)
            g12 = S.sb("g12", [128, 2 * D], F32, esB)
            with ExitStack() as esB1:
                S.cache_es = esB1
                G = S.g
                for gi, base in ((0, 16), (1, 40)):
                    for c in range(8):
                        dg = S.g(f"dg{c % 2}", [128, 128], F32)
                        S.ts('dve', dg[:], C['ident'], mod[:, base + c:base + c + 1], ALU.mult, [cb, mod], [dg])
                        pg = S.P()
                        S.mm(pg[:, 0:128], ones_f[:], dg[:], [ones_f, dg], [pg])
                        S.cp('act', g12[:, gi * D + c * 128:gi * D + (c + 1) * 128], pg[:, 0:128], [pg], [g12])
                Wout = S.sb("Wout", [128, 8, D], BF16, esB1)
                S.dma('pool', Wout[:], w_out.rearrange("(c p) n -> p c n", p=128), writes=[Wout], sembuf=Wout)
                Wr = S.sb("Wr", [128, 8, 36], F32, esB1)
                S.dma('sp', Wr[:], w_r.rearrange("(c p) n -> p c n", p=128), writes=[Wr], sembuf=Wr)
                brt = S.sb("brt", [128, 36], F32, esB1)
                S.dma('sp', brt[:], b_r[:, :], writes=[brt], sembuf=brt)
                for tt in range(NTB):
                    sl = slice(tt * 128, (tt + 1) * 128)
                    xst = G(f"xst{tt % 2}", [128, D], F32)
                    S.dma('sp', xst[:], xs[tt * 128:(tt + 1) * 128, :], writes=[xst], sembuf=xst)
                    x1 = G(f"x1_{tt % 2}", [128, D], F32)
                    for half in range(2):
                        hs = slice(half * 512, (half + 1) * 512)
                        pm_ = S.P()
                        for c in range(8):
                            S.mm(pm_[:, :], catT[:, c, sl], Wout[:, c, hs], [catT, Wout], [pm_], start=(c == 0), stop=(c == 7))
                        tmp = G(f"mixg{half}", [128, 512], F32)
                        S.tt('dve', tmp[:], pm_[:, :], g12[:, half * 512:(half + 1) * 512], ALU.mult, [pm_, g12], [tmp])
                        S.tt('pool', x1[:, hs], tmp[:], xst[:, hs], ALU.add, [tmp, xst], [x1])
                    S.dma('sp', x1_d.ap[tt * 128:(tt + 1) * 128, :], x1[:], reads=[x1], writes=[x1_d], sembuf=x1)
                    junk = G("bjunk", [128, D], F32)
                    ssq = G(f"bssq{tt % 2}", [128, 4], F32)
                    S.op('pool', lambda e, ssq=ssq: e.memset(ssq[:], 0.0), [], [ssq])
                    S.act(junk[:], x1[:], AF.Square, [x1], [junk, ssq], accum_out=ssq[:, 0:1])
                    S.rsqrt((ssq[:, 2:3], ssq), ssq[:, 0:1], 1.0 / D, [ssq], (ssq[:, 1:2], ssq))
                    xn = G("bxn", [128, D], F32)
                    S.ts('dve', xn[:], x1[:], ssq[:, 2:3], ALU.mult, [x1, ssq], [xn])
                    h2f = G("h2f", [128, 8, 128], F32)
                    for c in range(8):
                        ptp = S.P()
                        S.tr(ptp[:, 0:128], xn[:, c * 128:(c + 1) * 128], C['ident'], [xn, cb], [ptp])
                        S.act(h2f[:, c, :], ptp[:, 0:128], AF.Identity, [ptp, a2, mod], [h2f], bias=mod[:, 24 + c:25 + c], scale=a2[:, c:c + 1])
                    S.cp('pool', h2T[:, :, sl], h2f[:], [h2f], [h2T])
                    plg = S.P()
                    for c in range(8):
                        S.mm(plg[:, 0:36], h2f[:, c, :], Wr[:, c, :], [h2f, Wr], [plg], start=(c == 0), stop=(c == 7))
                    r = G("rt", [128, 256], F32)
                    S.tt('dve', r[:, 0:36], plg[:, 0:36], brt[:], ALU.add, [plg, brt], [r])
                    S.op('dve', lambda e, r=r: e.reduce_max(out=r[:, 36:37], in_=r[:, 0:4], axis=AX.X), [r], [r])
                    S.ts('dve', r[:, 40:44], r[:, 0:4], r[:, 36:37], ALU.is_equal, [r], [r])
                    S.ts('dve', r[:, 44:48], r[:, 0:4], r[:, 36:37], ALU.subtract, [r], [r])
                    S.act(r[:, 44:48], r[:, 44:48], AF.Exp, [r], [r])
                    S.op('dve', lambda e, r=r: e.reduce_sum(out=r[:, 48:49], in_=r[:, 44:48], axis=AX.X), [r], [r])
                    S.op('dve', lambda e, r=r: e.reciprocal(out=r[:, 49:50], in_=r[:, 48:49]), [r], [r])
                    S.ts('dve', r[:, 44:48], r[:, 40:44], 1.0, ALU.subtract, [r], [r], s2=1e30, op1=ALU.mult)
                    S.tt('dve', r[:, 64:96].rearrange("p (g e) -> p g e", e=8), r[:, 4:36].rearrange("p (g e) -> p g e", e=8),
                         r[:, 44:48].unsqueeze(2).to_broadcast([128, 4, 8]), ALU.add, [r], [r])
                    S.op('dve', lambda e, r=r: e.reduce_max(out=r[:, 96:97], in_=r[:, 64:96], axis=AX.X), [r], [r])
                    S.ts('dve', r[:, 100:132], r[:, 64:96], r[:, 96:97], ALU.is_equal, [r], [r])
                    S.stt(r[:, 132:164], r[:, 100:132], -1e30, r[:, 64:96], ALU.mult, ALU.add, [r], [r])
                    S.op('dve', lambda e, r=r: e.reduce_max(out=r[:, 164:165], in_=r[:, 132:164], axis=AX.X), [r], [r])
                    S.ts('dve', r[:, 168:200], r[:, 132:164], r[:, 164:165], ALU.is_equal, [r], [r])
                    S.tt('dve', r[:, 200:201], r[:, 164:165], r[:, 96:97], ALU.subtract, [r], [r])
                    S.act(r[:, 200:201], r[:, 200:201], AF.Exp, [r], [r])
                    S.ts('dve', r[:, 201:202], r[:, 200:201], 1.0, ALU.add, [r], [r])
                    S.op('dve', lambda e, r=r: e.reciprocal(out=r[:, 202:203], in_=r[:, 201:202]), [r], [r])
                    S.tt('dve', r[:, 202:203], r[:, 202:203], r[:, 49:50], ALU.mult, [r], [r])
                    S.tt('dve', r[:, 203:204], r[:, 202:203], r[:, 200:201], ALU.mult, [r], [r])
                    S.ts('dve', r[:, 204:236], r[:, 100:132], r[:, 202:203], ALU.mult, [r], [r])
                    S.stt(Gt[:, tt, :], r[:, 168:200], r[:, 203:204], r[:, 204:236], ALU.mult, ALU.add, [r], [Gt])
                S.barrier()
            S.cache = {}
            if os.environ.get('KSTOP') == 'b1':
                return nc
            acc = S.sb("acc", [128, NTB, D], F32, esB)
            S.op('pool', lambda e: e.memset(acc[:], 0.0), [], [acc])
            with ExitStack() as esB2:
                S.cache_es = esB2
                G = S.g
                NB = max(1, TS // 512)
                BW = min(512, TS)
                for ex_ in range(NE):
                    w1t = G(f"w1t{ex_ % 2}", [128, 8, DEXP], BF16)
                    w3t = G(f"w3t{ex_ % 2}", [128, 8, DEXP], BF16)
                    w2t = G(f"w2t{ex_ % 2}", [128, 4, D], BF16)
                    S.dma('pool', w1t[:], w1[ex_].rearrange("(c p) n -> p c n", p=128), writes=[w1t], sembuf=w1t)
                    S.dma('pool', w3t[:], w3[ex_].rearrange("(c p) n -> p c n", p=128), writes=[w3t], sembuf=w3t)
                    S.dma('pool', w2t[:], w2[ex_].rearrange("(c p) n -> p c n", p=128), writes=[w2t], sembuf=w2t)
                    for tb in range(NB):
                        bsl = slice(tb * BW, (tb + 1) * BW)
                        hid = G(f"hid{tb % 2}", [128, 4, BW], BF16)
                        for hc in range(4):
                            p1 = S.P()
                            for c in range(8):
                                S.mm(p1[:, 0:BW], w1t[:, c, hc * 128:(hc + 1) * 128], h2T[:, c, bsl], [w1t, h2T], [p1], start=(c == 0), stop=(c == 7))
                            p3 = S.P()
                            for c in range(8):
                                S.mm(p3[:, 0:BW], w3t[:, c, hc * 128:(hc + 1) * 128], h2T[:, c, bsl], [w3t, h2T], [p3], start=(c == 0), stop=(c == 7))
                            sl_ = G(f"silu{hc % 2}", [128, BW], F32)
                            S.act(sl_[:], p1[:, 0:BW], AF.Silu, [p1], [sl_])
                            S.tt('dve', hid[:, hc, :], sl_[:], p3[:, 0:BW], ALU.mult, [sl_, p3], [hid])
                        for t4 in range(BW // 128):
                            tt = tb * (BW // 128) + t4
                            for half in range(2):
                                py = S.P()
                                for hc in range(4):
                                    S.mm(py[:, :], hid[:, hc, t4 * 128:(t4 + 1) * 128], w2t[:, hc, half * 512:(half + 1) * 512], [hid, w2t], [py],
                                         start=(hc == 0), stop=(hc == 3))
                                S.stt(acc[:, tt, half * 512:(half + 1) * 512], py[:, :], Gt[:, tt, ex_:ex_ + 1], acc[:, tt, half * 512:(half + 1) * 512],
                                      ALU.mult, ALU.add, [py, Gt, acc], [acc])
                S.barrier()
            S.cache = {}
            if os.environ.get('KSTOP') == 'b2':
                return nc
            with ExitStack() as esB3:
                S.cache_es = esB3
                G = S.g
                for tt in range(NTB):
                    x1 = G(f"fx1_{tt % 2}", [128, D], F32)
                    S.dma('sp', x1[:], x1_d.ap[tt * 128:(tt + 1) * 128, :], reads=[x1_d], writes=[x1], sembuf=x1)
                    x2 = G(f"fx2_{tt % 2}", [128, D], F32)
                    S.tt('pool', x2[:], acc[:, tt, :], g12[:, D:2 * D], ALU.mult, [acc, g12], [x2])
                    S.tt('dve', x2[:], x2[:], x1[:], ALU.add, [x2, x1], [x2])
                    junk = G("fjunk", [128, D], F32)
                    ssq = G(f"fssq{tt % 2}", [128, 4], F32)
                    S.op('pool', lambda e, ssq=ssq: e.memset(ssq[:], 0.0), [], [ssq])
                    S.act(junk[:], x2[:], AF.Square, [x2], [junk, ssq], accum_out=ssq[:, 0:1])
                    S.rsqrt((ssq[:, 2:3], ssq), ssq[:, 0:1], 1.0 / D, [ssq], (ssq[:, 1:2], ssq))
                    ot = G(f"fot{tt % 2}", [128, D], F32)
                    S.stt(ot[:], x2[:], ssq[:, 2:3], fnwt[:], ALU.mult, ALU.mult, [x2, ssq, fnwt], [ot])
                    S.dma('sp', out[tt * 128:(tt + 1) * 128, :], ot[:], reads=[ot], sembuf=ot)
                S.barrier()
        print("ninst", S.ninst, "nsem", S.nsem, flush=True)
    return nc


def _host_inputs(inp, T, TS):
    NH = NHEAD
    NE_ = int(os.environ.get('KNEXP', NEXP))
    f = lambda a: np.ascontiguousarray(np.asarray(a), dtype=np.float32)
    x = f(inp['x'])
    B = x.shape[0]
    w_in = f(inp['w_in'])[0]
    swap = np.concatenate([np.arange(64, 128), np.arange(0, 64)])
    cols = []
    for h in range(NH):
        q = np.arange(h * 128, (h + 1) * 128)
        k = 512 + q
        v = 1024 + q
        g = 1536 + q
        cols += [q, q[swap], k, k[swap], v, g]
    for h in range(NH):
        q = 2048 + np.arange(h * 128, (h + 1) * 128)
        cols += [q, q + 512, q + 1024, q + 1536]
    cols = np.concatenate(cols)
    w_inA = np.ascontiguousarray(w_in[:, cols])
    w_ab = np.ascontiguousarray(w_in[:, 4096:4104])
    conv = f(inp['conv_w'])[0]
    convw = np.zeros((128, NH * 12), np.float32)
    for h in range(NH):
        for w_ in range(3):
            ch = w_ * 512 + h * 128 + np.arange(128)
            convw[:, h * 12 + w_ * 4:h * 12 + w_ * 4 + 4] = conv[:, ch].T
    hv = np.broadcast_to(np.concatenate([f(inp['a_log'])[0], f(inp['dt_bias'])[0]])[None, :], (128, 2 * NH)).copy()
    dnw = f(inp['dn_norm_w'])[0].reshape(128, 1).copy()
    w_out = f(inp['w_out'])[0]
    rows = []
    for h in range(NH):
        rows += [np.arange(h * 128, (h + 1) * 128), 512 + np.arange(h * 128, (h + 1) * 128)]
    w_outP = np.ascontiguousarray(w_out[np.concatenate(rows), :])
    w_r = np.ascontiguousarray(np.concatenate([f(inp['w_group'])[0], f(inp['w_expert'])[0]], axis=1))
    b_r = np.broadcast_to(np.concatenate([f(inp['b_group'])[0], f(inp['b_expert'])[0]])[None, :], (128, 36)).copy()
    n12 = np.concatenate([f(inp['norm1_w'])[0].reshape(8, 128).T, f(inp['norm2_w'])[0].reshape(8, 128).T], axis=1).copy()
    b_adaT = np.ascontiguousarray(f(inp['b_ada'])[0].reshape(48, 128).T)
    fnw = np.broadcast_to(f(inp['final_norm_w'])[None, :], (128, D)).copy()
    shared = dict(w_ada=f(inp['w_ada'])[0], b_adaT=b_adaT, n12=n12, w_inA=w_inA, w_ab=w_ab, convw=convw, hv=hv, dnw=dnw,
                  w_out=w_outP, w_r=w_r, b_r=b_r, w1=f(inp['w1'])[0][:NE_], w3=f(inp['w3'])[0][:NE_], w2=f(inp['w2'])[0][:NE_], fnw=fnw,
                  cbig=CBIG, ccol=CCOL)
    pos = np.asarray(inp['positions']).astype(np.int32)
    c = f(inp['c'])
    maps = []
    nsl = T // TS
    for core in range(B * nsl):
        b, s = core // nsl, core % nsl
        m = dict(shared)
        npad = (nsl - 1 - s) * TS
        xTp = np.zeros((D, T), np.float32)
        xTp[:, npad:] = x[b, :(s + 1) * TS, :].T
        m['xT'] = xTp
        SBW_ = min(256, TS)
        vmk = np.zeros((128, T // SBW_), np.float32)
        vmk[:, npad // SBW_:] = 1.0
        m['vmask'] = vmk
        m['xs'] = np.ascontiguousarray(x[b, s * TS:(s + 1) * TS, :])
        posp = np.zeros((1, T), np.int32)
        posp[0, npad:] = pos[b, :(s + 1) * TS]
        m['pos'] = posp
        m['cT'] = np.ascontiguousarray(c[b].reshape(8, 128).T)
        sm = np.zeros((128, 8), np.float32)
        sm[:, s] = 1.0
        m['selm'] = sm
        maps.append(m)
    return maps


_NC_CACHE = {}


def _run(inp, T, TS):
    key = (T, TS)
    if key not in _NC_CACHE:
        _NC_CACHE[key] = build(T, TS, SBW=min(256, TS))
    nc = _NC_CACHE[key]
    maps = _host_inputs(inp, T, TS)
    res = run_bass_kernel_spmd(nc, maps, core_ids=list(range(len(maps))))
    B = np.asarray(inp['x']).shape[0]
    nsl = T // TS
    out = np.zeros((B, T, D), np.float32)
    for core in range(B * nsl):
        b, s = core // nsl, core % nsl
        out[b, s * TS:(s + 1) * TS, :] = np.asarray(res.results[core]['out'])
    return out


def kernel(**inputs):
    return _run(inputs, 8192, 2048)
```

```python
import math
import os
from contextlib import ExitStack
import numpy as np
import concourse.bass as bass
import concourse.mybir as mybir
from concourse.bass_utils import run_bass_kernel_spmd

F32 = mybir.dt.float32
BF16 = mybir.dt.bfloat16
I32 = mybir.dt.int32
AF = mybir.ActivationFunctionType
ALU = mybir.AluOpType
AX = mybir.AxisListType

D = 1024
NHEAD = 4
EPS = 1e-6
NEXP = 32
DEXP = 512
TWO_PI = 2.0 * math.pi


class Buf:
    def __init__(self, ap, name):
        self.ap = ap
        self.name = name
        self.ws = {}
        self.rs = {}
        self.dsem = None
        self.dcnt = 0
        self.psum = False

    def __getitem__(self, k):
        return self.ap[k]


class Sched:
    EPOCH = 30000

    def __init__(self, nc, es):
        self.nc = nc
        self.es = es
        self.eng = {'pe': nc.tensor, 'act': nc.scalar, 'dve': nc.vector, 'pool': nc.gpsimd, 'sp': nc.sync}
        self.sem = {}
        self.cnt = {}
        self.waited = {e: {} for e in self.eng}
        self.nsem = 0
        self.allsems = {}
        for e in self.eng:
            self._newsem(e)
        self.bufs = []
        self.ninst = {e: 0 for e in self.eng}
        self.cache = {}
        self.pbanks = []
        self.pi = 0

    def _mksem(self, name):
        self.nsem += 1
        s = self.es.enter_context(self.nc.semaphore(name))
        return s

    def _newsem(self, e):
        self.sem[e] = self._mksem(f"c_{e}_{self.nsem}")
        self.cnt[e] = 0

    def sb(self, name, shape, dt, es=None):
        self.uid = getattr(self, 'uid', 0) + 1
        name = f"s{self.uid}_{name}"
        t = (es or self.es).enter_context(self.nc.sbuf_tensor(name, list(shape), dt))
        b = Buf(t, name)
        self.bufs.append(b)
        return b

    def g(self, name, shape, dt):
        if name not in self.cache:
            self.cache[name] = self.sb(name, shape, dt, es=self.cache_es)
        return self.cache[name]

    def mkpsum(self):
        for i in range(8):
            t = self.es.enter_context(self.nc.psum_tensor(f"pb{i}", [128, 512], F32))
            b = Buf(t, f"pb{i}")
            b.psum = True
            self.bufs.append(b)
            self.pbanks.append(b)

    def P(self):
        b = self.pbanks[self.pi % 8]
        self.pi += 1
        return b

    def dram(self, name, shape, dt, kind="Internal"):
        t = self.nc.dram_tensor(name, list(shape), dt, kind=kind).ap()
        b = Buf(t, name)
        self.bufs.append(b)
        return b

    def _deps(self, reads, writes, e=None):
        deps = {}

        def add(d):
            for k, (s, v) in d.items():
                if k not in deps or deps[k][1] < v:
                    deps[k] = (s, v)
        for b in reads:
            add(b.ws)
            if b.psum:
                own = id(self.sem[e]) if e in self.sem else None
                add({k: v for k, v in b.rs.items() if k != own})
        for b in writes:
            add(b.ws)
            add(b.rs)
        return deps

    def _wait(self, e, deps):
        for k, (s, v) in deps.items():
            if e == 'pe' and s is self.sem['pe']:
                continue
            if self.waited[e].get(k, 0) >= v:
                continue
            self.eng[e].wait_ge(s, v)
            self.ninst[e] += 1
            self.waited[e][k] = v

    def _record(self, ev, reads, writes):
        k = id(ev[0])
        for b in reads:
            if k not in b.rs or b.rs[k][1] < ev[1]:
                b.rs[k] = ev
        for b in writes:
            if k not in b.ws or b.ws[k][1] < ev[1]:
                b.ws[k] = ev

    def op(self, e, fn, reads=(), writes=()):
        if e == 'pool' and os.environ.get('KNOPOOL'):
            e = 'dve'
        self._wait(e, self._deps(reads, writes, e))
        ins = fn(self.eng[e])
        if self.cnt[e] >= self.EPOCH:
            self._newsem(e)
        self.cnt[e] += 1
        ins.then_inc(self.sem[e], 1)
        self.ninst[e] += 1
        self._record((self.sem[e], self.cnt[e]), reads, writes)
        return ins

    def dma(self, q, out_ap, in_ap, reads=(), writes=(), sembuf=None, **kw):
        self._wait(q, self._deps(reads, writes))
        if sembuf.dsem is None:
            sembuf.dsem = self._mksem(f"d_{sembuf.name}")
        ins = self.eng[q].dma_start(out=out_ap, in_=in_ap, **kw)
        sembuf.dcnt += 1
        ins.then_inc(sembuf.dsem, 16)
        self.ninst[q] += 1
        self._record((sembuf.dsem, 16 * sembuf.dcnt), reads, writes)
        return ins

    def barrier(self, engines=('pe', 'act', 'dve', 'pool', 'sp')):
        deps = {}
        for b in self.bufs:
            for d in (b.ws, b.rs):
                for k, (s, v) in d.items():
                    if k not in deps or deps[k][1] < v:
                        deps[k] = (s, v)
        for e in engines:
            self._wait(e, deps)

    def act(self, out, in_, func, reads, writes, **kw):
        return self.op('act', lambda e: e.activation(out=out, in_=in_, func=func, **kw), reads, writes)

    def tt(self, eng, out, a, b, op, reads, writes):
        return self.op(eng, lambda e: e.tensor_tensor(out=out, in0=a, in1=b, op=op), reads, writes)

    def ts(self, eng, out, a, s1, op0, reads, writes, s2=None, op1=None):
        if op1 is None:
            return self.op(eng, lambda e: e.tensor_scalar(out=out, in0=a, scalar1=s1, scalar2=None, op0=op0), reads, writes)
        return self.op(eng, lambda e: e.tensor_scalar(out=out, in0=a, scalar1=s1, scalar2=s2, op0=op0, op1=op1), reads, writes)

    def stt(self, out, a, sc, b, op0, op1, reads, writes):
        return self.op('dve', lambda e: e.scalar_tensor_tensor(out=out, in0=a, scalar=sc, in1=b, op0=op0, op1=op1), reads, writes)

    def cp(self, eng, out, in_, reads, writes):
        if eng == 'act':
            return self.act(out, in_, AF.Copy, reads, writes)
        return self.op(eng, lambda e: e.tensor_copy(out=out, in_=in_), reads, writes)

    def mm(self, out, lhsT, rhs, reads, writes, start=True, stop=True):
        return self.op('pe', lambda e: e.matmul(out, lhsT=lhsT, rhs=rhs, start=start, stop=stop), reads, writes)

    def tr(self, out, in_, ident, reads, writes):
        return self.op('pe', lambda e: e.transpose(out=out, in_=in_, identity=ident), reads, writes)

    def rsqrt(self, out, in_, scale, reads_in, tmp, eps=EPS):
        self.act(tmp[0], in_, AF.Ln, list(reads_in) + [self.epsbuf], [tmp[1]], bias=self.epsc[:in_.shape[0], 0:1], scale=scale)
        self.act(out[0], tmp[0], AF.Exp, [tmp[1]], [out[1]], scale=-0.5)


def rr(gens):
    gens = list(gens)
    while gens:
        nxt = []
        for g_ in gens:
            try:
                next(g_)
                nxt.append(g_)
            except StopIteration:
                pass
        gens = nxt


def _consts():
    i = np.arange(128)
    c = {}
    c['ident'] = np.eye(128, dtype=np.float32)
    c['triU'] = (i[:, None] <= i[None, :]).astype(np.float32)
    c['negtriU'] = -c['triU']
    c['negmask'] = np.where(i[None, :] <= i[:, None], 0.0, -1e30).astype(np.float32)
    c['m2L'] = ((i[:, None] == i[None, :] + 1) & (i[:, None] % 2 == 1)).astype(np.float32)
    for s in (4, 8, 16, 32, 64):
        c[f'bm{s}'] = ((i[:, None] // s) == (i[None, :] // s)).astype(np.float32)
    gam = 1.0 - np.power(2.0, -5.0 - np.arange(NHEAD))
    lg = np.log1p(-np.power(2.0, -5.0 - np.arange(NHEAD, dtype=np.float64)))
    rel = i[None, :] - i[:, None]
    for h in range(NHEAD):
        c[f'decT{h}'] = (np.where(rel >= 0, np.exp(lg[h] * np.maximum(rel, 0)), 0.0) * 128 ** -0.5).astype(np.float32)
        c[f'qdec{h}'] = np.broadcast_to(np.exp(lg[h] * (i + 1.0))[None, :], (128, 128)).astype(np.float32).copy()
    kws = np.stack([np.exp(lg[h] * (127 - i)) * 128 ** -0.5 for h in range(NHEAD)], axis=1)
    cd = [float(np.exp(lg[h] * 128)) for h in range(NHEAD)]
    invf = (10000.0 ** (-(np.arange(0, 128, 2, dtype=np.float32)) / 128.0)).astype(np.float32)
    col = np.zeros((128, 8), np.float32)
    col[:, 0] = np.concatenate([invf, invf])
    col[:, 1] = np.concatenate([-np.ones(64), np.ones(64)])
    col[:, 2] = EPS
    col[:, 3] = math.pi / 2
    col[:, 4:8] = kws
    names = ['ident', 'triU', 'negtriU', 'negmask', 'm2L', 'bm4', 'bm8', 'bm16', 'bm32', 'bm64'] + \
            [f'decT{h}' for h in range(NHEAD)] + [f'qdec{h}' for h in range(NHEAD)]
    big = np.concatenate([c[n] for n in names], axis=1).astype(np.float32)
    return names, big, col, cd


CNAMES, CBIG, CCOL, CD = _consts()


def build(T, TS, SBW=256, dbg=False):
    NH = NHEAD
    NSB = T // SBW
    NT = SBW // 128
    NCOL = NH * 10 * 128
    NE = int(os.environ.get('KNEXP', NEXP))
    nc = bass.Bass("TRN2", target_bir_lowering=False)
    es = ExitStack()
    with es:
        S = Sched(nc, es)
        S.mkpsum()
        dt_in = {}

        def din(name, shape, dt=F32):
            dt_in[name] = nc.dram_tensor(name, list(shape), dt, kind="ExternalInput").ap()
            return dt_in[name]
        xT = din("xT", [D, T])
        xs = din("xs", [TS, D])
        pos = din("pos", [1, T], I32)
        cT = din("cT", [128, 8])
        w_ada = din("w_ada", [D, 6 * D])
        b_adaT = din("b_adaT", [128, 48])
        n12 = din("n12", [128, 16])
        w_inA = din("w_inA", [D, NCOL])
        w_ab = din("w_ab", [D, 2 * NH])
        convw = din("convw", [128, NH * 12])
        hv = din("hv", [128, 2 * NH])
        dnw = din("dnw", [128, 1])
        w_out = din("w_out", [D, D])
        w_r = din("w_r", [D, 36])
        b_r = din("b_r", [128, 36])
        w1 = din("w1", [NE, D, DEXP])
        w3 = din("w3", [NE, D, DEXP])
        w2 = din("w2", [NE, DEXP, D])
        fnw = din("fnw", [128, D])
        cbig = din("cbig", [128, CBIG.shape[1]])
        ccol = din("ccol", [128, 8])
        selm_in = din("selm", [128, 8])
        vmask_in = din("vmask", [128, NSB])
        out = nc.dram_tensor("out", [TS, D], F32, kind="ExternalOutput").ap()
        x1_d = S.dram("x1_d", [TS, D], F32)
        dbg_outs = {}

        cb = S.sb("cbig", [128, CBIG.shape[1]], F32)
        S.dma('sp', cb[:], cbig[:, :], writes=[cb], sembuf=cb)
        cc = S.sb("ccol", [128, 8], F32)
        S.dma('sp', cc[:], ccol[:, :], writes=[cc], sembuf=cc)
        S.epsc = cc.ap[:, 2:3]
        S.epsbuf = cc
        C = {n: cb.ap[:, k * 128:(k + 1) * 128] for k, n in enumerate(CNAMES)}
        ident_b = S.sb("ident_b", [128, 128], BF16)
        S.cp('dve', ident_b[:], C['ident'], [cb], [ident_b])
        ones_b = S.sb("ones_b", [128, 128], BF16)
        S.op('pool', lambda e: e.memset(ones_b[:], 1.0), [], [ones_b])
        ones_f = S.sb("ones_f", [128, 128], F32)
        S.op('pool', lambda e: e.memset(ones_f[:], 1.0), [], [ones_f])
        bmb = {}
        for s_ in (4, 8, 16, 32, 64):
            bmb[s_] = S.sb(f"bmb{s_}", [128, 128], BF16)
            S.cp('dve', bmb[s_][:], C[f'bm{s_}'], [cb], [bmb[s_]])
        m2Lb = S.sb("m2Lb", [128, 128], BF16)
        S.cp('dve', m2Lb[:], C['m2L'], [cb], [m2Lb])
        selm = S.sb("selm", [128, 8], F32)
        S.dma('sp', selm[:], selm_in[:, :], writes=[selm], sembuf=selm)
        vm = S.sb("vmask", [128, NSB], F32)
        S.dma('sp', vm[:], vmask_in[:, :], writes=[vm], sembuf=vm)
        NFULL0 = NSB - TS // SBW
        catT = S.sb("catT", [128, 8, TS], BF16)
        S.op('pool', lambda e: e.memset(catT[:], 0.0), [], [catT])
        mod = S.sb("mod", [128, 48], F32)
        a1 = S.sb("a1", [128, 8], F32)
        a2 = S.sb("a2", [128, 8], F32)

        with ExitStack() as es0:
            S.cache_es = es0
            ct = S.sb("ct", [128, 8], F32, es0)
            S.dma('sp', ct[:], cT[:, :], writes=[ct], sembuf=ct)
            sct = S.sb("sct", [128, 8], F32, es0)
            S.act(sct[:], ct[:], AF.Silu, [ct], [sct])
            bad = S.sb("bad", [128, 48], F32, es0)
            S.dma('sp', bad[:], b_adaT[:, :], writes=[bad], sembuf=bad)
            n12t = S.sb("n12t", [128, 16], F32, es0)
            S.dma('sp', n12t[:], n12[:, :], writes=[n12t], sembuf=n12t)
            pm = S.P()
            for j in range(6):
                wa = S.g(f"wada{j % 2}", [128, 8, D], F32)
                S.dma('sp', wa[:], w_ada[:, j * D:(j + 1) * D].rearrange("(c p) n -> p c n", p=128), writes=[wa], sembuf=wa)
                for oc in range(8):
                    col_ = j * 8 + oc
                    for c in range(8):
                        S.mm(pm[:, col_:col_ + 1], wa[:, c, oc * 128:(oc + 1) * 128], sct[:, c:c + 1], [wa, sct], [pm],
                             start=(c == 0), stop=(c == 7))
            S.tt('dve', mod[:], pm[:, 0:48], bad[:], ALU.add, [pm, bad], [mod])
            S.stt(a1[:], mod[:, 8:16], 1.0, n12t[:, 0:8], ALU.add, ALU.mult, [mod, n12t], [a1])
            S.stt(a2[:], mod[:, 32:40], 1.0, n12t[:, 8:16], ALU.add, ALU.mult, [mod, n12t], [a2])
            S.barrier()
        S.cache = {}
        if os.environ.get('KSTOP') == 'pre':
            return nc

        for phase in ('ret', 'gdn'):
            with ExitStack() as esA:
                S.cache_es = esA
                G = S.g
                if phase == 'ret':
                    PC0, PCN = 0, NH * 6 * 128
                else:
                    PC0, PCN = NH * 6 * 128, NH * 4 * 128
                Win = S.sb("Win" + phase, [128, 8, PCN], BF16, esA)
                for c in range(8):
                    S.dma('pool', Win[:, c, :], w_inA[c * 128:(c + 1) * 128, PC0:PC0 + PCN], writes=[Win], sembuf=Win)
                Wab = S.sb("Wab" + phase, [128, 8, 2 * NH], BF16, esA)
                S.dma('pool', Wab[:], w_ab.rearrange("(c p) n -> p c n", p=128), writes=[Wab], sembuf=Wab)
                cw = S.sb("cw" + phase, [128, NH * 12], F32, esA)
                S.dma('sp', cw[:], convw[:, :], writes=[cw], sembuf=cw)
                hvt = S.sb("hvt" + phase, [128, 2 * NH], F32, esA)
                S.dma('sp', hvt[:], hv[:, :], writes=[hvt], sembuf=hvt)
                dnwt = S.sb("dnwt" + phase, [128, 1], F32, esA)
                S.dma('sp', dnwt[:], dnw[:, :], writes=[dnwt], sembuf=dnwt)
                nea = S.sb("nea" + phase, [128, NH], F32, esA)
                S.act(nea[:], hvt[:, 0:NH], AF.Exp, [hvt], [nea])
                S.ts('dve', nea[:], nea[:], -1.0, ALU.mult, [nea], [nea])
                Sr, Srb, Sg, Sgb, cbuf = [], [], [], [], []
                for h in range(NH):
                    for lst, nm, dt in ((Sr, "Sr", F32), (Srb, "Srb", BF16), (Sg, "Sg", F32), (Sgb, "Sgb", BF16)):
                        b = S.sb(f"{nm}{h}{phase}", [128, 128], dt, esA)
                        S.op('pool', lambda e, b=b: e.memset(b[:], 0.0), [], [b])
                        lst.append(b)
                    row = []
                    for w_ in range(3):
                        b = S.sb(f"cbuf{h}_{w_}{phase}", [128, SBW + 3], F32, esA)
                        S.op('pool', lambda e, b=b: e.memset(b[:], 0.0), [], [b])
                        row.append(b)
                    cbuf.append(row)

                def proj(hT, col):
                    ps = S.P()
                    for c in range(8):
                        S.mm(ps[:, 0:SBW], Win[:, c, col - PC0:col - PC0 + 128], hT[:, c, :], [Win, hT], [ps], start=(c == 0), stop=(c == 7))
                    return ps

                def emitA(sb_):
                    t0 = sb_ * SBW
                    xt = G("xt", [128, 8, SBW], F32)
                    S.dma('sp', xt[:], xT[:, t0:t0 + SBW].rearrange("(c p) t -> p c t", p=128), writes=[xt], sembuf=xt)
                    xsq = G("xsq", [128, 8, SBW], BF16)
                    S.act(xsq[:], xt[:], AF.Square, [xt], [xsq])
                    pss = S.P()
                    for c in range(8):
                        S.mm(pss[:, 0:SBW], ones_b[:], xsq[:, c, :], [ones_b, xsq], [pss], start=(c == 0), stop=(c == 7))
                    lnt = G("lnt", [128, SBW], F32)
                    rstd = G("rstd", [128, SBW], F32)
                    S.rsqrt((rstd[:], rstd), pss[:, 0:SBW], 1.0 / D, [pss], (lnt[:], lnt))
                    hT = G("hT", [128, 8, SBW], BF16)
                    for c in range(8):
                        tmp = G(f"xn{c % 2}", [128, SBW], F32)
                        S.tt('dve' if c % 2 == 0 else 'pool', tmp[:], xt[:, c, :], rstd[:], ALU.mult, [xt, rstd], [tmp])
                        S.act(hT[:, c, :], tmp[:], AF.Identity, [tmp, a1, mod], [hT], bias=mod[:, c:c + 1], scale=a1[:, c:c + 1])
                    return hT

                for sb in range(NSB):
                    t0 = sb * SBW
                    if phase == 'ret' or sb == 0:
                        hT = emitA(sb)
                    if os.environ.get('KSTOP') == 'ret1':
                        S.barrier(engines=('sp',))
                        return nc
                    if phase == 'ret':
                        posi = G("posi", [128, SBW], I32)
                        S.dma('sp', posi[:], pos[0:1, t0:t0 + SBW].partition_broadcast(128), writes=[posi], sembuf=posi)
                        posf = G("posf", [128, SBW], F32)
                        S.cp('dve', posf[:], posi[:], [posi], [posf])
                        ang = G("ang", [128, SBW], F32)
                        S.ts('dve', ang[:], posf[:], cc[:, 0:1], ALU.mult, [posf, cc], [ang])
                        tabs = []
                        for which in range(2):
                            if which == 1:
                                ang2 = G("ang2", [128, SBW], F32)
                                S.ts('pool', ang2[:], ang[:], math.pi / 2, ALU.add, [ang], [ang2])
                                a_ = ang2
                            else:
                                a_ = ang
                            ki = G(f"ki{which}", [128, SBW], I32)
                            S.ts('dve', ki[:], a_[:], 1.0 / TWO_PI, ALU.mult, [a_], [ki])
                            kf = G(f"kf{which}", [128, SBW], F32)
                            S.cp('pool', kf[:], ki[:], [ki], [kf])
                            rr_ = G(f"rr{which}", [128, SBW], F32)
                            S.stt(rr_[:], kf[:], -TWO_PI, a_[:], ALU.mult, ALU.add, [kf, a_], [rr_])
                            S.ts('pool', rr_[:], rr_[:], math.pi, ALU.min, [rr_], [rr_], s2=-math.pi, op1=ALU.max)
                            tb = G(f"tab{which}", [128, SBW], F32)
                            S.act(tb[:], rr_[:], AF.Sin, [rr_], [tb])
                            tabs.append(tb)
                        sint, cost = tabs
                        sins = G("sins", [128, SBW], F32)
                        S.act(sins[:], sint[:], AF.Identity, [sint, cc], [sins], scale=cc[:, 1:2])

                        def ret_prep(h, full):
                            base = h * 6 * 128
                            for nm, off in ((("q", 0), ("k", 2)) if full else (("k", 2),)):
                                t1 = G(f"rt1{nm}{h}", [128, SBW], F32)
                                t2 = G(f"rt2{nm}{h}", [128, SBW], F32)
                                p1 = proj(hT, base + off * 128)
                                S.tt('dve', t1[:], p1[:, 0:SBW], cost[:], ALU.mult, [p1, cost], [t1])
                                yield
                                p2 = proj(hT, base + (off + 1) * 128)
                                S.tt('dve', t2[:], p2[:, 0:SBW], sins[:], ALU.mult, [p2, sins], [t2])
                                yield
                                o_ = G(f"r{nm}T{h}", [128, SBW], BF16)
                                S.tt('pool', o_[:], t1[:], t2[:], ALU.add, [t1, t2], [o_])
                                yield
                            pv = proj(hT, base + 4 * 128)
                            vT = G(f"rvT{h}", [128, SBW], BF16)
                            S.cp('act', vT[:], pv[:, 0:SBW], [pv], [vT])
                            yield
                            if full:
                                pg_ = proj(hT, base + 5 * 128)
                                sgT = G(f"rsgT{h}", [128, SBW], F32)
                                S.act(sgT[:], pg_[:, 0:SBW], AF.Silu, [pg_], [sgT])
                                yield

                        def ret_chain(tt, h, sb_, full):
                            sl = slice(tt * 128, (tt + 1) * 128)
                            krT, vT = S.cache[f"rkT{h}"], S.cache[f"rvT{h}"]
                            if full:
                                qrT, sgT = S.cache[f"rqT{h}"], S.cache[f"rsgT{h}"]
                            pk = S.P()
                            pkb = pk.ap[:].bitcast(BF16)
                            S.tr(pkb[:, 0:128], krT[:, sl], ident_b[:], [krT, ident_b], [pk])
                            S.tr(pkb[:, 128:256], vT[:, sl], ident_b[:], [vT, ident_b], [pk])
                            kw = G(f"rkw{h}", [128, 128], BF16)
                            S.ts('dve', kw[:], pkb[:, 0:128], cc[:, 4 + h:5 + h], ALU.mult, [pk, cc], [kw])
                            vtok = G(f"rvtok{h}", [128, 128], BF16)
                            S.act(vtok[:], pkb[:, 128:256], AF.Identity, [pk, vm], [vtok], scale=vm[:, sb_:sb_ + 1])
                            if not full:
                                yield
                                po = S.P()
                                S.mm(po[:, 128:256], kw[:], vtok[:], [kw, vtok], [po])
                                S.stt(Sr[h][:], Sr[h][:], CD[h], po[:, 128:256], ALU.mult, ALU.add, [Sr[h], po], [Sr[h]])
                                yield
                                S.cp('pool', Srb[h][:], Sr[h][:], [Sr[h]], [Srb[h]])
                                yield
                                return
                            qwT = G(f"rqw{h}", [128, 128], BF16)
                            S.tt('pool', qwT[:], qrT[:, sl], C[f'qdec{h}'], ALU.mult, [qrT, cb], [qwT])
                            yield
                            psc = S.P()
                            S.mm(psc[:, 0:128], krT[:, sl], qrT[:, sl], [krT, qrT], [psc])
                            sT = G(f"rsT{h}", [128, 128], BF16)
                            S.tt('dve', sT[:], psc[:, 0:128], C[f'decT{h}'], ALU.mult, [psc, cb], [sT])
                            yield
                            po = S.P()
                            S.mm(po[:, 0:128], sT[:], vtok[:], [sT, vtok], [po], start=True, stop=False)
                            S.mm(po[:, 0:128], qwT[:], Srb[h][:], [qwT, Srb[h]], [po], start=False, stop=True)
                            S.mm(po[:, 128:256], kw[:], vtok[:], [kw, vtok], [po])
                            S.stt(Sr[h][:], Sr[h][:], CD[h], po[:, 128:256], ALU.mult, ALU.add, [Sr[h], po], [Sr[h]])
                            osb = G(f"rosb{h}", [128, 128], F32)
                            S.cp('act', osb[:], po[:, 0:128], [po], [osb])
                            yield
                            S.cp('pool', Srb[h][:], Sr[h][:], [Sr[h]], [Srb[h]])
                            junk = G(f"rjunk{h}", [128, 128], F32)
                            ssq = G(f"rssq{h}", [128, 4], F32)
                            S.op('pool', lambda e, ssq=ssq: e.memset(ssq[:], 0.0), [], [ssq])
                            yield
                            S.act(junk[:], osb[:], AF.Square, [osb], [junk, ssq], accum_out=ssq[:, 0:1])
                            yield
                            S.act(ssq[:, 1:2], ssq[:, 0:1], AF.Ln, [ssq, cc], [ssq], bias=cc[:, 2:3], scale=1.0 / 128)
                            yield
                            S.act(ssq[:, 2:3], ssq[:, 1:2], AF.Exp, [ssq], [ssq], scale=-0.5)
                            yield
                            on = G(f"ron{h}", [128, 128], F32)
                            S.ts('dve', on[:], osb[:], ssq[:, 2:3], ALU.mult, [osb, ssq], [on])
                            yield
                            pt = S.P()
                            S.tr(pt[:, 0:128], on[:], C['ident'], [on, cb], [pt])
                            loc = (sb_ - NFULL0) * SBW + tt * 128
                            S.tt('dve', catT[:, 2 * h, loc:loc + 128], pt[:, 0:128], sgT[:, sl], ALU.mult, [pt, sgT], [catT])
                            yield

                        full = sb >= NFULL0
                        rr([ret_prep(h, full) for h in range(NH)])
                        for tt in range(NT):
                            rr([ret_chain(tt, h, sb, full) for h in range(NH)])

                    if phase == 'gdn':
                        def gdn_prep(h, w_, p, sb_):
                            nm = ("q", "k", "v")[w_]
                            base = NH * 6 * 128 + h * 4 * 128
                            cbf = cbuf[h][w_]
                            ps = proj(hT, base + w_ * 128)
                            S.act(cbf[:, 3:3 + SBW], ps[:, 0:SBW], AF.Identity, [ps, vm], [cbf], scale=vm[:, sb_:sb_ + 1])
                            yield
                            acc = G(f"gacc{h}_{w_}", [128, SBW], F32)
                            wc = h * 12 + w_ * 4
                            S.act(acc[:], cbf[:, 0:SBW], AF.Identity, [cbf, cw], [acc], scale=cw[:, wc:wc + 1])
                            yield
                            for j in range(1, 4):
                                S.stt(acc[:], cbf[:, j:j + SBW], cw[:, wc + j:wc + j + 1], acc[:], ALU.mult, ALU.add, [cbf, cw, acc], [acc])
                                yield
                            tl = G(f"gtail{h}_{w_}", [128, 4], F32)
                            S.cp('pool', tl[:, 0:3], cbf[:, SBW:SBW + 3], [cbf], [tl])
                            yield
                            S.cp('pool', cbf[:, 0:3], tl[:, 0:3], [tl], [cbf])
                            yield
                            if nm == "v":
                                vT = G(f"gvT{h}_{p}", [128, SBW], BF16)
                                S.act(vT[:], acc[:], AF.Silu, [acc], [vT])
                                yield
                            else:
                                y = acc
                                S.act(y[:], acc[:], AF.Silu, [acc], [y])
                                yield
                                sq = G(f"gsq{h}_{w_}", [128, SBW], BF16)
                                S.act(sq[:], y[:], AF.Square, [y], [sq])
                                yield
                                pn = S.P()
                                S.mm(pn[:, 0:SBW], ones_b[:], sq[:], [ones_b, sq], [pn])
                                rn = G(f"grn{h}_{w_}", [128, SBW], F32)
                                S.act(rn[:], pn[:, 0:SBW], AF.Ln, [pn, cc], [rn], bias=cc[:, 2:3], scale=1.0)
                                yield
                                S.act(rn[:], rn[:], AF.Exp, [rn], [rn], scale=-0.5)
                                yield
                                o_ = G(f"g{nm}nT{h}_{p}", [128, SBW], BF16)
                                if nm == "q":
                                    S.stt(o_[:], y[:], 128 ** -0.5, rn[:], ALU.mult, ALU.mult, [y, rn], [o_])
                                else:
                                    S.tt('pool', o_[:], y[:], rn[:], ALU.mult, [y, rn], [o_])
                                yield

                        def gdn_prep_z(h, p):
                            base = NH * 6 * 128 + h * 4 * 128
                            pz = proj(hT, base + 3 * 128)
                            szT = G(f"gszT{h}_{p}", [128, SBW], F32)
                            S.act(szT[:], pz[:, 0:SBW], AF.Silu, [pz], [szT])
                            yield

                        def gdn_scal(tt, p, sb_):
                            sl = slice(tt * 128, (tt + 1) * 128)
                            sc = G(f"gsc{tt}_{p}", [128, 64], F32)
                            pab = S.P()
                            for c in range(8):
                                S.mm(pab[:, 0:2 * NH], hT[:, c, sl], Wab[:, c, :], [hT, Wab], [pab], start=(c == 0), stop=(c == 7))
                            S.tt('dve', sc[:, 0:4], pab[:, 0:NH], hvt[:, NH:2 * NH], ALU.add, [pab, hvt], [sc])
                            S.act(sc[:, 16:20], pab[:, NH:2 * NH], AF.Exp, [pab], [sc], scale=-1.0)
                            yield
                            S.act(sc[:, 4:8], sc[:, 0:4], AF.Exp, [sc], [sc])
                            yield
                            S.act(sc[:, 8:12], sc[:, 4:8], AF.Ln, [sc], [sc], bias=1.0)
                            yield
                            S.tt('dve', sc[:, 12:16], sc[:, 8:12], nea[:], ALU.mult, [sc, nea], [sc])
                            yield
                            S.ts('dve', sc[:, 16:20], sc[:, 16:20], 1.0, ALU.add, [sc], [sc])
                            yield
                            S.op('dve', lambda e, sc=sc: e.reciprocal(out=sc[:, 20:24], in_=sc[:, 16:20]), [sc], [sc])
                            yield
                            S.ts('dve', sc[:, 20:24], sc[:, 20:24], vm[:, sb_:sb_ + 1], ALU.mult, [sc, vm], [sc])
                            yield
                            pgc = S.P()
                            S.mm(pgc[:, 0:NH], C['triU'], sc[:, 12:16], [cb, sc], [pgc])
                            S.mm(pgc[:, 8:8 + NH], ones_f[:], sc[:, 12:16], [ones_f, sc], [pgc])
                            S.cp('dve', sc[:, 24:28], pgc[:, 0:NH], [pgc], [sc])
                            S.cp('dve', sc[:, 28:32], pgc[:, 8:8 + NH], [pgc], [sc])
                            yield
                            S.act(sc[:, 32:40], sc[:, 24:32], AF.Exp, [sc], [sc])
                            yield
                            S.tt('dve', sc[:, 40:44], sc[:, 28:32], sc[:, 24:28], ALU.subtract, [sc], [sc])
                            yield
                            S.act(sc[:, 44:48], sc[:, 40:44], AF.Exp, [sc], [sc])
                            yield
                            S.tt('dve', sc[:, 48:52], sc[:, 20:24], sc[:, 32:36], ALU.mult, [sc], [sc])
                            yield

                        def chain_pre(ch):
                            key, tt, h, sl, p = ch['key'], ch['tt'], ch['h'], ch['sl'], ch['p']
                            sc = S.cache[f"gsc{tt}_{p}"]
                            ch['sc'] = sc
                            full = ch['full']
                            knT, vT = S.cache[f"gknT{h}_{p}"], S.cache[f"gvT{h}_{p}"]
                            qnT = S.cache[f"gqnT{h}_{p}"] if full else None
                            F = [G(f"gF{i}_{key}", [128, 128], F32) for i in range(4)]
                            Bq = [G(f"gB{i}_{key}", [128, 128], BF16) for i in range(2)]
                            gb = F[0]
                            S.act(gb[:], ones_f[:], AF.Identity, [ones_f, sc], [gb], scale=sc[:, 12 + h:13 + h])
                            pT = S.P()
                            pTb = pT.ap[:].bitcast(BF16)
                            S.tr(pTb[:, 128:256], knT[:, sl], ident_b[:], [knT, ident_b], [pT])
                            S.tr(pTb[:, 256:384], vT[:, sl], ident_b[:], [vT, ident_b], [pT])
                            kbg = G(f"gkbg{key}", [128, 128], BF16)
                            S.act(kbg[:], pTb[:, 128:256], AF.Identity, [pT, sc], [kbg], scale=sc[:, 48 + h:49 + h])
                            kt = G(f"gkt{key}", [128, 128], BF16)
                            S.ts('dve', kt[:], pTb[:, 128:256], sc[:, 44 + h:45 + h], ALU.mult, [pT, sc], [kt])
                            vb = G(f"gvb{key}", [128, 128], BF16)
                            S.act(vb[:], pTb[:, 256:384], AF.Identity, [pT, sc], [vb], scale=sc[:, 20 + h:21 + h])
                            yield
                            pG = S.P()
                            S.mm(pG[:, 0:128], C['triU'], gb[:], [cb, gb], [pG], start=True, stop=False)
                            S.mm(pG[:, 0:128], gb[:], C['negtriU'], [cb, gb], [pG], start=False, stop=True)
                            ex = F[1]
                            S.stt(ex[:], pG[:, 0:128], 0.0, C['negmask'], ALU.min, ALU.add, [pG, cb], [ex])
                            yield
                            dec_i = F[1]
                            S.act(dec_i[:], ex[:], AF.Exp, [ex], [dec_i])
                            yield
                            dec_s = F[2]
                            S.tt('pool', dec_s[:], dec_i[:], C['ident'], ALU.subtract, [dec_i, cb], [dec_s])
                            yield
                            pK = S.P()
                            S.mm(pK[:, 0:128], knT[:, sl], knT[:, sl], [knT], [pK])
                            if full:
                                S.mm(pK[:, 128:256], qnT[:, sl], knT[:, sl], [qnT, knT], [pK])
                            A = G(f"gA{key}", [128, 128], BF16)
                            S.stt(A[:], pK[:, 0:128], sc[:, 20 + h:21 + h], dec_s[:], ALU.mult, ALU.mult, [pK, sc, dec_s], [A])
                            attn = Bq[0]
                            if full:
                                S.tt('dve', attn[:], pK[:, 128:256], dec_i[:], ALU.mult, [pK, dec_i], [attn])
                            yield
                            tm = Bq[1]
                            S.tt('pool', tm[:], A[:], m2Lb[:], ALU.mult, [A, m2Lb], [tm])
                            attnT = G(f"gattnT{key}", [128, 128], BF16)
                            if full:
                                pT2 = S.P()
                                pT2b = pT2.ap[:].bitcast(BF16)
                                S.tr(pT2b[:, 0:128], attn[:], ident_b[:], [attn, ident_b], [pT2])
                                S.cp('act', attnT[:], pT2b[:, 0:128], [pT2], [attnT])
                            yield
                            T1 = G(f"gT0{key}", [128, 128], BF16)
                            S.tt('pool', T1[:], ident_b[:], tm[:], ALU.subtract, [ident_b, tm], [T1])
                            yield
                            pU = S.P()
                            pUb = pU.ap[:].bitcast(BF16)
                            S.tr(pUb[:, 0:128], T1[:], ident_b[:], [T1, ident_b], [pU])
                            U = G(f"gU0{key}", [128, 128], BF16)
                            S.cp('dve', U[:], pUb[:, 0:128], [pU], [U])
                            yield
                            Tk = T1
                            for lvl, bs in enumerate((4, 8, 16, 32, 64, None)):
                                pW = S.P()
                                S.mm(pW[:, 0:128], A[:], U[:], [A, U], [pW])
                                W = Bq[0]
                                S.tt('dve', W[:], pW[:, 0:128], U[:], ALU.add, [pW, U], [W])
                                yield
                                pW2 = S.P()
                                S.mm(pW2[:, 0:128], Tk[:], W[:], [Tk, W], [pW2])
                                Un = G(f"gU{(lvl + 1) % 2}{key}", [128, 128], BF16)
                                if bs is not None:
                                    tmpf = F[0]
                                    S.stt(tmpf[:], U[:], 2.0, pW2[:, 0:128], ALU.mult, ALU.subtract, [U, pW2], [tmpf])
                                    yield
                                    S.tt('pool', Un[:], tmpf[:], C[f'bm{bs}'], ALU.mult, [tmpf, cb], [Un])
                                    yield
                                    pX = S.P()
                                    pXb = pX.ap[:].bitcast(BF16)
                                    S.tr(pXb[:, 0:128], Un[:], ident_b[:], [Un, ident_b], [pX])
                                    Tn = G(f"gT{(lvl + 1) % 2}{key}", [128, 128], BF16)
                                    S.cp('act', Tn[:], pXb[:, 0:128], [pX], [Tn])
                                    yield
                                    Tk = Tn
                                else:
                                    S.stt(Un[:], U[:], 2.0, pW2[:, 0:128], ALU.mult, ALU.subtract, [U, pW2], [Un])
                                    yield
                                U = Un
                            pw = S.P()
                            S.mm(pw[:, 0:128], kbg[:], U[:], [kbg, U], [pw])
                            S.mm(pw[:, 128:256], U[:], vb[:], [U, vb], [pw])
                            wT = G(f"gwT{key}", [128, 128], BF16)
                            S.cp('act', wT[:], pw[:, 0:128], [pw], [wT])
                            u = F[3]
                            S.cp('dve', u[:], pw[:, 128:256], [pw], [u])
                            yield
                            ch.update(kt=kt, attnT=attnT, wT=wT, u=u, F=F, Bq=Bq)

                        def chain_scan(ch):
                            key, h, sl, sc, F, Bq = ch['key'], ch['h'], ch['sl'], ch['sc'], ch['F'], ch['Bq']
                            full = ch['full']
                            if not full:
                                p1 = S.P()
                                S.mm(p1[:, 0:128], ch['wT'][:], Sgb[h][:], [ch['wT'], Sgb[h]], [p1])
                                vn = Bq[1]
                                S.tt('dve', vn[:], ch['u'][:], p1[:, 0:128], ALU.subtract, [ch['u'], p1], [vn])
                                yield
                                p2 = S.P()
                                S.mm(p2[:, 0:128], ch['kt'][:], vn[:], [ch['kt'], vn], [p2])
                                S.stt(Sg[h][:], Sg[h][:], sc[:, 36 + h:37 + h], p2[:, 0:128], ALU.mult, ALU.add, [Sg[h], sc, p2], [Sg[h]])
                                yield
                                S.cp('pool', Sgb[h][:], Sg[h][:], [Sg[h]], [Sgb[h]])
                                yield
                                return
                            qnT, szT = S.cache[f"gqnT{h}_{ch['p']}"], S.cache[f"gszT{h}_{ch['p']}"]
                            p1 = S.P()
                            S.mm(p1[:, 0:128], ch['wT'][:], Sgb[h][:], [ch['wT'], Sgb[h]], [p1])
                            S.mm(p1[:, 128:256], qnT[:, sl], Sgb[h][:], [qnT, Sgb[h]], [p1])
                            vn = Bq[1]
                            S.tt('dve', vn[:], ch['u'][:], p1[:, 0:128], ALU.subtract, [ch['u'], p1], [vn])
                            o1 = F[1]
                            S.act(o1[:], p1[:, 128:256], AF.Identity, [p1, sc], [o1], scale=sc[:, 32 + h:33 + h])
                            yield
                            p2 = S.P()
                            S.mm(p2[:, 0:128], ch['kt'][:], vn[:], [ch['kt'], vn], [p2])
                            S.mm(p2[:, 128:256], ch['attnT'][:], vn[:], [ch['attnT'], vn], [p2])
                            S.stt(Sg[h][:], Sg[h][:], sc[:, 36 + h:37 + h], p2[:, 0:128], ALU.mult, ALU.add, [Sg[h], sc, p2], [Sg[h]])
                            o = F[2]
                            S.tt('dve', o[:], o1[:], p2[:, 128:256], ALU.add, [o1, p2], [o])
                            yield
                            S.cp('pool', Sgb[h][:], Sg[h][:], [Sg[h]], [Sgb[h]])
                            junk = F[0]
                            ssq = G(f"gssq{key}", [128, 4], F32)
                            S.op('pool', lambda e, ssq=ssq: e.memset(ssq[:], 0.0), [], [ssq])
                            yield
                            S.act(junk[:], o[:], AF.Square, [o], [junk, ssq], accum_out=ssq[:, 0:1])
                            yield
                            S.act(ssq[:, 1:2], ssq[:, 0:1], AF.Ln, [ssq, cc], [ssq], bias=cc[:, 2:3], scale=1.0 / 128)
                            yield
                            S.act(ssq[:, 2:3], ssq[:, 1:2], AF.Exp, [ssq], [ssq], scale=-0.5)
                            yield
                            on = F[1]
                            S.act(on[:], o[:], AF.Identity, [o, ssq], [on], scale=ssq[:, 2:3])
                            yield
                            pt = S.P()
                            S.tr(pt[:, 0:128], on[:], C['ident'], [on, cb], [pt])
                            loc = (ch['sb'] - NFULL0) * SBW + ch['tt'] * 128
                            S.stt(catT[:, 2 * h + 1, loc:loc + 128], pt[:, 0:128], dnwt[:, 0:1], szT[:, sl], ALU.mult, ALU.mult, [pt, dnwt, szT], [catT])
                            yield

                        def Bgens(p, sb_):
                            full_ = sb_ >= NFULL0
                            return ([gdn_prep(h, w_, p, sb_) for h in range(NH) for w_ in range(3) if (full_ or w_ > 0 or sb_ == NFULL0 - 1)]
                                    + ([gdn_prep_z(h, p) for h in range(NH)] if full_ else [])
                                    + [gdn_scal(tt, p, sb_) for tt in range(NT)])
                        p = sb % 2
                        if sb == 0:
                            rr(Bgens(0, 0))
                        chains = [dict(key=f"{tt}_{h}", tt=tt, h=h, p=p, sb=sb, full=(sb >= NFULL0), sl=slice(tt * 128, (tt + 1) * 128))
                                  for tt in range(NT) for h in range(NH)]
                        gens = [chain_pre(ch) for ch in chains]
                        if sb + 1 < NSB:
                            hT = emitA(sb + 1)
                            gens = gens + Bgens(1 - p, sb + 1)
                        rr(gens)
                        for tt in range(NT):
                            rr([chain_scan(ch) for ch in chains if ch['tt'] == tt])
                print('SBUF remaining in phase', phase, nc.sbuf_bytes_remaining, flush=True)
                S.barrier()
            S.cache = {}
            print('SBUF remaining after phase', phase, nc.sbuf_bytes_remaining, flush=True) if False else None
            if os.environ.get('KSTOP') == phase:
                return nc

        NTB = TS // 128
        NSL = T // TS
        with ExitStack() as esB:
            fnwt = S.sb("fnwt", [128, D], F32, esB)
            S.dma('sp', fnwt[:], fnw[:, :], writes=[fnwt], sembuf=fnwt)
            h2T = catT
            Gt = S.sb("Gt", [128, NTB, 32], F32, esB)
            g12 = S.sb("g12", [128, 2 * D], F32, esB)
            with ExitStack() as esB1:
                S.cache_es = esB1
                G = S.g
                for gi, base in ((0, 16), (1, 40)):
                    for c in range(8):
                        dg = S.g(f"dg{c % 2}", [128, 128], F32)
                        S.ts('dve', dg[:], C['ident'], mod[:, base + c:base + c + 1], ALU.mult, [cb, mod], [dg])
                        pg = S.P()
                        S.mm(pg[:, 0:128], ones_f[:], dg[:], [ones_f, dg], [pg])
                        S.cp('act', g12[:, gi * D + c * 128:gi * D + (c + 1) * 128], pg[:, 0:128], [pg], [g12])
                Wout = S.sb("Wout", [128, 8, D], BF16, esB1)
                S.dma('pool', Wout[:], w_out.rearrange("(c p) n -> p c n", p=128), writes=[Wout], sembuf=Wout)
                Wr = S.sb("Wr", [128, 8, 36], F32, esB1)
                S.dma('sp', Wr[:], w_r.rearrange("(c p) n -> p c n", p=128), writes=[Wr], sembuf=Wr)
                brt = S.sb("brt", [128, 36], F32, esB1)
                S.dma('sp', brt[:], b_r[:, :], writes=[brt], sembuf=brt)
                for tt in range(NTB):
                    sl = slice(tt * 128, (tt + 1) * 128)
                    xst = G(f"xst{tt % 2}", [128, D], F32)
                    S.dma('sp', xst[:], xs[tt * 128:(tt + 1) * 128, :], writes=[xst], sembuf=xst)
                    x1 = G(f"x1_{tt % 2}", [128, D], F32)
                    for half in range(2):
                        hs = slice(half * 512, (half + 1) * 512)
                        pm_ = S.P()
                        for c in range(8):
                            S.mm(pm_[:, :], catT[:, c, sl], Wout[:, c, hs], [catT, Wout], [pm_], start=(c == 0), stop=(c == 7))
                        tmp = G(f"mixg{half}", [128, 512], F32)
                        S.tt('dve', tmp[:], pm_[:, :], g12[:, half * 512:(half + 1) * 512], ALU.mult, [pm_, g12], [tmp])
                        S.tt('pool', x1[:, hs], tmp[:], xst[:, hs], ALU.add, [tmp, xst], [x1])
                    S.dma('sp', x1_d.ap[tt * 128:(tt + 1) * 128, :], x1[:], reads=[x1], writes=[x1_d], sembuf=x1)
                    junk = G("bjunk", [128, D], F32)
                    ssq = G(f"bssq{tt % 2}", [128, 4], F32)
                    S.op('pool', lambda e, ssq=ssq: e.memset(ssq[:], 0.0), [], [ssq])
                    S.act(junk[:], x1[:], AF.Square, [x1], [junk, ssq], accum_out=ssq[:, 0:1])
                    S.rsqrt((ssq[:, 2:3], ssq), ssq[:, 0:1], 1.0 / D, [ssq], (ssq[:, 1:2], ssq))
                    xn = G("bxn", [128, D], F32)
                    S.ts('dve', xn[:], x1[:], ssq[:, 2:3], ALU.mult, [x1, ssq], [xn])
                    h2f = G("h2f", [128, 8, 128], F32)
                    for c in range(8):
                        ptp = S.P()
                        S.tr(ptp[:, 0:128], xn[:, c * 128:(c + 1) * 128], C['ident'], [xn, cb], [ptp])
                        S.act(h2f[:, c, :], ptp[:, 0:128], AF.Identity, [ptp, a2, mod], [h2f], bias=mod[:, 24 + c:25 + c], scale=a2[:, c:c + 1])
                    S.cp('pool', h2T[:, :, sl], h2f[:], [h2f], [h2T])
                    plg = S.P()
                    for c in range(8):
                        S.mm(plg[:, 0:36], h2f[:, c, :], Wr[:, c, :], [h2f, Wr], [plg], start=(c == 0), stop=(c == 7))
                    r = G("rt", [128, 256], F32)
                    S.tt('dve', r[:, 0:36], plg[:, 0:36], brt[:], ALU.add, [plg, brt], [r])
                    S.op('dve', lambda e, r=r: e.reduce_max(out=r[:, 36:37], in_=r[:, 0:4], axis=AX.X), [r], [r])
                    S.ts('dve', r[:, 40:44], r[:, 0:4], r[:, 36:37], ALU.is_equal, [r], [r])
                    S.ts('dve', r[:, 44:48], r[:, 0:4], r[:, 36:37], ALU.subtract, [r], [r])
                    S.act(r[:, 44:48], r[:, 44:48], AF.Exp, [r], [r])
                    S.op('dve', lambda e, r=r: e.reduce_sum(out=r[:, 48:49], in_=r[:, 44:48], axis=AX.X), [r], [r])
                    S.op('dve', lambda e, r=r: e.reciprocal(out=r[:, 49:50], in_=r[:, 48:49]), [r], [r])
                    S.ts('dve', r[:, 44:48], r[:, 40:44], 1.0, ALU.subtract, [r], [r], s2=1e30, op1=ALU.mult)
                    S.tt('dve', r[:, 64:96].rearrange("p (g e) -> p g e", e=8), r[:, 4:36].rearrange("p (g e) -> p g e", e=8),
                         r[:, 44:48].unsqueeze(2).to_broadcast([128, 4, 8]), ALU.add, [r], [r])
                    S.op('dve', lambda e, r=r: e.reduce_max(out=r[:, 96:97], in_=r[:, 64:96], axis=AX.X), [r], [r])
                    S.ts('dve', r[:, 100:132], r[:, 64:96], r[:, 96:97], ALU.is_equal, [r], [r])
                    S.stt(r[:, 132:164], r[:, 100:132], -1e30, r[:, 64:96], ALU.mult, ALU.add, [r], [r])
                    S.op('dve', lambda e, r=r: e.reduce_max(out=r[:, 164:165], in_=r[:, 132:164], axis=AX.X), [r], [r])
                    S.ts('dve', r[:, 168:200], r[:, 132:164], r[:, 164:165], ALU.is_equal, [r], [r])
                    S.tt('dve', r[:, 200:201], r[:, 164:165], r[:, 96:97], ALU.subtract, [r], [r])
                    S.act(r[:, 200:201], r[:, 200:201], AF.Exp, [r], [r])
                    S.ts('dve', r[:, 201:202], r[:, 200:201], 1.0, ALU.add, [r], [r])
                    S.op('dve', lambda e, r=r: e.reciprocal(out=r[:, 202:203], in_=r[:, 201:202]), [r], [r])
                    S.tt('dve', r[:, 202:203], r[:, 202:203], r[:, 49:50], ALU.mult, [r], [r])
                    S.tt('dve', r[:, 203:204], r[:, 202:203], r[:, 200:201], ALU.mult, [r], [r])
                    S.ts('dve', r[:, 204:236], r[:, 100:132], r[:, 202:203], ALU.mult, [r], [r])
                    S.stt(Gt[:, tt, :], r[:, 168:200], r[:, 203:204], r[:, 204:236], ALU.mult, ALU.add, [r], [Gt])
                S.barrier()
            S.cache = {}
            if os.environ.get('KSTOP') == 'b1':
                return nc
            acc = S.sb("acc", [128, NTB, D], F32, esB)
            S.op('pool', lambda e: e.memset(acc[:], 0.0), [], [acc])
            with ExitStack() as esB2:
                S.cache_es = esB2
                G = S.g
                NB = max(1, TS // 512)
                BW = min(512, TS)
                for ex_ in range(NE):
                    w1t = G(f"w1t{ex_ % 2}", [128, 8, DEXP], BF16)
                    w3t = G(f"w3t{ex_ % 2}", [128, 8, DEXP], BF16)
                    w2t = G(f"w2t{ex_ % 2}", [128, 4, D], BF16)
                    S.dma('pool', w1t[:], w1[ex_].rearrange("(c p) n -> p c n", p=128), writes=[w1t], sembuf=w1t)
                    S.dma('pool', w3t[:], w3[ex_].rearrange("(c p) n -> p c n", p=128), writes=[w3t], sembuf=w3t)
                    S.dma('pool', w2t[:], w2[ex_].rearrange("(c p) n -> p c n", p=128), writes=[w2t], sembuf=w2t)
                    for tb in range(NB):
                        bsl = slice(tb * BW, (tb + 1) * BW)
                        hid = G(f"hid{tb % 2}", [128, 4, BW], BF16)
                        for hc in range(4):
                            p1 = S.P()
                            for c in range(8):
                                S.mm(p1[:, 0:BW], w1t[:, c, hc * 128:(hc + 1) * 128], h2T[:, c, bsl], [w1t, h2T], [p1], start=(c == 0), stop=(c == 7))
                            p3 = S.P()
                            for c in range(8):
                                S.mm(p3[:, 0:BW], w3t[:, c, hc * 128:(hc + 1) * 128], h2T[:, c, bsl], [w3t, h2T], [p3], start=(c == 0), stop=(c == 7))
                            sl_ = G(f"silu{hc % 2}", [128, BW], F32)
                            S.act(sl_[:], p1[:, 0:BW], AF.Silu, [p1], [sl_])
                            S.tt('dve', hid[:, hc, :], sl_[:], p3[:, 0:BW], ALU.mult, [sl_, p3], [hid])
                        for t4 in range(BW // 128):
                            tt = tb * (BW // 128) + t4
                            for half in range(2):
                                py = S.P()
                                for hc in range(4):
                                    S.mm(py[:, :], hid[:, hc, t4 * 128:(t4 + 1) * 128], w2t[:, hc, half * 512:(half + 1) * 512], [hid, w2t], [py],
                                         start=(hc == 0), stop=(hc == 3))
                                S.stt(acc[:, tt, half * 512:(half + 1) * 512], py[:, :], Gt[:, tt, ex_:ex_ + 1], acc[:, tt, half * 512:(half + 1) * 512],
                                      ALU.mult, ALU.add, [py, Gt, acc], [acc])
                S.barrier()
            S.cache = {}
            if os.environ.get('KSTOP') == 'b2':
                return nc
            with ExitStack() as esB3:
                S.cache_es = esB3
                G = S.g
                for tt in range(NTB):
                    x1 = G(f"fx1_{tt % 2}", [128, D], F32)
                    S.dma('sp', x1[:], x1_d.ap[tt * 128:(tt + 1) * 128, :], reads=[x1_d], writes=[x1], sembuf=x1)
                    x2 = G(f"fx2_{tt % 2}", [128, D], F32)
                    S.tt('pool', x2[:], acc[:, tt, :], g12[:, D:2 * D], ALU.mult, [acc, g12], [x2])
                    S.tt('dve', x2[:], x2[:], x1[:], ALU.add, [x2, x1], [x2])
                    junk = G("fjunk", [128, D], F32)
                    ssq = G(f"fssq{tt % 2}", [128, 4], F32)
                    S.op('pool', lambda e, ssq=ssq: e.memset(ssq[:], 0.0), [], [ssq])
                    S.act(junk[:], x2[:], AF.Square, [x2], [junk, ssq], accum_out=ssq[:, 0:1])
                    S.rsqrt((ssq[:, 2:3], ssq), ssq[:, 0:1], 1.0 / D, [ssq], (ssq[:, 1:2], ssq))
                    ot = G(f"fot{tt % 2}", [128, D], F32)
                    S.stt(ot[:], x2[:], ssq[:, 2:3], fnwt[:], ALU.mult, ALU.mult, [x2, ssq, fnwt], [ot])
                    S.dma('sp', out[tt * 128:(tt + 1) * 128, :], ot[:], reads=[ot], sembuf=ot)
                S.barrier()
        print("ninst", S.ninst, "nsem", S.nsem, flush=True)
    return nc


def _host_inputs(inp, T, TS):
    NH = NHEAD
    NE_ = int(os.environ.get('KNEXP', NEXP))
    f = lambda a: np.ascontiguousarray(np.asarray(a), dtype=np.float32)
    x = f(inp['x'])
    B = x.shape[0]
    w_in = f(inp['w_in'])[0]
    swap = np.concatenate([np.arange(64, 128), np.arange(0, 64)])
    cols = []
    for h in range(NH):
        q = np.arange(h * 128, (h + 1) * 128)
        k = 512 + q
        v = 1024 + q
        g = 1536 + q
        cols += [q, q[swap], k, k[swap], v, g]
    for h in range(NH):
        q = 2048 + np.arange(h * 128, (h + 1) * 128)
        cols += [q, q + 512, q + 1024, q + 1536]
    cols = np.concatenate(cols)
    w_inA = np.ascontiguousarray(w_in[:, cols])
    w_ab = np.ascontiguousarray(w_in[:, 4096:4104])
    conv = f(inp['conv_w'])[0]
    convw = np.zeros((128, NH * 12), np.float32)
    for h in range(NH):
        for w_ in range(3):
            ch = w_ * 512 + h * 128 + np.arange(128)
            convw[:, h * 12 + w_ * 4:h * 12 + w_ * 4 + 4] = conv[:, ch].T
    hv = np.broadcast_to(np.concatenate([f(inp['a_log'])[0], f(inp['dt_bias'])[0]])[None, :], (128, 2 * NH)).copy()
    dnw = f(inp['dn_norm_w'])[0].reshape(128, 1).copy()
    w_out = f(inp['w_out'])[0]
    rows = []
    for h in range(NH):
        rows += [np.arange(h * 128, (h + 1) * 128), 512 + np.arange(h * 128, (h + 1) * 128)]
    w_outP = np.ascontiguousarray(w_out[np.concatenate(rows), :])
    w_r = np.ascontiguousarray(np.concatenate([f(inp['w_group'])[0], f(inp['w_expert'])[0]], axis=1))
    b_r = np.broadcast_to(np.concatenate([f(inp['b_group'])[0], f(inp['b_expert'])[0]])[None, :], (128, 36)).copy()
    n12 = np.concatenate([f(inp['norm1_w'])[0].reshape(8, 128).T, f(inp['norm2_w'])[0].reshape(8, 128).T], axis=1).copy()
    b_adaT = np.ascontiguousarray(f(inp['b_ada'])[0].reshape(48, 128).T)
    fnw = np.broadcast_to(f(inp['final_norm_w'])[None, :], (128, D)).copy()
    shared = dict(w_ada=f(inp['w_ada'])[0], b_adaT=b_adaT, n12=n12, w_inA=w_inA, w_ab=w_ab, convw=convw, hv=hv, dnw=dnw,
                  w_out=w_outP, w_r=w_r, b_r=b_r, w1=f(inp['w1'])[0][:NE_], w3=f(inp['w3'])[0][:NE_], w2=f(inp['w2'])[0][:NE_], fnw=fnw,
                  cbig=CBIG, ccol=CCOL)
    pos = np.asarray(inp['positions']).astype(np.int32)
    c = f(inp['c'])
    maps = []
    nsl = T // TS
    for core in range(B * nsl):
        b, s = core // nsl, core % nsl
        m = dict(shared)
        npad = (nsl - 1 - s) * TS
        xTp = np.zeros((D, T), np.float32)
        xTp[:, npad:] = x[b, :(s + 1) * TS, :].T
        m['xT'] = xTp
        SBW_ = min(256, TS)
        vmk = np.zeros((128, T // SBW_), np.float32)
        vmk[:, npad // SBW_:] = 1.0
        m['vmask'] = vmk
        m['xs'] = np.ascontiguousarray(x[b, s * TS:(s + 1) * TS, :])
        posp = np.zeros((1, T), np.int32)
        posp[0, npad:] = pos[b, :(s + 1) * TS]
        m['pos'] = posp
        m['cT'] = np.ascontiguousarray(c[b].reshape(8, 128).T)
        sm = np.zeros((128, 8), np.float32)
        sm[:, s] = 1.0
        m['selm'] = sm
        maps.append(m)
    return maps


_NC_CACHE = {}


def _run(inp, T, TS):
    key = (T, TS)
    if key not in _NC_CACHE:
        _NC_CACHE[key] = build(T, TS, SBW=min(256, TS))
    nc = _NC_CACHE[key]
    maps = _host_inputs(inp, T, TS)
    res = run_bass_kernel_spmd(nc, maps, core_ids=list(range(len(maps))))
    B = np.asarray(inp['x']).shape[0]
    nsl = T // TS
    out = np.zeros((B, T, D), np.float32)
    for core in range(B * nsl):
        b, s = core // nsl, core % nsl
        out[b, s * TS:(s + 1) * TS, :] = np.asarray(res.results[core]['out'])
    return out


def kernel(**inputs):
    return _run(inputs, 8192, 2048)
```

```python
import math
import os
from contextlib import ExitStack
import numpy as np
import concourse.bass as bass
import concourse.mybir as mybir
from concourse.bass_utils import run_bass_kernel_spmd

F32 = mybir.dt.float32
BF16 = mybir.dt.bfloat16
I32 = mybir.dt.int32
AF = mybir.ActivationFunctionType
ALU = mybir.AluOpType
AX = mybir.AxisListType

D = 1024
NHEAD = 4
EPS = 1e-6
NEXP = 32
DEXP = 512
TWO_PI = 2.0 * math.pi


class Buf:
    def __init__(self, ap, name):
        self.ap = ap
        self.name = name
        self.ws = {}
        self.rs = {}
        self.dsem = None
        self.dcnt = 0
        self.psum = False

    def __getitem__(self, k):
        return self.ap[k]


class Sched:
    EPOCH = 30000

    def __init__(self, nc, es):
        self.nc = nc
        self.es = es
        self.eng = {'pe': nc.tensor, 'act': nc.scalar, 'dve': nc.vector, 'pool': nc.gpsimd, 'sp': nc.sync}
        self.sem = {}
        self.cnt = {}
        self.waited = {e: {} for e in self.eng}
        self.nsem = 0
        self.allsems = {}
        for e in self.eng:
            self._newsem(e)
        self.bufs = []
        self.ninst = {e: 0 for e in self.eng}
        self.cache = {}
        self.pbanks = []
        self.pi = 0

    def _mksem(self, name):
        self.nsem += 1
        s = self.es.enter_context(self.nc.semaphore(name))
        return s

    def _newsem(self, e):
        self.sem[e] = self._mksem(f"c_{e}_{self.nsem}")
        self.cnt[e] = 0

    def sb(self, name, shape, dt, es=None):
        self.uid = getattr(self, 'uid', 0) + 1
        name = f"s{self.uid}_{name}"
        t = (es or self.es).enter_context(self.nc.sbuf_tensor(name, list(shape), dt))
        b = Buf(t, name)
        self.bufs.append(b)
        return b

    def g(self, name, shape, dt):
        if name not in self.cache:
            self.cache[name] = self.sb(name, shape, dt, es=self.cache_es)
        return self.cache[name]

    def mkpsum(self):
        for i in range(8):
            t = self.es.enter_context(self.nc.psum_tensor(f"pb{i}", [128, 512], F32))
            b = Buf(t, f"pb{i}")
            b.psum = True
            self.bufs.append(b)
            self.pbanks.append(b)

    def P(self):
        b = self.pbanks[self.pi % 8]
        self.pi += 1
        return b

    def dram(self, name, shape, dt, kind="Internal"):
        t = self.nc.dram_tensor(name, list(shape), dt, kind=kind).ap()
        b = Buf(t, name)
        self.bufs.append(b)
        return b

    def _deps(self, reads, writes, e=None):
        deps = {}

        def add(d):
            for k, (s, v) in d.items():
                if k not in deps or deps[k][1] < v:
                    deps[k] = (s, v)
        for b in reads:
            add(b.ws)
            if b.psum:
                own = id(self.sem[e]) if e in self.sem else None
                add({k: v for k, v in b.rs.items() if k != own})
        for b in writes:
            add(b.ws)
            add(b.rs)
        return deps

    def _wait(self, e, deps):
        for k, (s, v) in deps.items():
            if e == 'pe' and s is self.sem['pe']:
                continue
            if self.waited[e].get(k, 0) >= v:
                continue
            self.eng[e].wait_ge(s, v)
            self.ninst[e] += 1
            self.waited[e][k] = v

    def _record(self, ev, reads, writes):
        k = id(ev[0])
        for b in reads:
            if k not in b.rs or b.rs[k][1] < ev[1]:
                b.rs[k] = ev
        for b in writes:
            if k not in b.ws or b.ws[k][1] < ev[1]:
                b.ws[k] = ev

    def op(self, e, fn, reads=(), writes=()):
        if e == 'pool' and os.environ.get('KNOPOOL'):
            e = 'dve'
        self._wait(e, self._deps(reads, writes, e))
        ins = fn(self.eng[e])
        if self.cnt[e] >= self.EPOCH:
            self._newsem(e)
        self.cnt[e] += 1
        ins.then_inc(self.sem[e], 1)
        self.ninst[e] += 1
        self._record((self.sem[e], self.cnt[e]), reads, writes)
        return ins

    def dma(self, q, out_ap, in_ap, reads=(), writes=(), sembuf=None, **kw):
        self._wait(q, self._deps(reads, writes))
        if sembuf.dsem is None:
            sembuf.dsem = self._mksem(f"d_{sembuf.name}")
        ins = self.eng[q].dma_start(out=out_ap, in_=in_ap, **kw)
        sembuf.dcnt += 1
        ins.then_inc(sembuf.dsem, 16)
        self.ninst[q] += 1
        self._record((sembuf.dsem, 16 * sembuf.dcnt), reads, writes)
        return ins

    def barrier(self, engines=('pe', 'act', 'dve', 'pool', 'sp')):
        deps = {}
        for b in self.bufs:
            for d in (b.ws, b.rs):
                for k, (s, v) in d.items():
                    if k not in deps or deps[k][1] < v:
                        deps[k] = (s, v)
        for e in engines:
            self._wait(e, deps)

    def act(self, out, in_, func, reads, writes, **kw):
        return self.op('act', lambda e: e.activation(out=out, in_=in_, func=func, **kw), reads, writes)

    def tt(self, eng, out, a, b, op, reads, writes):
        return self.op(eng, lambda e: e.tensor_tensor(out=out, in0=a, in1=b, op=op), reads, writes)

    def ts(self, eng, out, a, s1, op0, reads, writes, s2=None, op1=None):
        if op1 is None:
            return self.op(eng, lambda e: e.tensor_scalar(out=out, in0=a, scalar1=s1, scalar2=None, op0=op0), reads, writes)
        return self.op(eng, lambda e: e.tensor_scalar(out=out, in0=a, scalar1=s1, scalar2=s2, op0=op0, op1=op1), reads, writes)

    def stt(self, out, a, sc, b, op0, op1, reads, writes):
        return self.op('dve', lambda e: e.scalar_tensor_tensor(out=out, in0=a, scalar=sc, in1=b, op0=op0, op1=op1), reads, writes)

    def cp(self, eng, out, in_, reads, writes):
        if eng == 'act':
            return self.act(out, in_, AF.Copy, reads, writes)
        return self.op(eng, lambda e: e.tensor_copy(out=out, in_=in_), reads, writes)

    def mm(self, out, lhsT, rhs, reads, writes, start=True, stop=True):
        return self.op('pe', lambda e: e.matmul(out, lhsT=lhsT, rhs=rhs, start=start, stop=stop), reads, writes)

    def tr(self, out, in_, ident, reads, writes):
        return self.op('pe', lambda e: e.transpose(out=out, in_=in_, identity=ident), reads, writes)

    def rsqrt(self, out, in_, scale, reads_in, tmp, eps=EPS):
        self.act(tmp[0], in_, AF.Ln, list(reads_in) + [self.epsbuf], [tmp[1]], bias=self.epsc[:in_.shape[0], 0:1], scale=scale)
        self.act(out[0], tmp[0], AF.Exp, [tmp[1]], [out[1]], scale=-0.5)


def rr(gens):
    gens = list(gens)
    while gens:
        nxt = []
        for g_ in gens:
            try:
                next(g_)
                nxt.append(g_)
            except StopIteration:
                pass
        gens = nxt


def _consts():
    i = np.arange(128)
    c = {}
    c['ident'] = np.eye(128, dtype=np.float32)
    c['triU'] = (i[:, None] <= i[None, :]).astype(np.float32)
    c['negtriU'] = -c['triU']
    c['negmask'] = np.where(i[None, :] <= i[:, None], 0.0, -1e30).astype(np.float32)
    c['m2L'] = ((i[:, None] == i[None, :] + 1) & (i[:, None] % 2 == 1)).astype(np.float32)
    for s in (4, 8, 16, 32, 64):
        c[f'bm{s}'] = ((i[:, None] // s) == (i[None, :] // s)).astype(np.float32)
    gam = 1.0 - np.power(2.0, -5.0 - np.arange(NHEAD))
    lg = np.log1p(-np.power(2.0, -5.0 - np.arange(NHEAD, dtype=np.float64)))
    rel = i[None, :] - i[:, None]
    for h in range(NHEAD):
        c[f'decT{h}'] = (np.where(rel >= 0, np.exp(lg[h] * np.maximum(rel, 0)), 0.0) * 128 ** -0.5).astype(np.float32)
        c[f'qdec{h}'] = np.broadcast_to(np.exp(lg[h] * (i + 1.0))[None, :], (128, 128)).astype(np.float32).copy()
    kws = np.stack([np.exp(lg[h] * (127 - i)) * 128 ** -0.5 for h in range(NHEAD)], axis=1)
    cd = [float(np.exp(lg[h] * 128)) for h in range(NHEAD)]
    invf = (10000.0 ** (-(np.arange(0, 128, 2, dtype=np.float32)) / 128.0)).astype(np.float32)
    col = np.zeros((128, 8), np.float32)
    col[:, 0] = np.concatenate([invf, invf])
    col[:, 1] = np.concatenate([-np.ones(64), np.ones(64)])
    col[:, 2] = EPS
    col[:, 3] = math.pi / 2
    col[:, 4:8] = kws
    names = ['ident', 'triU', 'negtriU', 'negmask', 'm2L', 'bm4', 'bm8', 'bm16', 'bm32', 'bm64'] + \
            [f'decT{h}' for h in range(NHEAD)] + [f'qdec{h}' for h in range(NHEAD)]
    big = np.concatenate([c[n] for n in names], axis=1).astype(np.float32)
    return names, big, col, cd


CNAMES, CBIG, CCOL, CD = _consts()


def build(T, TS, SBW=256, dbg=False):
    NH = NHEAD
    NSB = T // SBW
    NT = SBW // 128
    NCOL = NH * 10 * 128
    NE = int(os.environ.get('KNEXP', NEXP))
    nc = bass.Bass("TRN2", target_bir_lowering=False)
    es = ExitStack()
    with es:
        S = Sched(nc, es)
        S.mkpsum()
        dt_in = {}

        def din(name, shape, dt=F32):
            dt_in[name] = nc.dram_tensor(name, list(shape), dt, kind="ExternalInput").ap()
            return dt_in[name]
        xT = din("xT", [D, T])
        xs = din("xs", [TS, D])
        pos = din("pos", [1, T], I32)
        cT = din("cT", [128, 8])
        w_ada = din("w_ada", [D, 6 * D])
        b_adaT = din("b_adaT", [128, 48])
        n12 = din("n12", [128, 16])
        w_inA = din("w_inA", [D, NCOL])
        w_ab = din("w_ab", [D, 2 * NH])
        convw = din("convw", [128, NH * 12])
        hv = din("hv", [128, 2 * NH])
        dnw = din("dnw", [128, 1])
        w_out = din("w_out", [D, D])
        w_r = din("w_r", [D, 36])
        b_r = din("b_r", [128, 36])
        w1 = din("w1", [NE, D, DEXP])
        w3 = din("w3", [NE, D, DEXP])
        w2 = din("w2", [NE, DEXP, D])
        fnw = din("fnw", [128, D])
        cbig = din("cbig", [128, CBIG.shape[1]])
        ccol = din("ccol", [128, 8])
        selm_in = din("selm", [128, 8])
        vmask_in = din("vmask", [128, NSB])
        out = nc.dram_tensor("out", [TS, D], F32, kind="ExternalOutput").ap()
        x1_d = S.dram("x1_d", [TS, D], F32)
        dbg_outs = {}

        cb = S.sb("cbig", [128, CBIG.shape[1]], F32)
        S.dma('sp', cb[:], cbig[:, :], writes=[cb], sembuf=cb)
        cc = S.sb("ccol", [128, 8], F32)
        S.dma('sp', cc[:], ccol[:, :], writes=[cc], sembuf=cc)
        S.epsc = cc.ap[:, 2:3]
        S.epsbuf = cc
        C = {n: cb.ap[:, k * 128:(k + 1) * 128] for k, n in enumerate(CNAMES)}
        ident_b = S.sb("ident_b", [128, 128], BF16)
        S.cp('dve', ident_b[:], C['ident'], [cb], [ident_b])
        ones_b = S.sb("ones_b", [128, 128], BF16)
        S.op('pool', lambda e: e.memset(ones_b[:], 1.0), [], [ones_b])
        ones_f = S.sb("ones_f", [128, 128], F32)
        S.op('pool', lambda e: e.memset(ones_f[:], 1.0), [], [ones_f])
        bmb = {}
        for s_ in (4, 8, 16, 32, 64):
            bmb[s_] = S.sb(f"bmb{s_}", [128, 128], BF16)
            S.cp('dve', bmb[s_][:], C[f'bm{s_}'], [cb], [bmb[s_]])
        m2Lb = S.sb("m2Lb", [128, 128], BF16)
        S.cp('dve', m2Lb[:], C['m2L'], [cb], [m2Lb])
        selm = S.sb("selm", [128, 8], F32)
        S.dma('sp', selm[:], selm_in[:, :], writes=[selm], sembuf=selm)
        vm = S.sb("vmask", [128, NSB], F32)
        S.dma('sp', vm[:], vmask_in[:, :], writes=[vm], sembuf=vm)
        NFULL0 = NSB - TS // SBW
        catT = S.sb("catT", [128, 8, TS], BF16)
        S.op('pool', lambda e: e.memset(catT[:], 0.0), [], [catT])
        mod = S.sb("mod", [128, 48], F32)
        a1 = S.sb("a1", [128, 8], F32)
        a2 = S.sb("a2", [128, 8], F32)

        with ExitStack() as es0:
            S.cache_es = es0
            ct = S.sb("ct", [128, 8], F32, es0)
            S.dma('sp', ct[:], cT[:, :], writes=[ct], sembuf=ct)
            sct = S.sb("sct", [128, 8], F32, es0)
            S.act(sct[:], ct[:], AF.Silu, [ct], [sct])
            bad = S.sb("bad", [128, 48], F32, es0)
            S.dma('sp', bad[:], b_adaT[:, :], writes=[bad], sembuf=bad)
            n12t = S.sb("n12t", [128, 16], F32, es0)
            S.dma('sp', n12t[:], n12[:, :], writes=[n12t], sembuf=n12t)
            pm = S.P()
            for j in range(6):
                wa = S.g(f"wada{j % 2}", [128, 8, D], F32)
                S.dma('sp', wa[:], w_ada[:, j * D:(j + 1) * D].rearrange("(c p) n -> p c n", p=128), writes=[wa], sembuf=wa)
                for oc in range(8):
                    col_ = j * 8 + oc
                    for c in range(8):
                        S.mm(pm[:, col_:col_ + 1], wa[:, c, oc * 128:(oc + 1) * 128], sct[:, c:c + 1], [wa, sct], [pm],
                             start=(c == 0), stop=(c == 7))
            S.tt('dve', mod[:], pm[:, 0:48], bad[:], ALU.add, [pm, bad], [mod])
            S.stt(a1[:], mod[:, 8:16], 1.0, n12t[:, 0:8], ALU.add, ALU.mult, [mod, n12t], [a1])
            S.stt(a2[:], mod[:, 32:40], 1.0, n12t[:, 8:16], ALU.add, ALU.mult, [mod, n12t], [a2])
            S.barrier()
        S.cache = {}
        if os.environ.get('KSTOP') == 'pre':
            return nc

        for phase in ('ret', 'gdn'):
            with ExitStack() as esA:
                S.cache_es = esA
                G = S.g
                if phase == 'ret':
                    PC0, PCN = 0, NH * 6 * 128
                else:
                    PC0, PCN = NH * 6 * 128, NH * 4 * 128
                Win = S.sb("Win" + phase, [128, 8, PCN], BF16, esA)
                for c in range(8):
                    S.dma('pool', Win[:, c, :], w_inA[c * 128:(c + 1) * 128, PC0:PC0 + PCN], writes=[Win], sembuf=Win)
                Wab = S.sb("Wab" + phase, [128, 8, 2 * NH], BF16, esA)
                S.dma('pool', Wab[:], w_ab.rearrange("(c p) n -> p c n", p=128), writes=[Wab], sembuf=Wab)
                cw = S.sb("cw" + phase, [128, NH * 12], F32, esA)
                S.dma('sp', cw[:], convw[:, :], writes=[cw], sembuf=cw)
                hvt = S.sb("hvt" + phase, [128, 2 * NH], F32, esA)
                S.dma('sp', hvt[:], hv[:, :], writes=[hvt], sembuf=hvt)
                dnwt = S.sb("dnwt" + phase, [128, 1], F32, esA)
                S.dma('sp', dnwt[:], dnw[:, :], writes=[dnwt], sembuf=dnwt)
                nea = S.sb("nea" + phase, [128, NH], F32, esA)
                S.act(nea[:], hvt[:, 0:NH], AF.Exp, [hvt], [nea])
                S.ts('dve', nea[:], nea[:], -1.0, ALU.mult, [nea], [nea])
                Sr, Srb, Sg, Sgb, cbuf = [], [], [], [], []
                for h in range(NH):
                    for lst, nm, dt in ((Sr, "Sr", F32), (Srb, "Srb", BF16), (Sg, "Sg", F32), (Sgb, "Sgb", BF16)):
                        b = S.sb(f"{nm}{h}{phase}", [128, 128], dt, esA)
                        S.op('pool', lambda e, b=b: e.memset(b[:], 0.0), [], [b])
                        lst.append(b)
                    row = []
                    for w_ in range(3):
                        b = S.sb(f"cbuf{h}_{w_}{phase}", [128, SBW + 3], F32, esA)
                        S.op('pool', lambda e, b=b: e.memset(b[:], 0.0), [], [b])
                        row.append(b)
                    cbuf.append(row)

                def proj(hT, col):
                    ps = S.P()
                    for c in range(8):
                        S.mm(ps[:, 0:SBW], Win[:, c, col - PC0:col - PC0 + 128], hT[:, c, :], [Win, hT], [ps], start=(c == 0), stop=(c == 7))
                    return ps

                def emitA(sb_):
                    t0 = sb_ * SBW
                    xt = G("xt", [128, 8, SBW], F32)
                    S.dma('sp', xt[:], xT[:, t0:t0 + SBW].rearrange("(c p) t -> p c t", p=128), writes=[xt], sembuf=xt)
                    xsq = G("xsq", [128, 8, SBW], BF16)
                    S.act(xsq[:], xt[:], AF.Square, [xt], [xsq])
                    pss = S.P()
                    for c in range(8):
                        S.mm(pss[:, 0:SBW], ones_b[:], xsq[:, c, :], [ones_b, xsq], [pss], start=(c == 0), stop=(c == 7))
                    lnt = G("lnt", [128, SBW], F32)
                    rstd = G("rstd", [128, SBW], F32)
                    S.rsqrt((rstd[:], rstd), pss[:, 0:SBW], 1.0 / D, [pss], (lnt[:], lnt))
                    hT = G("hT", [128, 8, SBW], BF16)
                    for c in range(8):
                        tmp = G(f"xn{c % 2}", [128, SBW], F32)
                        S.tt('dve' if c % 2 == 0 else 'pool', tmp[:], xt[:, c, :], rstd[:], ALU.mult, [xt, rstd], [tmp])
                        S.act(hT[:, c, :], tmp[:], AF.Identity, [tmp, a1, mod], [hT], bias=mod[:, c:c + 1], scale=a1[:, c:c + 1])
                    return hT

                for sb in range(NSB):
                    t0 = sb * SBW
                    if phase == 'ret' or sb == 0:
                        hT = emitA(sb)
                    if os.environ.get('KSTOP') == 'ret1':
                        S.barrier(engines=('sp',))
                        return nc
                    if phase == 'ret':
                        posi = G("posi", [128, SBW], I32)
                        S.dma('sp', posi[:], pos[0:1, t0:t0 + SBW].partition_broadcast(128), writes=[posi], sembuf=posi)
                        posf = G("posf", [128, SBW], F32)
                        S.cp('dve', posf[:], posi[:], [posi], [posf])
                        ang = G("ang", [128, SBW], F32)
                        S.ts('dve', ang[:], posf[:], cc[:, 0:1], ALU.mult, [posf, cc], [ang])
                        tabs = []
                        for which in range(2):
                            if which == 1:
                                ang2 = G("ang2", [128, SBW], F32)
                                S.ts('pool', ang2[:], ang[:], math.pi / 2, ALU.add, [ang], [ang2])
                                a_ = ang2
                            else:
                                a_ = ang
                            ki = G(f"ki{which}", [128, SBW], I32)
                            S.ts('dve', ki[:], a_[:], 1.0 / TWO_PI, ALU.mult, [a_], [ki])
                            kf = G(f"kf{which}", [128, SBW], F32)
                            S.cp('pool', kf[:], ki[:], [ki], [kf])
                            rr_ = G(f"rr{which}", [128, SBW], F32)
                            S.stt(rr_[:], kf[:], -TWO_PI, a_[:], ALU.mult, ALU.add, [kf, a_], [rr_])
                            S.ts('pool', rr_[:], rr_[:], math.pi, ALU.min, [rr_], [rr_], s2=-math.pi, op1=ALU.max)
                            tb = G(f"tab{which}", [128, SBW], F32)
                            S.act(tb[:], rr_[:], AF.Sin, [rr_], [tb])
                            tabs.append(tb)
                        sint, cost = tabs
                        sins = G("sins", [128, SBW], F32)
                        S.act(sins[:], sint[:], AF.Identity, [sint, cc], [sins], scale=cc[:, 1:2])

                        def ret_prep(h, full):
                            base = h * 6 * 128
                            for nm, off in ((("q", 0), ("k", 2)) if full else (("k", 2),)):
                                t1 = G(f"rt1{nm}{h}", [128, SBW], F32)
                                t2 = G(f"rt2{nm}{h}", [128, SBW], F32)
                                p1 = proj(hT, base + off * 128)
                                S.tt('dve', t1[:], p1[:, 0:SBW], cost[:], ALU.mult, [p1, cost], [t1])
                                yield
                                p2 = proj(hT, base + (off + 1) * 128)
                                S.tt('dve', t2[:], p2[:, 0:SBW], sins[:], ALU.mult, [p2, sins], [t2])
                                yield
                                o_ = G(f"r{nm}T{h}", [128, SBW], BF16)
                                S.tt('pool', o_[:], t1[:], t2[:], ALU.add, [t1, t2], [o_])
                                yield
                            pv = proj(hT, base + 4 * 128)
                            vT = G(f"rvT{h}", [128, SBW], BF16)
                            S.cp('act', vT[:], pv[:, 0:SBW], [pv], [vT])
                            yield
                            if full:
                                pg_ = proj(hT, base + 5 * 128)
                                sgT = G(f"rsgT{h}", [128, SBW], F32)
                                S.act(sgT[:], pg_[:, 0:SBW], AF.Silu, [pg_], [sgT])
                                yield

                        def ret_chain(tt, h, sb_, full):
                            sl = slice(tt * 128, (tt + 1) * 128)
                            krT, vT = S.cache[f"rkT{h}"], S.cache[f"rvT{h}"]
                            if full:
                                qrT, sgT = S.cache[f"rqT{h}"], S.cache[f"rsgT{h}"]
                            pk = S.P()
                            pkb = pk.ap[:].bitcast(BF16)
                            S.tr(pkb[:, 0:128], krT[:, sl], ident_b[:], [krT, ident_b], [pk])
                            S.tr(pkb[:, 128:256], vT[:, sl], ident_b[:], [vT, ident_b], [pk])
                            kw = G(f"rkw{h}", [128, 128], BF16)
                            S.ts('dve', kw[:], pkb[:, 0:128], cc[:, 4 + h:5 + h], ALU.mult, [pk, cc], [kw])
                            vtok = G(f"rvtok{h}", [128, 128], BF16)
                            S.act(vtok[:], pkb[:, 128:256], AF.Identity, [pk, vm], [vtok], scale=vm[:, sb_:sb_ + 1])
                            if not full:
                                yield
                                po = S.P()
                                S.mm(po[:, 128:256], kw[:], vtok[:], [kw, vtok], [po])
                                S.stt(Sr[h][:], Sr[h][:], CD[h], po[:, 128:256], ALU.mult, ALU.add, [Sr[h], po], [Sr[h]])
                                yield
                                S.cp('pool', Srb[h][:], Sr[h][:], [Sr[h]], [Srb[h]])
                                yield
                                return
                            qwT = G(f"rqw{h}", [128, 128], BF16)
                            S.tt('pool', qwT[:], qrT[:, sl], C[f'qdec{h}'], ALU.mult, [qrT, cb], [qwT])
                            yield
                            psc = S.P()
                            S.mm(psc[:, 0:128], krT[:, sl], qrT[:, sl], [krT, qrT], [psc])
                            sT = G(f"rsT{h}", [128, 128], BF16)
                            S.tt('dve', sT[:], psc[:, 0:128], C[f'decT{h}'], ALU.mult, [psc, cb], [sT])
                            yield
                            po = S.P()
                            S.mm(po[:, 0:128], sT[:], vtok[:], [sT, vtok], [po], start=True, stop=False)
                            S.mm(po[:, 0:128], qwT[:], Srb[h][:], [qwT, Srb[h]], [po], start=False, stop=True)
                            S.mm(po[:, 128:256], kw[:], vtok[:], [kw, vtok], [po])
                            S.stt(Sr[h][:], Sr[h][:], CD[h], po[:, 128:256], ALU.mult, ALU.add, [Sr[h], po], [Sr[h]])
                            osb = G(f"rosb{h}", [128, 128], F32)
                            S.cp('act', osb[:], po[:, 0:128], [po], [osb])
                            yield
                            S.cp('pool', Srb[h][:], Sr[h][:], [Sr[h]], [Srb[h]])
                            junk = G(f"rjunk{h}", [128, 128], F32)
                            ssq = G(f"rssq{h}", [128, 4], F32)
                            S.op('pool', lambda e, ssq=ssq: e.memset(ssq[:], 0.0), [], [ssq])
                            yield
                            S.act(junk[:], osb[:], AF.Square, [osb], [junk, ssq], accum_out=ssq[:, 0:1])
                            yield
                            S.act(ssq[:, 1:2], ssq[:, 0:1], AF.Ln, [ssq, cc], [ssq], bias=cc[:, 2:3], scale=1.0 / 128)
                            yield
                            S.act(ssq[:, 2:3], ssq[:, 1:2], AF.Exp, [ssq], [ssq], scale=-0.5)
                            yield
                            on = G(f"ron{h}", [128, 128], F32)
                            S.ts('dve', on[:], osb[:], ssq[:, 2:3], ALU.mult, [osb, ssq], [on])
                            yield
                            pt = S.P()
                            S.tr(pt[:, 0:128], on[:], C['ident'], [on, cb], [pt])
                            loc = (sb_ - NFULL0) * SBW + tt * 128
                            S.tt('dve', catT[:, 2 * h, loc:loc + 128], pt[:, 0:128], sgT[:, sl], ALU.mult, [pt, sgT], [catT])
                            yield

                        full = sb >= NFULL0
                        rr([ret_prep(h, full) for h in range(NH)])
                        for tt in range(NT):
                            rr([ret_chain(tt, h, sb, full) for h in range(NH)])

                    if phase == 'gdn':
                        def gdn_prep(h, w_, p, sb_):
                            nm = ("q", "k", "v")[w_]
                            base = NH * 6 * 128 + h * 4 * 128
                            cbf = cbuf[h][w_]
                            ps = proj(hT, base + w_ * 128)
                            S.act(cbf[:, 3:3 + SBW], ps[:, 0:SBW], AF.Identity, [ps, vm], [cbf], scale=vm[:, sb_:sb_ + 1])
                            yield
                            acc = G(f"gacc{h}_{w_}", [128, SBW], F32)
                            wc = h * 12 + w_ * 4
                            S.act(acc[:], cbf[:, 0:SBW], AF.Identity, [cbf, cw], [acc], scale=cw[:, wc:wc + 1])
                            yield
                            for j in range(1, 4):
                                S.stt(acc[:], cbf[:, j:j + SBW], cw[:, wc + j:wc + j + 1], acc[:], ALU.mult, ALU.add, [cbf, cw, acc], [acc])
                                yield
                            tl = G(f"gtail{h}_{w_}", [128, 4], F32)
                            S.cp('pool', tl[:, 0:3], cbf[:, SBW:SBW + 3], [cbf], [tl])
                            yield
                            S.cp('pool', cbf[:, 0:3], tl[:, 0:3], [tl], [cbf])
                            yield
                            if nm == "v":
                                vT = G(f"gvT{h}_{p}", [128, SBW], BF16)
                                S.act(vT[:], acc[:], AF.Silu, [acc], [vT])
                                yield
                            else:
                                y = acc
                                S.act(y[:], acc[:], AF.Silu, [acc], [y])
                                yield
                                sq = G(f"gsq{h}_{w_}", [128, SBW], BF16)
                                S.act(sq[:], y[:], AF.Square, [y], [sq])
                                yield
                                pn = S.P()
                                S.mm(pn[:, 0:SBW], ones_b[:], sq[:], [ones_b, sq], [pn])
                                rn = G(f"grn{h}_{w_}", [128, SBW], F32)
                                S.act(rn[:], pn[:, 0:SBW], AF.Ln, [pn, cc], [rn], bias=cc[:, 2:3], scale=1.0)
                                yield
                                S.act(rn[:], rn[:], AF.Exp, [rn], [rn], scale=-0.5)
                                yield
                                o_ = G(f"g{nm}nT{h}_{p}", [128, SBW], BF16)
                                if nm == "q":
                                    S.stt(o_[:], y[:], 128 ** -0.5, rn[:], ALU.mult, ALU.mult, [y, rn], [o_])
                                else:
                                    S.tt('pool', o_[:], y[:], rn[:], ALU.mult, [y, rn], [o_])
                                yield

                        def gdn_prep_z(h, p):
                            base = NH * 6 * 128 + h * 4 * 128
                            pz = proj(hT, base + 3 * 128)
                            szT = G(f"gszT{h}_{p}", [128, SBW], F32)
                            S.act(szT[:], pz[:, 0:SBW], AF.Silu, [pz], [szT])
                            yield

                        def gdn_scal(tt, p, sb_):
                            sl = slice(tt * 128, (tt + 1) * 128)
                            sc = G(f"gsc{tt}_{p}", [128, 64], F32)
                            pab = S.P()
                            for c in range(8):
                                S.mm(pab[:, 0:2 * NH], hT[:, c, sl], Wab[:, c, :], [hT, Wab], [pab], start=(c == 0), stop=(c == 7))
                            S.tt('dve', sc[:, 0:4], pab[:, 0:NH], hvt[:, NH:2 * NH], ALU.add, [pab, hvt], [sc])
                            S.act(sc[:, 16:20], pab[:, NH:2 * NH], AF.Exp, [pab], [sc], scale=-1.0)
                            yield
                            S.act(sc[:, 4:8], sc[:, 0:4], AF.Exp, [sc], [sc])
                            yield
                            S.act(sc[:, 8:12], sc[:, 4:8], AF.Ln, [sc], [sc], bias=1.0)
                            yield
                            S.tt('dve', sc[:, 12:16], sc[:, 8:12], nea[:], ALU.mult, [sc, nea], [sc])
                            yield
                            S.ts('dve', sc[:, 16:20], sc[:, 16:20], 1.0, ALU.add, [sc], [sc])
                            yield
                            S.op('dve', lambda e, sc=sc: e.reciprocal(out=sc[:, 20:24], in_=sc[:, 16:20]), [sc], [sc])
                            yield
                            S.ts('dve', sc[:, 20:24], sc[:, 20:24], vm[:, sb_:sb_ + 1], ALU.mult, [sc, vm], [sc])
                            yield
                            pgc = S.P()
                            S.mm(pgc[:, 0:NH], C['triU'], sc[:, 12:16], [cb, sc], [pgc])
                            S.mm(pgc[:, 8:8 + NH], ones_f[:], sc[:, 12:16], [ones_f, sc], [pgc])
                            S.cp('dve', sc[:, 24:28], pgc[:, 0:NH], [pgc], [sc])
                            S.cp('dve', sc[:, 28:32], pgc[:, 8:8 + NH], [pgc], [sc])
                            yield
                            S.act(sc[:, 32:40], sc[:, 24:32], AF.Exp, [sc], [sc])
                            yield
                            S.tt('dve', sc[:, 40:44], sc[:, 28:32], sc[:, 24:28], ALU.subtract, [sc], [sc])
                            yield
                            S.act(sc[:, 44:48], sc[:, 40:44], AF.Exp, [sc], [sc])
                            yield
                            S.tt('dve', sc[:, 48:52], sc[:, 20:24], sc[:, 32:36], ALU.mult, [sc], [sc])
                            yield

                        def chain_pre(ch):
                            key, tt, h, sl, p = ch['key'], ch['tt'], ch['h'], ch['sl'], ch['p']
                            sc = S.cache[f"gsc{tt}_{p}"]
                            ch['sc'] = sc
                            full = ch['full']
                            knT, vT = S.cache[f"gknT{h}_{p}"], S.cache[f"gvT{h}_{p}"]
                            qnT = S.cache[f"gqnT{h}_{p}"] if full else None
                            F = [G(f"gF{i}_{key}", [128, 128], F32) for i in range(4)]
                            Bq = [G(f"gB{i}_{key}", [128, 128], BF16) for i in range(2)]
                            gb = F[0]
                            S.act(gb[:], ones_f[:], AF.Identity, [ones_f, sc], [gb], scale=sc[:, 12 + h:13 + h])
                            pT = S.P()
                            pTb = pT.ap[:].bitcast(BF16)
                            S.tr(pTb[:, 128:256], knT[:, sl], ident_b[:], [knT, ident_b], [pT])
                            S.tr(pTb[:, 256:384], vT[:, sl], ident_b[:], [vT, ident_b], [pT])
                            kbg = G(f"gkbg{key}", [128, 128], BF16)
                            S.act(kbg[:], pTb[:, 128:256], AF.Identity, [pT, sc], [kbg], scale=sc[:, 48 + h:49 + h])
                            kt = G(f"gkt{key}", [128, 128], BF16)
                            S.ts('dve', kt[:], pTb[:, 128:256], sc[:, 44 + h:45 + h], ALU.mult, [pT, sc], [kt])
                            vb = G(f"gvb{key}", [128, 128], BF16)
                            S.act(vb[:], pTb[:, 256:384], AF.Identity, [pT, sc], [vb], scale=sc[:, 20 + h:21 + h])
                            yield
                            pG = S.P()
                            S.mm(pG[:, 0:128], C['triU'], gb[:], [cb, gb], [pG], start=True, stop=False)
                            S.mm(pG[:, 0:128], gb[:], C['negtriU'], [cb, gb], [pG], start=False, stop=True)
                            ex = F[1]
                            S.stt(ex[:], pG[:, 0:128], 0.0, C['negmask'], ALU.min, ALU.add, [pG, cb], [ex])
                            yield
                            dec_i = F[1]
                            S.act(dec_i[:], ex[:], AF.Exp, [ex], [dec_i])
                            yield
                            dec_s = F[2]
                            S.tt('pool', dec_s[:], dec_i[:], C['ident'], ALU.subtract, [dec_i, cb], [dec_s])
                            yield
                            pK = S.P()
                            S.mm(pK[:, 0:128], knT[:, sl], knT[:, sl], [knT], [pK])
                            if full:
                                S.mm(pK[:, 128:256], qnT[:, sl], knT[:, sl], [qnT, knT], [pK])
                            A = G(f"gA{key}", [128, 128], BF16)
                            S.stt(A[:], pK[:, 0:128], sc[:, 20 + h:21 + h], dec_s[:], ALU.mult, ALU.mult, [pK, sc, dec_s], [A])
                            attn = Bq[0]
                            if full:
                                S.tt('dve', attn[:], pK[:, 128:256], dec_i[:], ALU.mult, [pK, dec_i], [attn])
                            yield
                            tm = Bq[1]
                            S.tt('pool', tm[:], A[:], m2Lb[:], ALU.mult, [A, m2Lb], [tm])
                            attnT = G(f"gattnT{key}", [128, 128], BF16)
                            if full:
                                pT2 = S.P()
                                pT2b = pT2.ap[:].bitcast(BF16)
                                S.tr(pT2b[:, 0:128], attn[:], ident_b[:], [attn, ident_b], [pT2])
                                S.cp('act', attnT[:], pT2b[:, 0:128], [pT2], [attnT])
                            yield
                            T1 = G(f"gT0{key}", [128, 128], BF16)
                            S.tt('pool', T1[:], ident_b[:], tm[:], ALU.subtract, [ident_b, tm], [T1])
                            yield
                            pU = S.P()
                            pUb = pU.ap[:].bitcast(BF16)
                            S.tr(pUb[:, 0:128], T1[:], ident_b[:], [T1, ident_b], [pU])
                            U = G(f"gU0{key}", [128, 128], BF16)
                            S.cp('dve', U[:], pUb[:, 0:128], [pU], [U])
                            yield
                            Tk = T1
                            for lvl, bs in enumerate((4, 8, 16, 32, 64, None)):
                                pW = S.P()
                                S.mm(pW[:, 0:128], A[:], U[:], [A, U], [pW])
                                W = Bq[0]
                                S.tt('dve', W[:], pW[:, 0:128], U[:], ALU.add, [pW, U], [W])
                                yield
                                pW2 = S.P()
                                S.mm(pW2[:, 0:128], Tk[:], W[:], [Tk, W], [pW2])
                                Un = G(f"gU{(lvl + 1) % 2}{key}", [128, 128], BF16)
                                if bs is not None:
                                    tmpf = F[0]
                                    S.stt(tmpf[:], U[:], 2.0, pW2[:, 0:128], ALU.mult, ALU.subtract, [U, pW2], [tmpf])
                                    yield
                                    S.tt('pool', Un[:], tmpf[:], C[f'bm{bs}'], ALU.mult, [tmpf, cb], [Un])
                                    yield
                                    pX = S.P()
                                    pXb = pX.ap[:].bitcast(BF16)
                                    S.tr(pXb[:, 0:128], Un[:], ident_b[:], [Un, ident_b], [pX])
                                    Tn = G(f"gT{(lvl + 1) % 2}{key}", [128, 128], BF16)
                                    S.cp('act' if lvl % 2 == 0 else 'dve', Tn[:], pXb[:, 0:128], [pX], [Tn])
                                    yield
                                    Tk = Tn
                                else:
                                    S.stt(Un[:], U[:], 2.0, pW2[:, 0:128], ALU.mult, ALU.subtract, [U, pW2], [Un])
                                    yield
                                U = Un
                            pw = S.P()
                            S.mm(pw[:, 0:128], kbg[:], U[:], [kbg, U], [pw])
                            S.mm(pw[:, 128:256], U[:], vb[:], [U, vb], [pw])
                            wT = G(f"gwT{key}", [128, 128], BF16)
                            S.cp('act' if h % 2 == 0 else 'dve', wT[:], pw[:, 0:128], [pw], [wT])
                            u = F[3]
                            S.cp('dve', u[:], pw[:, 128:256], [pw], [u])
                            yield
                            ch.update(kt=kt, attnT=attnT, wT=wT, u=u, F=F, Bq=Bq)

                        def chain_scan(ch):
                            key, h, sl, sc, F, Bq = ch['key'], ch['h'], ch['sl'], ch['sc'], ch['F'], ch['Bq']
                            full = ch['full']
                            if not full:
                                p1 = S.P()
                                S.mm(p1[:, 0:128], ch['wT'][:], Sgb[h][:], [ch['wT'], Sgb[h]], [p1])
                                vn = Bq[1]
                                S.tt('dve', vn[:], ch['u'][:], p1[:, 0:128], ALU.subtract, [ch['u'], p1], [vn])
                                yield
                                p2 = S.P()
                                S.mm(p2[:, 0:128], ch['kt'][:], vn[:], [ch['kt'], vn], [p2])
                                S.stt(Sg[h][:], Sg[h][:], sc[:, 36 + h:37 + h], p2[:, 0:128], ALU.mult, ALU.add, [Sg[h], sc, p2], [Sg[h]])
                                yield
                                S.cp('pool', Sgb[h][:], Sg[h][:], [Sg[h]], [Sgb[h]])
                                yield
                                return
                            qnT, szT = S.cache[f"gqnT{h}_{ch['p']}"], S.cache[f"gszT{h}_{ch['p']}"]
                            p1 = S.P()
                            S.mm(p1[:, 0:128], ch['wT'][:], Sgb[h][:], [ch['wT'], Sgb[h]], [p1])
                            S.mm(p1[:, 128:256], qnT[:, sl], Sgb[h][:], [qnT, Sgb[h]], [p1])
                            vn = Bq[1]
                            S.tt('dve', vn[:], ch['u'][:], p1[:, 0:128], ALU.subtract, [ch['u'], p1], [vn])
                            o1 = F[1]
                            S.act(o1[:], p1[:, 128:256], AF.Identity, [p1, sc], [o1], scale=sc[:, 32 + h:33 + h])
                            yield
                            p2 = S.P()
                            S.mm(p2[:, 0:128], ch['kt'][:], vn[:], [ch['kt'], vn], [p2])
                            S.mm(p2[:, 128:256], ch['attnT'][:], vn[:], [ch['attnT'], vn], [p2])
                            S.stt(Sg[h][:], Sg[h][:], sc[:, 36 + h:37 + h], p2[:, 0:128], ALU.mult, ALU.add, [Sg[h], sc, p2], [Sg[h]])
                            o = F[2]
                            S.tt('dve', o[:], o1[:], p2[:, 128:256], ALU.add, [o1, p2], [o])
                            yield
                            S.cp('pool', Sgb[h][:], Sg[h][:], [Sg[h]], [Sgb[h]])
                            junk = F[0]
                            ssq = G(f"gssq{key}", [128, 4], F32)
                            S.op('pool', lambda e, ssq=ssq: e.memset(ssq[:], 0.0), [], [ssq])
                            yield
                            S.act(junk[:], o[:], AF.Square, [o], [junk, ssq], accum_out=ssq[:, 0:1])
                            yield
                            S.act(ssq[:, 1:2], ssq[:, 0:1], AF.Ln, [ssq, cc], [ssq], bias=cc[:, 2:3], scale=1.0 / 128)
                            yield
                            S.act(ssq[:, 2:3], ssq[:, 1:2], AF.Exp, [ssq], [ssq], scale=-0.5)
                            yield
                            on = F[1]
                            S.act(on[:], o[:], AF.Identity, [o, ssq], [on], scale=ssq[:, 2:3])
                            yield
                            pt = S.P()
                            S.tr(pt[:, 0:128], on[:], C['ident'], [on, cb], [pt])
                            loc = (ch['sb'] - NFULL0) * SBW + ch['tt'] * 128
                            S.stt(catT[:, 2 * h + 1, loc:loc + 128], pt[:, 0:128], dnwt[:, 0:1], szT[:, sl], ALU.mult, ALU.mult, [pt, dnwt, szT], [catT])
                            yield

                        def Bgens(p, sb_):
                            full_ = sb_ >= NFULL0
                            return ([gdn_prep(h, w_, p, sb_) for h in range(NH) for w_ in range(3) if (full_ or w_ > 0 or sb_ == NFULL0 - 1)]
                                    + ([gdn_prep_z(h, p) for h in range(NH)] if full_ else [])
                                    + [gdn_scal(tt, p, sb_) for tt in range(NT)])
                        p = sb % 2
                        if sb == 0:
                            rr(Bgens(0, 0))
                        chains = [dict(key=f"{tt}_{h}", tt=tt, h=h, p=p, sb=sb, full=(sb >= NFULL0), sl=slice(tt * 128, (tt + 1) * 128))
                                  for tt in range(NT) for h in range(NH)]
                        gens = [chain_pre(ch) for ch in chains]
                        if sb + 1 < NSB:
                            hT = emitA(sb + 1)
                            gens = gens + Bgens(1 - p, sb + 1)
                        rr(gens)
                        for tt in range(NT):
                            rr([chain_scan(ch) for ch in chains if ch['tt'] == tt])
                print('SBUF remaining in phase', phase, nc.sbuf_bytes_remaining, flush=True)
                S.barrier()
            S.cache = {}
            print('SBUF remaining after phase', phase, nc.sbuf_bytes_remaining, flush=True) if False else None
            if os.environ.get('KSTOP') == phase:
                return nc

        NTB = TS // 128
        NSL = T // TS
        with ExitStack() as esB:
            fnwt = S.sb("fnwt", [128, D], F32, esB)
            S.dma('sp', fnwt[:], fnw[:, :], writes=[fnwt], sembuf=fnwt)
            h2T = catT
            Gt = S.sb("Gt", [128, NTB, 32], F32, esB)
            g12 = S.sb("g12", [128, 2 * D], F32, esB)
            with ExitStack() as esB1:
                S.cache_es = esB1
                G = S.g
                for gi, base in ((0, 16), (1, 40)):
                    for c in range(8):
                        dg = S.g(f"dg{c % 2}", [128, 128], F32)
                        S.ts('dve', dg[:], C['ident'], mod[:, base + c:base + c + 1], ALU.mult, [cb, mod], [dg])
                        pg = S.P()
                        S.mm(pg[:, 0:128], ones_f[:], dg[:], [ones_f, dg], [pg])
                        S.cp('act', g12[:, gi * D + c * 128:gi * D + (c + 1) * 128], pg[:, 0:128], [pg], [g12])
                Wout = S.sb("Wout", [128, 8, D], BF16, esB1)
                S.dma('pool', Wout[:], w_out.rearrange("(c p) n -> p c n", p=128), writes=[Wout], sembuf=Wout)
                Wr = S.sb("Wr", [128, 8, 36], F32, esB1)
                S.dma('sp', Wr[:], w_r.rearrange("(c p) n -> p c n", p=128), writes=[Wr], sembuf=Wr)
                brt = S.sb("brt", [128, 36], F32, esB1)
                S.dma('sp', brt[:], b_r[:, :], writes=[brt], sembuf=brt)
                for tt in range(NTB):
                    sl = slice(tt * 128, (tt + 1) * 128)
                    xst = G(f"xst{tt % 2}", [128, D], F32)
                    S.dma('sp', xst[:], xs[tt * 128:(tt + 1) * 128, :], writes=[xst], sembuf=xst)
                    x1 = G(f"x1_{tt % 2}", [128, D], F32)
                    for half in range(2):
                        hs = slice(half * 512, (half + 1) * 512)
                        pm_ = S.P()
                        for c in range(8):
                            S.mm(pm_[:, :], catT[:, c, sl], Wout[:, c, hs], [catT, Wout], [pm_], start=(c == 0), stop=(c == 7))
                        tmp = G(f"mixg{half}", [128, 512], F32)
                        S.tt('dve', tmp[:], pm_[:, :], g12[:, half * 512:(half + 1) * 512], ALU.mult, [pm_, g12], [tmp])
                        S.tt('pool', x1[:, hs], tmp[:], xst[:, hs], ALU.add, [tmp, xst], [x1])
                    S.dma('sp', x1_d.ap[tt * 128:(tt + 1) * 128, :], x1[:], reads=[x1], writes=[x1_d], sembuf=x1)
                    junk = G("bjunk", [128, D], F32)
                    ssq = G(f"bssq{tt % 2}", [128, 4], F32)
                    S.op('pool', lambda e, ssq=ssq: e.memset(ssq[:], 0.0), [], [ssq])
                    S.act(junk[:], x1[:], AF.Square, [x1], [junk, ssq], accum_out=ssq[:, 0:1])
                    S.rsqrt((ssq[:, 2:3], ssq), ssq[:, 0:1], 1.0 / D, [ssq], (ssq[:, 1:2], ssq))
                    xn = G("bxn", [128, D], F32)
                    S.ts('dve', xn[:], x1[:], ssq[:, 2:3], ALU.mult, [x1, ssq], [xn])
                    h2f = G("h2f", [128, 8, 128], F32)
                    for c in range(8):
                        ptp = S.P()
                        S.tr(ptp[:, 0:128], xn[:, c * 128:(c + 1) * 128], C['ident'], [xn, cb], [ptp])
                        S.act(h2f[:, c, :], ptp[:, 0:128], AF.Identity, [ptp, a2, mod], [h2f], bias=mod[:, 24 + c:25 + c], scale=a2[:, c:c + 1])
                    S.cp('pool', h2T[:, :, sl], h2f[:], [h2f], [h2T])
                    plg = S.P()
                    for c in range(8):
                        S.mm(plg[:, 0:36], h2f[:, c, :], Wr[:, c, :], [h2f, Wr], [plg], start=(c == 0), stop=(c == 7))
                    r = G("rt", [128, 256], F32)
                    S.tt('dve', r[:, 0:36], plg[:, 0:36], brt[:], ALU.add, [plg, brt], [r])
                    S.op('dve', lambda e, r=r: e.reduce_max(out=r[:, 36:37], in_=r[:, 0:4], axis=AX.X), [r], [r])
                    S.ts('dve', r[:, 40:44], r[:, 0:4], r[:, 36:37], ALU.is_equal, [r], [r])
                    S.ts('dve', r[:, 44:48], r[:, 0:4], r[:, 36:37], ALU.subtract, [r], [r])
                    S.act(r[:, 44:48], r[:, 44:48], AF.Exp, [r], [r])
                    S.op('dve', lambda e, r=r: e.reduce_sum(out=r[:, 48:49], in_=r[:, 44:48], axis=AX.X), [r], [r])
                    S.op('dve', lambda e, r=r: e.reciprocal(out=r[:, 49:50], in_=r[:, 48:49]), [r], [r])
                    S.ts('dve', r[:, 44:48], r[:, 40:44], 1.0, ALU.subtract, [r], [r], s2=1e30, op1=ALU.mult)
                    S.tt('dve', r[:, 64:96].rearrange("p (g e) -> p g e", e=8), r[:, 4:36].rearrange("p (g e) -> p g e", e=8),
                         r[:, 44:48].unsqueeze(2).to_broadcast([128, 4, 8]), ALU.add, [r], [r])
                    S.op('dve', lambda e, r=r: e.reduce_max(out=r[:, 96:97], in_=r[:, 64:96], axis=AX.X), [r], [r])
                    S.ts('dve', r[:, 100:132], r[:, 64:96], r[:, 96:97], ALU.is_equal, [r], [r])
                    S.stt(r[:, 132:164], r[:, 100:132], -1e30, r[:, 64:96], ALU.mult, ALU.add, [r], [r])
                    S.op('dve', lambda e, r=r: e.reduce_max(out=r[:, 164:165], in_=r[:, 132:164], axis=AX.X), [r], [r])
                    S.ts('dve', r[:, 168:200], r[:, 132:164], r[:, 164:165], ALU.is_equal, [r], [r])
                    S.tt('dve', r[:, 200:201], r[:, 164:165], r[:, 96:97], ALU.subtract, [r], [r])
                    S.act(r[:, 200:201], r[:, 200:201], AF.Exp, [r], [r])
                    S.ts('dve', r[:, 201:202], r[:, 200:201], 1.0, ALU.add, [r], [r])
                    S.op('dve', lambda e, r=r: e.reciprocal(out=r[:, 202:203], in_=r[:, 201:202]), [r], [r])
                    S.tt('dve', r[:, 202:203], r[:, 202:203], r[:, 49:50], ALU.mult, [r], [r])
                    S.tt('dve', r[:, 203:204], r[:, 202:203], r[:, 200:201], ALU.mult, [r], [r])
                    S.ts('dve', r[:, 204:236], r[:, 100:132], r[:, 202:203], ALU.mult, [r], [r])
                    S.stt(Gt[:, tt, :], r[:, 168:200], r[:, 203:204], r[:, 204:236], ALU.mult, ALU.add, [r], [Gt])
                S.barrier()
            S.cache = {}
            if os.environ.get('KSTOP') == 'b1':
                return nc
            acc = S.sb("acc", [128, NTB, D], F32, esB)
            S.op('pool', lambda e: e.memset(acc[:], 0.0), [], [acc])
            with ExitStack() as esB2:
                S.cache_es = esB2
                G = S.g
                NB = max(1, TS // 512)
                BW = min(512, TS)
                for ex_ in range(NE):
                    w1t = G(f"w1t{ex_ % 2}", [128, 8, DEXP], BF16)
                    w3t = G(f"w3t{ex_ % 2}", [128, 8, DEXP], BF16)
                    w2t = G(f"w2t{ex_ % 2}", [128, 4, D], BF16)
                    S.dma('pool', w1t[:], w1[ex_].rearrange("(c p) n -> p c n", p=128), writes=[w1t], sembuf=w1t)
                    S.dma('pool', w3t[:], w3[ex_].rearrange("(c p) n -> p c n", p=128), writes=[w3t], sembuf=w3t)
                    S.dma('pool', w2t[:], w2[ex_].rearrange("(c p) n -> p c n", p=128), writes=[w2t], sembuf=w2t)
                    for tb in range(NB):
                        bsl = slice(tb * BW, (tb + 1) * BW)
                        hid = G(f"hid{tb % 2}", [128, 4, BW], BF16)
                        for hc in range(4):
                            p1 = S.P()
                            for c in range(8):
                                S.mm(p1[:, 0:BW], w1t[:, c, hc * 128:(hc + 1) * 128], h2T[:, c, bsl], [w1t, h2T], [p1], start=(c == 0), stop=(c == 7))
                            p3 = S.P()
                            for c in range(8):
                                S.mm(p3[:, 0:BW], w3t[:, c, hc * 128:(hc + 1) * 128], h2T[:, c, bsl], [w3t, h2T], [p3], start=(c == 0), stop=(c == 7))
                            sl_ = G(f"silu{hc % 2}", [128, BW], F32)
                            S.act(sl_[:], p1[:, 0:BW], AF.Silu, [p1], [sl_])
                            S.tt('dve', hid[:, hc, :], sl_[:], p3[:, 0:BW], ALU.mult, [sl_, p3], [hid])
                        for t4 in range(BW // 128):
                            tt = tb * (BW // 128) + t4
                            for half in range(2):
                                py = S.P()
                                for hc in range(4):
                                    S.mm(py[:, :], hid[:, hc, t4 * 128:(t4 + 1) * 128], w2t[:, hc, half * 512:(half + 1) * 512], [hid, w2t], [py],
                                         start=(hc == 0), stop=(hc == 3))
                                S.stt(acc[:, tt, half * 512:(half + 1) * 512], py[:, :], Gt[:, tt, ex_:ex_ + 1], acc[:, tt, half * 512:(half + 1) * 512],
                                      ALU.mult, ALU.add, [py, Gt, acc], [acc])
                S.barrier()
            S.cache = {}
            if os.environ.get('KSTOP') == 'b2':
                return nc
            with ExitStack() as esB3:
                S.cache_es = esB3
                G = S.g
                for tt in range(NTB):
                    x1 = G(f"fx1_{tt % 2}", [128, D], F32)
                    S.dma('sp', x1[:], x1_d.ap[tt * 128:(tt + 1) * 128, :], reads=[x1_d], writes=[x1], sembuf=x1)
                    x2 = G(f"fx2_{tt % 2}", [128, D], F32)
                    S.tt('pool', x2[:], acc[:, tt, :], g12[:, D:2 * D], ALU.mult, [acc, g12], [x2])
                    S.tt('dve', x2[:], x2[:], x1[:], ALU.add, [x2, x1], [x2])
                    junk = G("fjunk", [128, D], F32)
                    ssq = G(f"fssq{tt % 2}", [128, 4], F32)
                    S.op('pool', lambda e, ssq=ssq: e.memset(ssq[:], 0.0), [], [ssq])
                    S.act(junk[:], x2[:], AF.Square, [x2], [junk, ssq], accum_out=ssq[:, 0:1])
                    S.rsqrt((ssq[:, 2:3], ssq), ssq[:, 0:1], 1.0 / D, [ssq], (ssq[:, 1:2], ssq))
                    ot = G(f"fot{tt % 2}", [128, D], F32)
                    S.stt(ot[:], x2[:], ssq[:, 2:3], fnwt[:], ALU.mult, ALU.mult, [x2, ssq, fnwt], [ot])
                    S.dma('sp', out[tt * 128:(tt + 1) * 128, :], ot[:], reads=[ot], sembuf=ot)
                S.barrier()
        print("ninst", S.ninst, "nsem", S.nsem, flush=True)
    return nc


def _host_inputs(inp, T, TS):
    NH = NHEAD
    NE_ = int(os.environ.get('KNEXP', NEXP))
    f = lambda a: np.ascontiguousarray(np.asarray(a), dtype=np.float32)
    x = f(inp['x'])
    B = x.shape[0]
    w_in = f(inp['w_in'])[0]
    swap = np.concatenate([np.arange(64, 128), np.arange(0, 64)])
    cols = []
    for h in range(NH):
        q = np.arange(h * 128, (h + 1) * 128)
        k = 512 + q
        v = 1024 + q
        g = 1536 + q
        cols += [q, q[swap], k, k[swap], v, g]
    for h in range(NH):
        q = 2048 + np.arange(h * 128, (h + 1) * 128)
        cols += [q, q + 512, q + 1024, q + 1536]
    cols = np.concatenate(cols)
    w_inA = np.ascontiguousarray(w_in[:, cols])
    w_ab = np.ascontiguousarray(w_in[:, 4096:4104])
    conv = f(inp['conv_w'])[0]
    convw = np.zeros((128, NH * 12), np.float32)
    for h in range(NH):
        for w_ in range(3):
            ch = w_ * 512 + h * 128 + np.arange(128)
            convw[:, h * 12 + w_ * 4:h * 12 + w_ * 4 + 4] = conv[:, ch].T
    hv = np.broadcast_to(np.concatenate([f(inp['a_log'])[0], f(inp['dt_bias'])[0]])[None, :], (128, 2 * NH)).copy()
    dnw = f(inp['dn_norm_w'])[0].reshape(128, 1).copy()
    w_out = f(inp['w_out'])[0]
    rows = []
    for h in range(NH):
        rows += [np.arange(h * 128, (h + 1) * 128), 512 + np.arange(h * 128, (h + 1) * 128)]
    w_outP = np.ascontiguousarray(w_out[np.concatenate(rows), :])
    w_r = np.ascontiguousarray(np.concatenate([f(inp['w_group'])[0], f(inp['w_expert'])[0]], axis=1))
    b_r = np.broadcast_to(np.concatenate([f(inp['b_group'])[0], f(inp['b_expert'])[0]])[None, :], (128, 36)).copy()
    n12 = np.concatenate([f(inp['norm1_w'])[0].reshape(8, 128).T, f(inp['norm2_w'])[0].reshape(8, 128).T], axis=1).copy()
    b_adaT = np.ascontiguousarray(f(inp['b_ada'])[0].reshape(48, 128).T)
    fnw = np.broadcast_to(f(inp['final_norm_w'])[None, :], (128, D)).copy()
    shared = dict(w_ada=f(inp['w_ada'])[0], b_adaT=b_adaT, n12=n12, w_inA=w_inA, w_ab=w_ab, convw=convw, hv=hv, dnw=dnw,
                  w_out=w_outP, w_r=w_r, b_r=b_r, w1=f(inp['w1'])[0][:NE_], w3=f(inp['w3'])[0][:NE_], w2=f(inp['w2'])[0][:NE_], fnw=fnw,
                  cbig=CBIG, ccol=CCOL)
    pos = np.asarray(inp['positions']).astype(np.int32)
    c = f(inp['c'])
    maps = []
    nsl = T // TS
    for core in range(B * nsl):
        b, s = core // nsl, core % nsl
        m = dict(shared)
        npad = (nsl - 1 - s) * TS
        xTp = np.zeros((D, T), np.float32)
        xTp[:, npad:] = x[b, :(s + 1) * TS, :].T
        m['xT'] = xTp
        SBW_ = min(256, TS)
        vmk = np.zeros((128, T // SBW_), np.float32)
        vmk[:, npad // SBW_:] = 1.0
        m['vmask'] = vmk
        m['xs'] = np.ascontiguousarray(x[b, s * TS:(s + 1) * TS, :])
        posp = np.zeros((1, T), np.int32)
        posp[0, npad:] = pos[b, :(s + 1) * TS]
        m['pos'] = posp
        m['cT'] = np.ascontiguousarray(c[b].reshape(8, 128).T)
        sm = np.zeros((128, 8), np.float32)
        sm[:, s] = 1.0
        m['selm'] = sm
        maps.append(m)
    return maps


_NC_CACHE = {}


def _run(inp, T, TS):
    key = (T, TS)
    if key not in _NC_CACHE:
        _NC_CACHE[key] = build(T, TS, SBW=min(256, TS))
    nc = _NC_CACHE[key]
    maps = _host_inputs(inp, T, TS)
    res = run_bass_kernel_spmd(nc, maps, core_ids=list(range(len(maps))))
    B = np.asarray(inp['x']).shape[0]
    nsl = T // TS
    out = np.zeros((B, T, D), np.float32)
    for core in range(B * nsl):
        b, s = core // nsl, core % nsl
        out[b, s * TS:(s + 1) * TS, :] = np.asarray(res.results[core]['out'])
    return out


def kernel(**inputs):
    return _run(inputs, 8192, 2048)
```
